# Optimizing a Trainium2 kernel written in Bass

```python
import jax, jax.numpy as jnp
from jax import lax
import numpy as np

D_MODEL = 1024
BATCH = 2
SEQ = 16384
DEPTH = 4

GRID_W = 64
CTX_LEN = 256
HEAD_DIM = 64
N_HEADS = 8
N_KV_HEADS = 2
Q_PER_KV = N_HEADS // N_KV_HEADS
ATTN_WIDTH = N_HEADS * HEAD_DIM
KV_WIDTH = N_KV_HEADS * HEAD_DIM
WINDOW = 128
BLOCK = 128
ROPE_BASE = 10000.0
SGU_GROUPS = 4
SGU_GROUP_DIM = 64
SGU_WIDTH = SGU_GROUPS * SGU_GROUP_DIM
CHUNK = 128
FNET_GROUPS = 4
FNET_GROUP_DIM = 64
FNET_WIDTH = FNET_GROUPS * FNET_GROUP_DIM
N_BRANCHES = 3
IN_SPLITS = [ATTN_WIDTH,
             ATTN_WIDTH + KV_WIDTH,
             ATTN_WIDTH + 2 * KV_WIDTH,
             ATTN_WIDTH + 2 * KV_WIDTH + SGU_WIDTH,
             ATTN_WIDTH + 2 * KV_WIDTH + 2 * SGU_WIDTH,
             ATTN_WIDTH + 2 * KV_WIDTH + 2 * SGU_WIDTH + FNET_WIDTH]
IN_WIDTH = IN_SPLITS[-1] + N_BRANCHES * D_MODEL
N_EXPERTS = 16
CAPACITY_FACTOR = 2
EXPERT_FF = 2048
N_MOD = 6
EPS = 1e-6
NEG_INF = -1e30

kernel_name = "hybrid_gated_branch_diffusion_block"


def rms_norm(x, g):
    xf = x.astype(jnp.float32)
    y = xf * lax.rsqrt(jnp.mean(xf * xf, axis=-1, keepdims=True) + EPS)
    return (y * g.astype(jnp.float32)).astype(x.dtype)


def modulate(h, shift, scale):
    return h * (1 + scale) + shift


def axial_rope(rows):
    row = jnp.repeat(jnp.arange(rows, dtype=jnp.float32), GRID_W)
    col = jnp.tile(jnp.arange(GRID_W, dtype=jnp.float32), rows)
    pairs = HEAD_DIM // 4
    inv = ROPE_BASE ** (-jnp.arange(pairs, dtype=jnp.float32) / pairs)
    ang = jnp.concatenate([row[:, None] * inv, col[:, None] * inv], axis=-1)
    return jnp.cos(ang)[:, None, :], jnp.sin(ang)[:, None, :]


def apply_rope(x, cos, sin):
    cos = cos.astype(x.dtype)
    sin = sin.astype(x.dtype)
    x1, x2 = jnp.split(x, 2, axis=-1)
    return jnp.concatenate([x1 * cos - x2 * sin, x2 * cos + x1 * sin], axis=-1)


def context_attention(q, k, v, sink):
    b, l, _, _ = q.shape
    qg = q.reshape(b, l, N_KV_HEADS, Q_PER_KV, HEAD_DIM).astype(jnp.float32) * (HEAD_DIM ** -0.5)
    s = jnp.einsum('blkgd,bmkd->bkglm', qg, k.astype(jnp.float32))
    sk = jnp.broadcast_to(sink.astype(jnp.float32).reshape(1, N_KV_HEADS, Q_PER_KV, 1, 1), s.shape[:-1] + (1,))
    p = jax.nn.softmax(jnp.concatenate([s, sk], axis=-1), axis=-1)
    o = jnp.einsum('bkglm,bmkd->blkgd', p[..., :l], v.astype(jnp.float32))
    return o.reshape(b, l, ATTN_WIDTH).astype(q.dtype)


def window_attention(q, k, v, k_ctx, v_ctx, sink):
    b, n, _, _ = q.shape
    l = k_ctx.shape[1]
    nb = n // BLOCK
    qb = q.reshape(b, nb, BLOCK, N_KV_HEADS, Q_PER_KV, HEAD_DIM).astype(jnp.float32) * (HEAD_DIM ** -0.5)

    def band(t):
        tp = jnp.pad(t, ((0, 0), (BLOCK, BLOCK), (0, 0), (0, 0))).reshape(b, nb + 2, BLOCK, N_KV_HEADS, HEAD_DIM)
        return jnp.concatenate([tp[:, :-2], tp[:, 1:-1], tp[:, 2:]], axis=2).astype(jnp.float32)

    kb = band(k)
    vb = band(v)
    s_loc = jnp.einsum('bnqkgd,bnskd->bnkgqs', qb, kb)
    qi = jnp.arange(BLOCK)[:, None]
    kj = jnp.arange(3 * BLOCK)[None, :]
    in_win = jnp.abs(kj - BLOCK - qi) <= WINDOW
    key_abs = (jnp.arange(nb)[:, None] - 1) * BLOCK + jnp.arange(3 * BLOCK)[None, :]
    in_range = (key_abs >= 0) & (key_abs < n)
    mask = in_win[None] & in_range[:, None, :]
    s_loc = jnp.where(mask[None, :, None, None], s_loc, NEG_INF)
    s_ctx = jnp.einsum('bnqkgd,bmkd->bnkgqm', qb, k_ctx.astype(jnp.float32))
    sk = jnp.broadcast_to(sink.astype(jnp.float32).reshape(1, 1, N_KV_HEADS, Q_PER_KV, 1, 1), s_ctx.shape[:-1] + (1,))
    p = jax.nn.softmax(jnp.concatenate([s_loc, s_ctx, sk], axis=-1), axis=-1)
    o = (jnp.einsum('bnkgqs,bnskd->bnqkgd', p[..., :3 * BLOCK], vb)
         + jnp.einsum('bnkgqm,bmkd->bnqkgd', p[..., 3 * BLOCK:3 * BLOCK + l], v_ctx.astype(jnp.float32)))
    return o.reshape(b, n, ATTN_WIDTH).astype(q.dtype)


def chunk_sgu(u, v, w_s, b_s):
    b, n, _ = u.shape
    u = jax.nn.gelu(u)
    vg = jax.nn.gelu(v).reshape(b, n // CHUNK, CHUNK, SGU_GROUPS, SGU_GROUP_DIM).astype(jnp.float32)
    vg = vg * lax.rsqrt(jnp.mean(vg * vg, axis=-1, keepdims=True) + EPS)
    z = jnp.einsum('gij,bcjgd->bcigd', w_s.astype(jnp.float32), vg) + b_s.astype(jnp.float32).T[None, None, :, :, None]
    return u * z.reshape(b, n, SGU_WIDTH).astype(u.dtype)


def fourier_mix(f):
    b, n, _ = f.shape
    fg = f.reshape(b, n, FNET_GROUPS, FNET_GROUP_DIM).astype(jnp.float32)
    y = jnp.fft.fft2(fg, axes=(1, 3), norm='ortho').real
    return y.reshape(b, n, FNET_WIDTH).astype(f.dtype)


def merge_branches(a, s, f, gates, w_ba, w_bs, w_bf, w_o):
    g_a, g_s, g_f = jnp.split(jax.nn.sigmoid(gates), N_BRANCHES, axis=-1)
    m = g_a * (a @ w_ba) + g_s * (s @ w_bs) + g_f * (f @ w_bf)
    return m @ w_o


def expert_choice(h, w_router, w_gate, w_up, w_down):
    b, n, d = h.shape
    cap = CAPACITY_FACTOR * n // N_EXPERTS
    aff = jax.nn.softmax((h @ w_router).astype(jnp.float32), axis=-1)
    gval, idx = lax.top_k(jnp.swapaxes(aff, 1, 2), cap)
    xe = jax.vmap(lambda hb, ib: hb[ib])(h, idx)
    a = jnp.einsum('becd,edf->becf', xe, w_gate)
    u = jnp.einsum('becd,edf->becf', xe, w_up)
    y = jnp.einsum('becf,efd->becd', jax.nn.silu(a) * u, w_down) * gval[..., None].astype(h.dtype)
    return jax.vmap(lambda yb, ib: jnp.zeros((n, d), h.dtype).at[ib.reshape(-1)].add(yb.reshape(-1, d)))(y, idx)


def setup_inputs(seed: int = 0) -> dict:
    key = jax.random.key(seed)
    ks = jax.random.split(key, 21)

    def nrm(k, shape, scale):
        return jax.random.normal(k, shape, jnp.float32) * scale

    D = D_MODEL
    return {
        "x": nrm(ks[0], (BATCH, SEQ, D), 1.0),
        "c": nrm(ks[1], (BATCH, D), 1.0),
        "ctx": nrm(ks[2], (BATCH, CTX_LEN, D), 1.0),
        "c_ctx": nrm(ks[3], (D,), 1.0),
        "w_mod": nrm(ks[4], (DEPTH, D, N_MOD * D), 0.5 * D ** -0.5),
        "b_mod": nrm(ks[5], (DEPTH, N_MOD * D), 0.02),
        "g_mix": 1.0 + nrm(ks[6], (DEPTH, D), 0.02),
        "g_ffn": 1.0 + nrm(ks[7], (DEPTH, D), 0.02),
        "w_in": nrm(ks[8], (DEPTH, D, IN_WIDTH), D ** -0.5),
        "attn_sink": nrm(ks[9], (DEPTH, N_HEADS), 0.5),
        "w_spatial": nrm(ks[10], (DEPTH, SGU_GROUPS, CHUNK, CHUNK), CHUNK ** -0.5),
        "b_spatial": nrm(ks[11], (DEPTH, SGU_GROUPS, CHUNK), 0.02),
        "w_branch_attn": nrm(ks[12], (DEPTH, ATTN_WIDTH, D), ATTN_WIDTH ** -0.5),
        "w_branch_sgu": nrm(ks[13], (DEPTH, SGU_WIDTH, D), SGU_WIDTH ** -0.5),
        "w_branch_fourier": nrm(ks[14], (DEPTH, FNET_WIDTH, D), FNET_WIDTH ** -0.5),
        "w_out": nrm(ks[15], (DEPTH, D, D), D ** -0.5),
        "w_router": nrm(ks[16], (DEPTH, D, N_EXPERTS), D ** -0.5),
        "w_gate": nrm(ks[17], (DEPTH, N_EXPERTS, D, EXPERT_FF), D ** -0.5),
        "w_up": nrm(ks[18], (DEPTH, N_EXPERTS, D, EXPERT_FF), D ** -0.5),
        "w_down": nrm(ks[19], (DEPTH, N_EXPERTS, EXPERT_FF, D), EXPERT_FF ** -0.5),
        "g_final": 1.0 + nrm(ks[20], (D,), 0.02),
    }


def reference(x, c, ctx, c_ctx, w_mod, b_mod, g_mix, g_ffn, w_in, attn_sink, w_spatial, b_spatial,
              w_branch_attn, w_branch_sgu, w_branch_fourier, w_out, w_router, w_gate, w_up, w_down, g_final):
    b, n, _ = x.shape
    l_ctx = ctx.shape[1]
    rows = n // GRID_W
    cos, sin = axial_rope(rows)
    for layer in range(DEPTH):
        last = layer == DEPTH - 1
        mod_lat = (jax.nn.silu(c) @ w_mod[layer] + b_mod[layer])[:, None, :]
        mod_ctx = (jax.nn.silu(c_ctx) @ w_mod[layer] + b_mod[layer])[None, None, :]
        sh1, sc1, gt1, sh2, sc2, gt2 = jnp.split(mod_lat, N_MOD, axis=-1)
        csh1, csc1, cgt1, csh2, csc2, cgt2 = jnp.split(mod_ctx, N_MOD, axis=-1)

        h_ctx = modulate(rms_norm(ctx, g_mix[layer]), csh1, csc1)
        if last:
            kv_c = h_ctx @ w_in[layer][:, IN_SPLITS[0]:IN_SPLITS[2]]
            kc, vc = jnp.split(kv_c, 2, axis=-1)
        else:
            qc, kc, vc, uc, vsc, fc, gc = jnp.split(h_ctx @ w_in[layer], IN_SPLITS, axis=-1)
        kc = kc.reshape(b, l_ctx, N_KV_HEADS, HEAD_DIM)
        vc = vc.reshape(b, l_ctx, N_KV_HEADS, HEAD_DIM)
        if not last:
            a_c = context_attention(qc.reshape(b, l_ctx, N_HEADS, HEAD_DIM), kc, vc, attn_sink[layer])
            s_c = chunk_sgu(uc, vsc, w_spatial[layer], b_spatial[layer])
            f_c = fourier_mix(fc)
            ctx_new = ctx + cgt1 * merge_branches(a_c, s_c, f_c, gc, w_branch_attn[layer], w_branch_sgu[layer],
                                                  w_branch_fourier[layer], w_out[layer])
            h2c = modulate(rms_norm(ctx_new, g_ffn[layer]), csh2, csc2)
            ctx_new = ctx_new + cgt2 * expert_choice(h2c, w_router[layer], w_gate[layer], w_up[layer], w_down[layer])

        h = modulate(rms_norm(x, g_mix[layer]), sh1, sc1)
        q, k, v, u, vs, f, gates = jnp.split(h @ w_in[layer], IN_SPLITS, axis=-1)
        q = apply_rope(q.reshape(b, n, N_HEADS, HEAD_DIM), cos, sin)
        k = apply_rope(k.reshape(b, n, N_KV_HEADS, HEAD_DIM), cos, sin)
        v = v.reshape(b, n, N_KV_HEADS, HEAD_DIM)
        a = window_attention(q, k, v, kc, vc, attn_sink[layer])
        s = chunk_sgu(u, vs, w_spatial[layer], b_spatial[layer])
        fo = fourier_mix(f)
        x = x + gt1 * merge_branches(a, s, fo, gates, w_branch_attn[layer], w_branch_sgu[layer],
                                     w_branch_fourier[layer], w_out[layer])
        h2 = modulate(rms_norm(x, g_ffn[layer]), sh2, sc2)
        x = x + gt2 * expert_choice(h2, w_router[layer], w_gate[layer], w_up[layer], w_down[layer])

        if not last:
            ctx = ctx_new
    return rms_norm(x, g_final)
```

```python
import contextlib
import numpy as np
import concourse.bass as bass
import concourse.mybir as mybir
from concourse.bass import IndirectOffsetOnAxis
from concourse.bass_utils import run_bass_kernel_spmd

F32 = mybir.dt.float32
BF16 = mybir.dt.bfloat16
I32 = mybir.dt.int32
U32 = mybir.dt.uint32
AF = mybir.ActivationFunctionType
ALU = mybir.AluOpType
AX = mybir.AxisListType

D = 1024
NCTX = 256
NE = 16
FF = 2048
EPS = 1e-6
Q0, K0, V0, VS0, F0, U0, G0 = 0, 512, 640, 768, 1024, 1280, 1536
SAME_ENG_SYNC = True
DEBUG_ALLOC = False
NDQ = 8


import os
STOP = int(os.environ.get("K_STOP", "99"))
SUB = int(os.environ.get("K_SUB", "99"))
SUB2 = int(os.environ.get("K_SUB2", "99"))


class SkipPhase(Exception):
    pass


class Phase(contextlib.ExitStack):
    def __exit__(self, et, ev, tb):
        r = super().__exit__(et, ev, tb)
        return bool(r) or (et is SkipPhase)


class Buf:
    __slots__ = ("name", "w", "r")

    def __init__(self, name):
        self.name = name
        self.w = None
        self.r = []


class Tile:
    def __init__(self, t, name):
        self.t = t
        self.b = Buf(name)

    def __getitem__(self, idx):
        return self.t[idx]


class Multi:
    def __init__(self, tiles, name):
        self.tiles = tiles
        self.b = Buf(name)

    def __getitem__(self, idx):
        p, kc, c = idx
        return self.tiles[kc][p, c]


class Eng:
    def __init__(self, k, name, obj):
        self.name = name
        self.obj = obj
        self.sem = k.new_sem()
        self.cnt = 0
        self.waited = {}


class DQ:
    def __init__(self, k, engname):
        self.engname = engname
        self.sems = [k.new_sem() for _ in range(NDQ)]
        self.n = 0


class K:
    def __init__(self, nc, stack):
        self.nc = nc
        self.stack = stack
        self.nsem = 0
        self.semid = {}
        self.engs = {}
        for n, o in (("tensor", nc.tensor), ("vector", nc.vector), ("scalar", nc.scalar),
                     ("gpsimd", nc.gpsimd), ("sync", nc.sync)):
            self.engs[n] = Eng(self, n, o)
        self.dq = {"sync": DQ(self, "sync"), "gpsimd": DQ(self, "gpsimd")}
        self.uid = 0

    def new_sem(self):
        s = self.stack.enter_context(self.nc.semaphore("s%d" % self.nsem))
        self.semid[id(s)] = self.nsem
        self.nsem += 1
        return s

    def sb(self, stack, shape, dt, name=None):
        self.uid += 1
        nm = "%s_%d" % (name or "t", self.uid)
        t = stack.enter_context(self.nc.sbuf_tensor(nm, list(shape), dt))
        if DEBUG_ALLOC:
            print("ALLOC", nm, shape, dt)
        return Tile(t, nm)

    def _bufs(self, lst):
        out = []
        for x in lst:
            if x is None:
                continue
            out.append(x.b if hasattr(x, 'b') else x)
        return out

    def _wait(self, E, dep):
        sem, val, src = dep
        if src == E.name and (E.name == "tensor" or not SAME_ENG_SYNC):
            return
        sid = self.semid[id(sem)]
        if E.waited.get(sid, 0) >= val:
            return
        E.obj.wait_ge(sem, val)
        E.waited[sid] = val

    def _wait_deps(self, E, reads, writes):
        for b in reads:
            if b.w is not None:
                self._wait(E, b.w)
        for b in writes:
            if b.w is not None:
                self._wait(E, b.w)
            for d in b.r:
                self._wait(E, d)

    def _record(self, dep, reads, writes):
        for b in writes:
            b.w = dep
            b.r = []
        for b in reads:
            b.r.append(dep)
            if len(b.r) > 64:
                b.r = b.r[-64:]

    def op(self, e, fn, reads=(), writes=()):
        E = self.engs[e]
        reads = self._bufs(reads)
        writes = self._bufs(writes)
        self._wait_deps(E, reads, writes)
        ins = fn(E.obj)
        if E.cnt >= 30000:
            E.sem = self.new_sem()
            E.cnt = 0
        E.cnt += 1
        ins.then_inc(E.sem, 1)
        self._record((E.sem, E.cnt, e), reads, writes)

    def dma(self, q, fn, reads=(), writes=()):
        Q = self.dq[q]
        E = self.engs[Q.engname]
        reads = self._bufs(reads)
        writes = self._bufs(writes)
        self._wait_deps(E, reads, writes)
        i = Q.n % NDQ
        rnd = Q.n // NDQ
        if rnd > 0:
            self._wait(E, (Q.sems[i], 16 * rnd, "dma"))
        ins = fn(E.obj)
        ins.then_inc(Q.sems[i], 16)
        Q.n += 1
        self._record((Q.sems[i], 16 * (rnd + 1), "dma"), reads, writes)

    def barrier(self):
        deps = []
        for n, E in self.engs.items():
            if E.cnt > 0:
                deps.append((E.sem, E.cnt, "x"))
        for Q in self.dq.values():
            for i in range(NDQ):
                cnt = (Q.n - i + NDQ - 1) // NDQ
                if cnt > 0:
                    deps.append((Q.sems[i], 16 * cnt, "dma"))
        for E in self.engs.values():
            for d in deps:
                self._wait(E, d)


def _consts(NB):
    NL = NB * 128
    NT = NL + NCTX
    c = {}
    c["ident_bf"] = np.eye(128, dtype=np.float32)
    c["ident_f"] = np.eye(128, dtype=np.float32)
    rot = np.zeros((128, 128), np.float32)
    for p in range(128):
        d = p % 64
        if d < 32:
            rot[p + 32, p] = -1.0
        else:
            rot[p - 32, p] = 1.0
    c["rotT"] = rot
    pos = np.arange(NL)
    row = (pos // 64).astype(np.float64)
    col = (pos % 64).astype(np.float64)
    inv = 10000.0 ** (-np.arange(16, dtype=np.float64) / 16)
    ang = np.concatenate([row[:, None] * inv, col[:, None] * inv], axis=-1)
    cosT = np.ones((128, NT), np.float32)
    sinT = np.zeros((128, NT), np.float32)
    for p in range(128):
        j = p % 32
        cosT[p, :NL] = np.cos(ang[:, j].astype(np.float32))
        sinT[p, :NL] = np.sin(ang[:, j].astype(np.float32))
    c["cosT"] = cosT
    c["sinT"] = sinT
    kk = np.arange(128)[:, None]
    ii = np.arange(128)[None, :]
    c["maskP"] = (kk >= ii).astype(np.float32)
    c["maskN"] = (kk <= ii).astype(np.float32)
    n1 = np.arange(NB)
    angA = 2 * np.pi * np.outer(n1, n1) / NB
    c["CA"] = np.cos(angA).astype(np.float32)
    c["nSA"] = (-np.sin(angA)).astype(np.float32)
    n2 = np.arange(128)
    angT = 2 * np.pi * np.outer(n2, n1) / NL
    c["Tr"] = np.cos(angT).astype(np.float32)
    c["Ti"] = (-np.sin(angT)).astype(np.float32)
    angC = 2 * np.pi * np.outer(n2, n2) / 128
    c["C128"] = np.cos(angC).astype(np.float32)
    c["S128"] = np.sin(angC).astype(np.float32)
    c["nS128"] = (-np.sin(angC)).astype(np.float32)
    dd = np.arange(64)
    angD = 2 * np.pi * np.outer(dd, dd) / 64
    c["CD"] = (np.cos(angD) / np.sqrt(64.0 * NL)).astype(np.float32)
    c["SD"] = (np.sin(angD) / np.sqrt(64.0 * NL)).astype(np.float32)
    c["CDc"] = (np.cos(angD) / np.sqrt(64.0 * NCTX)).astype(np.float32)
    c["SDc"] = (np.sin(angD) / np.sqrt(64.0 * NCTX)).astype(np.float32)
    nn = np.arange(NCTX)
    ang256 = 2 * np.pi * np.outer(nn, nn) / NCTX
    c["C256"] = np.cos(ang256).astype(np.float32).reshape(2, 128, NCTX).transpose(1, 0, 2).copy()
    c["nS256"] = (-np.sin(ang256)).astype(np.float32).reshape(2, 128, NCTX).transpose(1, 0, 2).copy()
    c["iota"] = np.tile(np.arange(2048, dtype=np.float32)[None, :], (128, 1))
    c["pcol"] = np.arange(128, dtype=np.float32)[:, None].copy()
    c["slotcol"] = (np.arange(128, dtype=np.float32)[:, None] + 128.0 * np.arange(16)[None, :]).astype(np.float32)
    c["ustrict"] = (np.arange(128)[:, None] < np.arange(128)[None, :]).astype(np.float32)
    c["ones_f"] = np.ones((128, 128), np.float32)
    return c


BF_CONSTS = ["ident_bf", "rotT", "maskP", "maskN", "CA", "nSA", "C128", "S128", "nS128",
             "CD", "SD", "CDc", "SDc", "C256", "nS256"]


def build(NB, DEPTH, dbg=False):
    NL = NB * 128
    NT = NL + NCTX
    CAP = NL // 8
    NS = max(1, CAP // 128)
    SP = min(128, CAP)
    assert CAP % SP == 0
    nc = bass.Bass("TRN2", target_bir_lowering=False)
    consts = _consts(NB)

    def din(name, shape, dt=F32):
        return nc.dram_tensor(name, list(shape), dt, kind="ExternalInput").ap()

    def dscr(name, shape, dt):
        kind = "ExternalOutput" if dbg else "Internal"
        return nc.dram_tensor(name, list(shape), dt, kind=kind).ap()

    x_in = din("x", [NL, D])
    ctx_in = din("ctx", [NCTX, D])
    cvec = din("cvec", [128, 8, 2])
    w_mod = din("w_mod", [DEPTH, D, 6 * D])
    b_modfm = din("b_modfm", [DEPTH, 128, 48])
    g_mixfm = din("g_mixfm", [DEPTH, 128, 8])
    g_ffnfm = din("g_ffnfm", [DEPTH, 128, 8])
    w_in = din("w_in", [DEPTH, D, 4608])
    sink = din("sink", [DEPTH, 8])
    wsT = din("wsT", [DEPTH, 128, 4, 128])
    b_sp = din("b_sp", [DEPTH, 4 * 128])
    w_ba = din("w_ba", [DEPTH, 64, 8, D])
    w_bs = din("w_bs", [DEPTH, 64, 4, D])
    w_bf = din("w_bf", [DEPTH, 64, 4, D])
    w_o = din("w_o", [DEPTH, D, D])
    w_r = din("w_r", [DEPTH, D, NE])
    if STOP < 6:
        w_g = din("w_g", [1, 1, 8, 8])
        w_u = din("w_u", [1, 1, 8, 8])
        w_d = din("w_d", [1, 1, 8, 8])
    else:
        w_g = din("w_g", [DEPTH, NE, D, FF])
        w_u = din("w_u", [DEPTH, NE, D, FF])
        w_d = din("w_d", [DEPTH, NE, FF, D])
    g_fin = din("g_fin", [D])
    cin = {k: din("c_" + k, list(v.shape)) for k, v in consts.items()}
    y_out = nc.dram_tensor("y", [NL, D], F32, kind="ExternalOutput").ap()

    R = dscr("R", [NT, D], F32)
    QT = dscr("QT", [4, 128, NT], BF16)
    KT = dscr("KT", [128, NT], BF16)
    Vd = dscr("Vd", [NT, 128], BF16)
    FX = dscr("FX", [4, NT, 64], BF16)
    GT = dscr("GT", [24, 128, NT], BF16)
    AT = dscr("AT", [8, 64, NT], BF16)
    ST = dscr("ST", [4, 64, NT], BF16)
    FY = dscr("FY", [4, 64, NT], BF16)
    H2 = dscr("H2", [NT, D], BF16)
    AFF = dscr("AFF", [NT, NE], F32)
    MODROW = dscr("MODROW", [12, D], F32)

    with contextlib.ExitStack() as top:
        k = K(nc, top)
        op = k.op
        dma = k.dma
        PS = []
        for i in range(8):
            t = top.enter_context(nc.psum_tensor("ps%d" % i, [128, 512], F32))
            PS.append(Tile(t, "ps%d" % i))

        C = {}
        stg = [k.sb(top, [128, 2048], F32, "stg") for _ in range(3)]
        cast_engs = ["gpsimd", "vector", "scalar"]
        stgi = [0]

        def load_cast(dst_ap, dst_tile, src_ap, shape):
            s = stg[stgi[0] % 3]
            ce = cast_engs[stgi[0] % len(cast_engs)]
            stgi[0] += 1
            p = shape[0]
            n = int(np.prod(shape[1:]))
            sv = s[0:p, 0:n]
            if len(shape) == 3:
                sv = sv.rearrange("p (a b) -> p a b", a=shape[1])
            dma("sync", lambda e: e.dma_start(out=sv, in_=src_ap), writes=[s])
            if ce == "scalar":
                op("scalar", lambda e: e.copy(out=dst_ap, in_=sv), reads=[s], writes=[dst_tile])
            else:
                op(ce, lambda e: e.tensor_copy(out=dst_ap, in_=sv), reads=[s], writes=[dst_tile])

        def load_f32(dst_ap, dst_tile, src_ap, q="sync", **kw):
            dma(q, lambda e: e.dma_start(out=dst_ap, in_=src_ap, **kw), writes=[dst_tile])

        for name, v in consts.items():
            shp = list(v.shape)
            if name in ("cosT", "sinT"):
                continue
            if name in BF_CONSTS:
                t = k.sb(top, shp, BF16, name)
                load_cast(t[:], t, cin[name], shp)
            else:
                t = k.sb(top, shp, F32, name)
                load_f32(t[:], t, cin[name])
            C[name] = t
        ones_bf = k.sb(top, [128, 64], BF16, "ones_bf")
        op("vector", lambda e: e.memset(ones_bf[:], 1.0), writes=[ones_bf])
        eps_t = k.sb(top, [128, 1], F32, "eps")
        op("vector", lambda e: e.memset(eps_t[:], EPS), writes=[eps_t])
        cv = k.sb(top, [128, 8, 2], F32, "cv")
        load_f32(cv[:], cv, cvec)
        csil = k.sb(top, [128, 8, 2], BF16, "csil")
        op("scalar", lambda e: e.activation(out=csil[:], in_=cv[:], func=AF.Silu), reads=[cv], writes=[csil])
        idx_t = k.sb(top, [128, NE * NS], I32, "idx")
        gwc = k.sb(top, [128, 2, NE], F32, "gwc")

        dma("sync", lambda e: e.dma_start(out=R[0:NL, :], in_=x_in[:, :]))
        dma("sync", lambda e: e.dma_start(out=R[NL:NT, :], in_=ctx_in[:, :]))
        k.barrier()

        def rms_rstd(st, xt, ss, sq, rstd, junk):
            op("scalar", lambda e: e.activation(out=junk[:], in_=xt[:], func=AF.Square, accum_out=ss[:]),
               reads=[xt], writes=[junk, ss])
            op("scalar", lambda e: e.activation(out=sq[:], in_=ss[:], func=AF.Sqrt, scale=1.0 / D, bias=eps_t[:]),
               reads=[ss, eps_t], writes=[sq])
            op("vector", lambda e: e.reciprocal(out=rstd[:], in_=sq[:]), reads=[sq], writes=[rstd])

        groups = [(g * 512, 512, 0) for g in range(NL // 512)] + [(NL, NCTX, 1)]

        for L in range(DEPTH):
            last = (L == DEPTH - 1)
            with Phase() as ph:
                if STOP < 0:
                    raise SkipPhase()
                modfm = k.sb(ph, [128, 48, 2], F32, "modfm")
                bm = k.sb(ph, [128, 48], F32, "bm")
                load_f32(bm[:], bm, b_modfm[L])
                gm = k.sb(ph, [128, 8], F32, "gm")
                gf = k.sb(ph, [128, 8], F32, "gf")
                load_f32(gm[:], gm, g_mixfm[L])
                load_f32(gf[:], gf, g_ffnfm[L])
                wm = [k.sb(ph, [128, 8, 1024], BF16, "wm") for _ in range(2)]
                pm = PS[0]
                pmv = pm[:, 0:96].rearrange("p (j v) -> p j v", v=2)
                for pc in range(6):
                    w = wm[pc % 2]
                    for kp in range(4):
                        src = w_mod[L, kp * 256:(kp + 1) * 256, pc * 1024:(pc + 1) * 1024].rearrange(
                            "(a p) n -> p a n", p=128)
                        load_cast(w[:, 2 * kp:2 * kp + 2, :], w, src, [128, 2, 1024])
                    for jj in range(8):
                        j = pc * 8 + jj
                        for kc in range(8):
                            op("tensor", lambda e, w=w, jj=jj, kc=kc, j=j: e.matmul(
                                pmv[:, j, :], w[:, kc, jj * 128:(jj + 1) * 128], csil[:, kc, :],
                                start=(kc == 0), stop=(kc == 7)), reads=[w, csil], writes=[pm])
                op("vector", lambda e: e.tensor_tensor(out=modfm[:], in0=pmv,
                                                       in1=bm[:].unsqueeze(2).to_broadcast([128, 48, 2]), op=ALU.add),
                   reads=[pm, bm], writes=[modfm])
                rows = k.sb(ph, [128, 6, 2, 8], F32, "rows")

                def mv(which):
                    return modfm[:, which * 8:(which + 1) * 8, :].rearrange("p k v -> p v k")

                for r, (sc_i, g_t) in ((0, (1, gm)), (3, (4, gf))):
                    op("vector", lambda e, r=r, sc_i=sc_i, g_t=g_t: e.scalar_tensor_tensor(
                        out=rows[:, r, :, :], in0=mv(sc_i), scalar=1.0, op0=ALU.add,
                        in1=g_t[:].unsqueeze(1).to_broadcast([128, 2, 8]), op1=ALU.mult),
                       reads=[modfm, g_t], writes=[rows])
                for r, wi in ((1, 0), (2, 2), (4, 3), (5, 5)):
                    op("vector", lambda e, r=r, wi=wi: e.tensor_copy(out=rows[:, r, :, :], in_=mv(wi)),
                       reads=[modfm], writes=[rows])
                pr = PS[1]
                op("tensor", lambda e: e.transpose(pr[0:96, 0:128], rows[:].rearrange("p r v k -> p (r v k)"),
                                                   C["ident_f"][:]), reads=[rows, C["ident_f"]], writes=[pr])
                rowsT = k.sb(ph, [96, 128], F32, "rowsT")
                op("vector", lambda e: e.tensor_copy(out=rowsT[:], in_=pr[0:96, 0:128]), reads=[pr], writes=[rowsT])
                dma("sync", lambda e: e.dma_start(out=MODROW.rearrange("r (kc p) -> (r kc) p", p=128), in_=rowsT[:]),
                    reads=[rowsT])
                k.barrier()

            def load_rows(ph, rlist):
                out = {}
                for r in rlist:
                    for v in range(2):
                        t = k.sb(ph, [128, D], F32, "row%d_%d" % (r, v))
                        load_f32(t[:], t, MODROW[r * 2 + v, :].partition_broadcast(128))
                        out[(r, v)] = t
                return out

            with Phase() as ph:
                if STOP < 1:
                    raise SkipPhase()
                win = Multi([k.sb(ph, [128, 4608], BF16, "win") for _ in range(8)], "win")
                for kc in range(8):
                    for c0 in (0, 2048, 4096):
                        cw = min(2048, 4608 - c0)
                        load_cast(win[:, kc, c0:c0 + cw], win, w_in[L, kc * 128:(kc + 1) * 128, c0:c0 + cw], [128, cw])
                rw = load_rows(ph, [0, 1])
                wst = k.sb(ph, [128, 4, 128], BF16, "wst")
                load_cast(wst[:], wst, wsT[L], [128, 4, 128])
                bsb = k.sb(ph, [64, 4, 128], F32, "bsb")
                load_f32(bsb[:].rearrange("p g i -> p (g i)"), bsb, b_sp[L, :].partition_broadcast(64))
                xt2 = [k.sb(ph, [128, D], F32, "xt") for _ in range(2)]
                t1 = k.sb(ph, [128, D], F32, "t1")
                junk = t1
                hb4 = [k.sb(ph, [128, D], BF16, "hb") for _ in range(4)]
                hT2 = [k.sb(ph, [128, 8, 512], BF16, "hT") for _ in range(2)]
                ss = k.sb(ph, [128, 1], F32, "ss")
                sq = k.sb(ph, [128, 1], F32, "sq")
                rstd = k.sb(ph, [128, 1], F32, "rstd")
                cs_t = [k.sb(ph, [128, 512], F32, "cos") for _ in range(2)]
                sn_t = [k.sb(ph, [128, 512], F32, "sin") for _ in range(2)]
                qf2 = [k.sb(ph, [128, 512], F32, "qf") for _ in range(2)]
                qb2 = [k.sb(ph, [128, 512], BF16, "qb") for _ in range(2)]
                r1 = k.sb(ph, [128, 512], F32, "r1")
                r2 = k.sb(ph, [128, 512], F32, "r2")
                qo2 = [k.sb(ph, [128, 512], BF16, "qo") for _ in range(3)]
                ug = k.sb(ph, [64, 4, 512], BF16, "ug")
                vb2 = [k.sb(ph, [128, 128], BF16, "vb") for _ in range(2)]
                fb2 = [k.sb(ph, [128, 256], BF16, "fb") for _ in range(2)]
                gv = k.sb(ph, [128, 4, 64], F32, "gv")
                gsq = k.sb(ph, [128, 4, 64], F32, "gsq")
                ms = k.sb(ph, [128, 4], F32, "ms")
                ms2 = k.sb(ph, [128, 4], F32, "ms2")
                rs = k.sb(ph, [128, 4], F32, "rs")
                vn2 = [k.sb(ph, [128, 4, 64], BF16, "vn") for _ in range(2)]
                tz = k.sb(ph, [64, 4, 128], F32, "tz")
                so2 = [k.sb(ph, [64, 4, 128], BF16, "so") for _ in range(2)]
                qoi = [0]

                def prepA(gi):
                    tok0, GW, v = groups[gi]
                    cs = cs_t[gi % 2]
                    sn = sn_t[gi % 2]
                    load_f32(cs[:, 0:GW], cs, cin["cosT"][:, tok0:tok0 + GW])
                    load_f32(sn[:, 0:GW], sn, cin["sinT"][:, tok0:tok0 + GW])
                    for j in range(GW // 128):
                        xt = xt2[j % 2]
                        hb = hb4[j]
                        r0 = tok0 + j * 128
                        load_f32(xt[:], xt, R[r0:r0 + 128, :])
                        rms_rstd(ph, xt, ss, sq, rstd, junk)
                        op("vector", lambda e, xt=xt, v=v: e.scalar_tensor_tensor(
                            out=t1[:], in0=xt[:], scalar=rstd[:, 0:1], op0=ALU.mult, in1=rw[(0, v)][:], op1=ALU.mult),
                           reads=[xt, rstd, rw[(0, v)]], writes=[t1])
                        op("vector", lambda e, hb=hb, v=v: e.tensor_tensor(out=hb[:], in0=t1[:], in1=rw[(1, v)][:],
                                                                          op=ALU.add),
                           reads=[t1, rw[(1, v)]], writes=[hb])

                def prepB(gi):
                    tok0, GW, v = groups[gi]
                    hT = hT2[gi % 2]
                    for j in range(GW // 128):
                        hb = hb4[j]
                        pt = PS[j % 2]
                        ptv = pt[:, :].bitcast(BF16)
                        for kc in range(8):
                            op("tensor", lambda e, kc=kc, ptv=ptv, hb=hb: e.transpose(
                                ptv[:, kc * 128:(kc + 1) * 128], hb[:, kc * 128:(kc + 1) * 128], C["ident_bf"][:]),
                               reads=[hb, C["ident_bf"]], writes=[pt])
                        op("scalar", lambda e, ptv=ptv, j=j, hT=hT: e.copy(
                            out=hT[:, :, j * 128:(j + 1) * 128], in_=ptv.rearrange("p (k t) -> p k t", k=8)),
                           reads=[pt], writes=[hT])

                prepA(0)
                prepB(0)
                for gi, (tok0, GW, v) in enumerate(groups):
                    ntile = GW // 128
                    hT = hT2[gi % 2]
                    cs = cs_t[gi % 2]
                    sn = sn_t[gi % 2]
                    if gi + 1 < len(groups):
                        prepA(gi + 1)
                    fmc = [0]

                    def fm_mm(col0, ncol, pq):
                        for kc in range(8):
                            op("tensor", lambda e, kc=kc: e.matmul(
                                pq[0:ncol, 0:GW], win[:, kc, col0:col0 + ncol], hT[:, kc, 0:GW],
                                start=(kc == 0), stop=(kc == 7)), reads=[win, hT], writes=[pq])

                    def next_pq():
                        pq = PS[2 + (fmc[0] % 2)]
                        fmc[0] += 1
                        return pq

                    def job_rope(ci):
                        pq = next_pq()
                        fm_mm(Q0 + ci * 128, 128, pq)
                        qf = qf2[ci % 2]
                        qb = qb2[ci % 2]
                        op("scalar", lambda e: e.copy(out=qf[:, 0:GW], in_=pq[:, 0:GW]), reads=[pq], writes=[qf])
                        op("vector", lambda e: e.tensor_copy(out=qb[:, 0:GW], in_=qf[:, 0:GW]), reads=[qf], writes=[qb])

                        def follow():
                            prr = PS[4]
                            op("tensor", lambda e: e.matmul(prr[:, 0:GW], C["rotT"][:], qb[:, 0:GW], start=True,
                                                            stop=True), reads=[C["rotT"], qb], writes=[prr])
                            op("vector", lambda e: e.tensor_tensor(out=r1[:, 0:GW], in0=qf[:, 0:GW], in1=cs[:, 0:GW],
                                                                   op=ALU.mult), reads=[qf, cs], writes=[r1])
                            op("vector", lambda e: e.tensor_tensor(out=r2[:, 0:GW], in0=prr[:, 0:GW], in1=sn[:, 0:GW],
                                                                   op=ALU.mult), reads=[prr, sn], writes=[r2])
                            qo = qo2[qoi[0] % 3]
                            qoi[0] += 1
                            op("vector", lambda e: e.tensor_tensor(out=qo[:, 0:GW], in0=r1[:, 0:GW], in1=r2[:, 0:GW],
                                                                   op=ALU.add), reads=[r1, r2], writes=[qo])
                            dst = QT[ci, :, tok0:tok0 + GW] if ci < 4 else KT[:, tok0:tok0 + GW]
                            dma("sync", lambda e: e.dma_start(out=dst, in_=qo[:, 0:GW]), reads=[qo])
                        return follow, 1

                    def job_gate(j):
                        pq = next_pq()
                        fm_mm(G0 + j * 128, 128, pq)
                        qo = qo2[qoi[0] % 3]
                        qoi[0] += 1
                        op("scalar", lambda e: e.activation(out=qo[:, 0:GW], in_=pq[:, 0:GW], func=AF.Sigmoid),
                           reads=[pq], writes=[qo])
                        dma("sync", lambda e: e.dma_start(out=GT[j, :, tok0:tok0 + GW], in_=qo[:, 0:GW]), reads=[qo])
                        return None, 0

                    def job_u(g):
                        pq = next_pq()
                        fm_mm(U0 + g * 64, 64, pq)
                        op("scalar", lambda e: e.activation(out=ug[:, g, 0:GW], in_=pq[0:64, 0:GW],
                                                            func=AF.Gelu_apprx_tanh), reads=[pq], writes=[ug])
                        return None, 0

                    def job_tm(j):
                        r0 = tok0 + j * 128
                        pa = PS[5]
                        pb = PS[6]
                        for kc in range(8):
                            op("tensor", lambda e, kc=kc: e.matmul(
                                pa[:, 0:384], hT[:, kc, j * 128:(j + 1) * 128], win[:, kc, V0:V0 + 384],
                                start=(kc == 0), stop=(kc == 7)), reads=[win, hT], writes=[pa])
                        for kc in range(8):
                            op("tensor", lambda e, kc=kc: e.matmul(
                                pb[:, 0:256], hT[:, kc, j * 128:(j + 1) * 128], win[:, kc, F0:F0 + 256],
                                start=(kc == 0), stop=(kc == 7)), reads=[win, hT], writes=[pb])
                        vb = vb2[j % 2]
                        fb = fb2[j % 2]
                        vn = vn2[j % 2]
                        op("scalar", lambda e: e.copy(out=vb[:], in_=pa[:, 0:128]), reads=[pa], writes=[vb])
                        dma("sync", lambda e: e.dma_start(out=Vd[r0:r0 + 128, :], in_=vb[:]), reads=[vb])
                        op("scalar", lambda e: e.copy(out=fb[:], in_=pb[:, 0:256]), reads=[pb], writes=[fb])
                        dma("sync", lambda e: e.dma_start(
                            out=FX[:, r0:r0 + 128, :].rearrange("g t d -> t g d"),
                            in_=fb[:].rearrange("p (g d) -> p g d", g=4)), reads=[fb])
                        op("scalar", lambda e: e.activation(out=gv[:].rearrange("p g d -> p (g d)"), in_=pa[:, 128:384],
                                                            func=AF.Gelu_apprx_tanh), reads=[pa], writes=[gv])
                        op("vector", lambda e: e.tensor_tensor(out=gsq[:], in0=gv[:], in1=gv[:], op=ALU.mult),
                           reads=[gv], writes=[gsq])
                        op("vector", lambda e: e.tensor_reduce(out=ms[:], in_=gsq[:], axis=AX.X, op=ALU.add),
                           reads=[gsq], writes=[ms])
                        op("scalar", lambda e: e.activation(out=ms2[:], in_=ms[:], func=AF.Sqrt, scale=1.0 / 64,
                                                            bias=eps_t[:]), reads=[ms, eps_t], writes=[ms2])
                        op("vector", lambda e: e.reciprocal(out=rs[:], in_=ms2[:]), reads=[ms2], writes=[rs])
                        op("vector", lambda e: e.tensor_tensor(out=vn[:], in0=gv[:],
                                                               in1=rs[:].unsqueeze(2).to_broadcast([128, 4, 64]),
                                                               op=ALU.mult), reads=[gv, rs], writes=[vn])

                        def follow():
                            pz = PS[7]
                            for g in range(4):
                                op("tensor", lambda e, g=g: e.matmul(pz[0:64, g * 128:(g + 1) * 128], vn[:, g, :],
                                                                     wst[:, g, :], start=True, stop=True),
                                   reads=[vn, wst], writes=[pz])
                            op("vector", lambda e: e.tensor_tensor(
                                out=tz[:], in0=pz[0:64, :].rearrange("p (g i) -> p g i", g=4), in1=bsb[:], op=ALU.add),
                               reads=[pz, bsb], writes=[tz])
                            so = so2[j % 2]
                            op("vector", lambda e: e.tensor_tensor(out=so[:], in0=tz[:],
                                                                   in1=ug[:, :, j * 128:(j + 1) * 128], op=ALU.mult),
                               reads=[tz, ug], writes=[so])
                            dma("sync", lambda e: e.dma_start(
                                out=ST[:, :, r0:r0 + 128].rearrange("g d t -> d g t"), in_=so[:]), reads=[so])
                        return follow, 2

                    jobs = [(job_rope, ci) for ci in range(5)] + [(job_u, g) for g in range(4)]
                    gper = 24 // ntile
                    for j in range(ntile):
                        jobs += [(job_gate, jj) for jj in range(j * gper, (j + 1) * gper)]
                        jobs.append((job_tm, j))
                    pendf = []
                    for n_, (fn_, arg_) in enumerate(jobs):
                        fo, dl = fn_(arg_)
                        due = [p for p in pendf if p[0] <= n_]
                        pendf = [p for p in pendf if p[0] > n_]
                        for p in due:
                            p[1]()
                        if fo is not None:
                            pendf.append((n_ + dl, fo))
                    for p in pendf:
                        p[1]()
                    if gi + 1 < len(groups):
                        prepB(gi + 1)
                k.barrier()

            with Phase() as ph:
                if STOP < 2:
                    raise SkipPhase()
                sk = k.sb(ph, [64, 8], F32, "sk")
                load_f32(sk[:], sk, sink[L, :].partition_broadcast(64))
                se = k.sb(ph, [64, 8], F32, "se")
                op("scalar", lambda e: e.activation(out=se[:], in_=sk[:], func=AF.Exp), reads=[sk], writes=[se])
                sexp = k.sb(ph, [64, 8, 128], F32, "sexp")
                op("vector", lambda e: e.tensor_copy(out=sexp[:], in_=se[:].unsqueeze(2).to_broadcast([64, 8, 128])),
                   reads=[se], writes=[sexp])
                kc_t = k.sb(ph, [128, NCTX], BF16, "kctx")
                vc_t = k.sb(ph, [128, 2, 128], BF16, "vctx")
                load_f32(kc_t[:], kc_t, KT[:, NL:NT])
                load_f32(vc_t[:], vc_t, Vd[NL:NT, :].rearrange("(b p) d -> p b d", p=128))
                q2 = [k.sb(ph, [128, 4, 512], BF16, "q4") for _ in range(2)]
                k2 = [k.sb(ph, [128, 768], BF16, "k6") for _ in range(2)]
                v2 = [k.sb(ph, [128, 6, 128], BF16, "v6") for _ in range(2)]
                pt2 = [k.sb(ph, [128, 512], BF16, "PT") for _ in range(4)]
                den = k.sb(ph, [64, 512], F32, "den")
                rden = k.sb(ph, [64, 512], F32, "rden")
                ao2 = [k.sb(ph, [64, 4, 128], BF16, "ao") for _ in range(2)]
                pti = 0
                aoi = [0]
                psi = 0
                agroups = [gr for gr in groups if not (gr[2] == 1 and last)]

                def p2_loads(gi):
                    tok0, GW, v = agroups[gi]
                    q4 = q2[gi % 2]
                    load_f32(q4[:, :, 0:GW], q4, QT[:, :, tok0:tok0 + GW].rearrange("c p t -> p c t"))
                    if v == 0:
                        k6 = k2[gi % 2]
                        v6 = v2[gi % 2]
                        lo = max(tok0 - 128, 0)
                        hi = min(tok0 + GW + 128, NL)
                        off = lo - (tok0 - 128)
                        load_f32(k6[:, off:off + hi - lo], k6, KT[:, lo:hi])
                        load_f32(v6[:, off // 128:(off + hi - lo) // 128, :], v6,
                                 Vd[lo:hi, :].rearrange("(b p) d -> p b d", p=128))

                p2_loads(0)
                pend = []
                jobc = 0

                def emit_pv(it):
                    (bi, nb_, g, par, kb, PT, tok0_, qi_) = it
                    kt_, kcol, vt_, vblk, msk = kb
                    po = PS[3 + 2 * par]
                    pd = PS[4 + 2 * par]
                    st = (bi == 0)
                    sp = (bi == nb_ - 1)
                    op("tensor", lambda e: e.matmul(
                        po[0:64, :], vt_[:, vblk, 64 * g:64 * g + 64], PT[:], start=st, stop=sp),
                       reads=[vt_, PT], writes=[po])
                    op("tensor", lambda e: e.matmul(
                        pd[0:64, :], ones_bf[:, 0:64], PT[:], start=st, stop=sp),
                       reads=[ones_bf, PT], writes=[pd])
                    if sp:
                        op("vector", lambda e: e.tensor_tensor(
                            out=den[:], in0=pd[0:64, :],
                            in1=sexp[:, 4 * g:4 * g + 4, :].rearrange("p c q -> p (c q)"), op=ALU.add),
                           reads=[pd, sexp], writes=[den])
                        op("vector", lambda e: e.reciprocal(out=rden[:], in_=den[:]), reads=[den], writes=[rden])
                        ao = ao2[aoi[0] % 2]
                        aoi[0] += 1
                        op("vector", lambda e: e.tensor_tensor(
                            out=ao[:].rearrange("p c q -> p (c q)"), in0=po[0:64, :], in1=rden[:], op=ALU.mult),
                           reads=[po, rden], writes=[ao])
                        r0 = tok0_ + qi_ * 128
                        dma("sync", lambda e: e.dma_start(
                            out=AT[4 * g:4 * g + 4, :, r0:r0 + 128].rearrange("h d t -> d h t"), in_=ao[:]),
                            reads=[ao])

                for gi, (tok0, GW, v) in enumerate(agroups):
                    nq = GW // 128
                    q4 = q2[gi % 2]
                    k6 = k2[gi % 2]
                    v6 = v2[gi % 2]
                    while pend:
                        emit_pv(pend.pop(0))
                    if gi + 1 < len(agroups):
                        p2_loads(gi + 1)
                    for g in range(2):
                        for qi in range(nq):
                            nblk = tok0 // 128 + qi
                            par = jobc % 2
                            jobc += 1
                            kbs = []
                            if v == 0:
                                if nblk > 0:
                                    kbs.append((k6, qi * 128, v6, qi, "P"))
                                kbs.append((k6, (qi + 1) * 128, v6, qi + 1, None))
                                if nblk < NB - 1:
                                    kbs.append((k6, (qi + 2) * 128, v6, qi + 2, "N"))
                            kbs.append((kc_t, 0, vc_t, 0, None))
                            kbs.append((kc_t, 128, vc_t, 1, None))
                            for bi, kb in enumerate(kbs):
                                kt_, kcol, vt_, vblk, msk = kb
                                psS = PS[psi % 3]
                                psi += 1
                                op("tensor", lambda e, kt_=kt_, kcol=kcol, psS=psS, g=g, qi=qi: e.matmul(
                                    psS[:, :].rearrange("p (c q) -> p c q", c=4),
                                    kt_[64 * g:64 * g + 64, kcol:kcol + 128],
                                    q4[64 * g:64 * g + 64, :, qi * 128:(qi + 1) * 128], start=True, stop=True),
                                   reads=[kt_, q4], writes=[psS])
                                PT = pt2[pti % 4]
                                pti += 1
                                op("scalar", lambda e, psS=psS, PT=PT: e.activation(out=PT[:], in_=psS[:, :], func=AF.Exp,
                                                                                    scale=0.125), reads=[psS], writes=[PT])
                                if msk is not None:
                                    mt = C["maskP"] if msk == "P" else C["maskN"]
                                    op("vector", lambda e, PT=PT, mt=mt: e.tensor_tensor(
                                        out=PT[:].rearrange("p (c q) -> p c q", c=4),
                                        in0=PT[:].rearrange("p (c q) -> p c q", c=4),
                                        in1=mt[:].unsqueeze(1).to_broadcast([128, 4, 128]), op=ALU.mult),
                                       reads=[PT, mt], writes=[PT])
                                pend.append((bi, len(kbs), g, par, kb, PT, tok0, qi))
                                if len(pend) > 2:
                                    emit_pv(pend.pop(0))
                while pend:
                    emit_pv(pend.pop(0))
                k.barrier()

            with Phase() as ph:
                if STOP < 3:
                    raise SkipPhase()
                Xg = k.sb(ph, [NB, 128, 64], BF16, "Xg")
                Bre = k.sb(ph, [128, 64, NB], BF16, "Bre")
                Bim = k.sb(ph, [128, 64, NB], BF16, "Bim")
                ZrT = k.sb(ph, [64, NL], BF16, "ZrT")
                ZiT = k.sb(ph, [64, NL], BF16, "ZiT")
                tw = [k.sb(ph, [128, 4, NB], F32, "tw") for _ in range(4)]
                yo2 = [k.sb(ph, [64, 512], BF16, "yo") for _ in range(2)]
                Trb = C["Tr"][:].unsqueeze(1).to_broadcast([128, 4, NB])
                Tib = C["Ti"][:].unsqueeze(1).to_broadcast([128, 4, NB])
                KG = max(1, min(4, NB))
                for g in range(4):
                    dma("sync", lambda e, g=g: e.dma_start(out=Xg[:], in_=FX[g, 0:NL, :].rearrange(
                        "(a b) c -> a b c", b=128)), writes=[Xg])
                    for cq in range(16):
                        par = PS[0 + (cq % 2) * 2]
                        pai = PS[1 + (cq % 2) * 2]
                        parv = par[:, 0:4 * NB].rearrange("p (c k) -> p c k", c=4)
                        paiv = pai[:, 0:4 * NB].rearrange("p (c k) -> p c k", c=4)
                        for cc in range(4):
                            ch = cq * 4 + cc
                            op("tensor", lambda e, ch=ch, cc=cc, parv=parv, par=par: e.matmul(
                                parv[:, cc, :], Xg[0:NB, :, ch], C["CA"][0:NB, 0:NB], start=True, stop=True),
                               reads=[Xg, C["CA"]], writes=[par])
                            op("tensor", lambda e, ch=ch, cc=cc, paiv=paiv, pai=pai: e.matmul(
                                paiv[:, cc, :], Xg[0:NB, :, ch], C["nSA"][0:NB, 0:NB], start=True, stop=True),
                               reads=[Xg, C["nSA"]], writes=[pai])
                        c0 = cq * 4
                        op("vector", lambda e, parv=parv, par=par: e.tensor_tensor(out=tw[0][:], in0=parv, in1=Trb,
                                                                                   op=ALU.mult),
                           reads=[par, C["Tr"]], writes=[tw[0]])
                        op("vector", lambda e, paiv=paiv, pai=pai: e.tensor_tensor(out=tw[1][:], in0=paiv, in1=Tib,
                                                                                   op=ALU.mult),
                           reads=[pai, C["Ti"]], writes=[tw[1]])
                        op("gpsimd", lambda e, c0=c0: e.tensor_tensor(out=Bre[:, c0:c0 + 4, :], in0=tw[0][:], in1=tw[1][:],
                                                                      op=ALU.subtract),
                           reads=[tw[0], tw[1]], writes=[Bre])
                        op("vector", lambda e, parv=parv, par=par: e.tensor_tensor(out=tw[2][:], in0=parv, in1=Tib,
                                                                                   op=ALU.mult),
                           reads=[par, C["Ti"]], writes=[tw[2]])
                        op("vector", lambda e, paiv=paiv, pai=pai: e.tensor_tensor(out=tw[3][:], in0=paiv, in1=Trb,
                                                                                   op=ALU.mult),
                           reads=[pai, C["Tr"]], writes=[tw[3]])
                        op("gpsimd", lambda e, c0=c0: e.tensor_tensor(out=Bim[:, c0:c0 + 4, :], in0=tw[2][:], in1=tw[3][:],
                                                                      op=ALU.add),
                           reads=[tw[2], tw[3]], writes=[Bim])
                    Zrv = ZrT[:].rearrange("d (k2 k1) -> d k1 k2", k1=NB)
                    Ziv = ZiT[:].rearrange("d (k2 k1) -> d k1 k2", k1=NB)
                    for kg in range(NB // KG):
                        pzr = PS[4 + (kg % 2) * 2]
                        pzi = PS[5 + (kg % 2) * 2]
                        for j in range(KG):
                            k1 = kg * KG + j
                            sl = slice(j * 128, (j + 1) * 128)
                            op("tensor", lambda e, k1=k1, sl=sl, pzr=pzr: e.matmul(
                                pzr[0:64, sl], Bre[:, :, k1], C["C128"][:], start=True, stop=False),
                               reads=[Bre, C["C128"]], writes=[pzr])
                            op("tensor", lambda e, k1=k1, sl=sl, pzr=pzr: e.matmul(
                                pzr[0:64, sl], Bim[:, :, k1], C["S128"][:], start=False, stop=True),
                               reads=[Bim, C["S128"]], writes=[pzr])
                            op("tensor", lambda e, k1=k1, sl=sl, pzi=pzi: e.matmul(
                                pzi[0:64, sl], Bim[:, :, k1], C["C128"][:], start=True, stop=False),
                               reads=[Bim, C["C128"]], writes=[pzi])
                            op("tensor", lambda e, k1=k1, sl=sl, pzi=pzi: e.matmul(
                                pzi[0:64, sl], Bre[:, :, k1], C["nS128"][:], start=False, stop=True),
                               reads=[Bre, C["nS128"]], writes=[pzi])
                        op("scalar", lambda e, kg=kg, pzr=pzr: e.copy(
                            out=Zrv[:, kg * KG:(kg + 1) * KG, :],
                            in_=pzr[0:64, 0:KG * 128].rearrange("p (j q) -> p j q", j=KG)), reads=[pzr], writes=[ZrT])
                        op("scalar", lambda e, kg=kg, pzi=pzi: e.copy(
                            out=Ziv[:, kg * KG:(kg + 1) * KG, :],
                            in_=pzi[0:64, 0:KG * 128].rearrange("p (j q) -> p j q", j=KG)), reads=[pzi], writes=[ZiT])
                    for ti in range(NL // 512):
                        py = PS[0 + (ti % 2)]
                        sl = slice(ti * 512, (ti + 1) * 512)
                        op("tensor", lambda e, py=py, sl=sl: e.matmul(py[0:64, :], C["CD"][:], ZrT[:, sl], start=True,
                                                                      stop=False), reads=[C["CD"], ZrT], writes=[py])
                        op("tensor", lambda e, py=py, sl=sl: e.matmul(py[0:64, :], C["SD"][:], ZiT[:, sl], start=False,
                                                                      stop=True), reads=[C["SD"], ZiT], writes=[py])
                        yo = yo2[ti % 2]
                        op("scalar", lambda e, py=py, yo=yo: e.copy(out=yo[:], in_=py[0:64, :]), reads=[py], writes=[yo])
                        dma("sync", lambda e, yo=yo, sl=sl, g=g: e.dma_start(out=FY[g, :, sl], in_=yo[:]), reads=[yo])
                if not last:
                    Xc = k.sb(ph, [128, 2, 4, 64], BF16, "Xc")
                    for j in range(2):
                        dma("sync", lambda e, j=j: e.dma_start(
                            out=Xc[:, j, :, :], in_=FX[:, NL + j * 128:NL + (j + 1) * 128, :].rearrange("g t d -> t g d")),
                            writes=[Xc])
                    zc = [k.sb(ph, [64, NCTX], BF16, "zc") for _ in range(2)]
                    for g in range(4):
                        pzr = PS[2]
                        pzi = PS[3]
                        for j in range(2):
                            op("tensor", lambda e, j=j, g=g: e.matmul(pzr[0:64, 0:NCTX], Xc[:, j, g, :], C["C256"][:, j, :],
                                                                      start=(j == 0), stop=(j == 1)),
                               reads=[Xc, C["C256"]], writes=[pzr])
                            op("tensor", lambda e, j=j, g=g: e.matmul(pzi[0:64, 0:NCTX], Xc[:, j, g, :], C["nS256"][:, j, :],
                                                                      start=(j == 0), stop=(j == 1)),
                               reads=[Xc, C["nS256"]], writes=[pzi])
                        op("scalar", lambda e: e.copy(out=zc[0][:], in_=pzr[0:64, 0:NCTX]), reads=[pzr], writes=[zc[0]])
                        op("scalar", lambda e: e.copy(out=zc[1][:], in_=pzi[0:64, 0:NCTX]), reads=[pzi],
                           writes=[zc[1]])
                        py = PS[0]
                        op("tensor", lambda e: e.matmul(py[0:64, 0:NCTX], C["CDc"][:], zc[0][:], start=True, stop=False),
                           reads=[C["CDc"], zc[0]], writes=[py])
                        op("tensor", lambda e: e.matmul(py[0:64, 0:NCTX], C["SDc"][:], zc[1][:], start=False, stop=True),
                           reads=[C["SDc"], zc[1]], writes=[py])
                        yo = yo2[g % 2]
                        op("scalar", lambda e, yo=yo: e.copy(out=yo[:, 0:NCTX], in_=py[0:64, 0:NCTX]), reads=[py],
                           writes=[yo])
                        dma("sync", lambda e, yo=yo, g=g: e.dma_start(out=FY[g, :, NL:NT], in_=yo[:, 0:NCTX]), reads=[yo])
                k.barrier()

            with Phase() as ph:
                if STOP < 4:
                    raise SkipPhase()
                wb = k.sb(ph, [64, 16, D], BF16, "wb")
                for h0 in (0, 2, 4, 6):
                    load_cast(wb[:, h0:h0 + 2, :], wb, w_ba[L, :, h0:h0 + 2, :], [64, 2, D])
                for h0 in (0, 2):
                    load_cast(wb[:, 8 + h0:10 + h0, :], wb, w_bs[L, :, h0:h0 + 2, :], [64, 2, D])
                    load_cast(wb[:, 12 + h0:14 + h0, :], wb, w_bf[L, :, h0:h0 + 2, :], [64, 2, D])
                wo = k.sb(ph, [128, 8, D], BF16, "wo")
                for kp in range(4):
                    load_cast(wo[:, 2 * kp:2 * kp + 2, :], wo,
                              w_o[L, kp * 256:(kp + 1) * 256, :].rearrange("(a p) n -> p a n", p=128), [128, 2, D])
                wr = k.sb(ph, [128, 8, NE], F32, "wr")
                load_f32(wr[:], wr, w_r[L].rearrange("(a p) n -> p a n", p=128))
                rw = load_rows(ph, [2, 3, 4])
                Bt2 = [k.sb(ph, [64, 16, 512], BF16, "Bt") for _ in range(2)]
                Gt3 = [k.sb(ph, [128, 3, 512], BF16, "Gt") for _ in range(3)]
                m1 = k.sb(ph, [128, 512], F32, "m1")
                m2 = k.sb(ph, [128, 512], F32, "m2")
                m3 = k.sb(ph, [128, 512], F32, "m3")
                mT = k.sb(ph, [128, 8, 512], BF16, "mT")
                xt2 = [k.sb(ph, [128, D], F32, "xt") for _ in range(2)]
                xm2 = [k.sb(ph, [128, D], F32, "xm") for _ in range(2)]
                tt = k.sb(ph, [128, D], F32, "tt")
                junk = k.sb(ph, [128, D], BF16, "junk")
                h2f2 = [k.sb(ph, [128, D], F32, "h2f") for _ in range(2)]
                h2b2 = [k.sb(ph, [128, D], BF16, "h2b")] * 2
                h2T = k.sb(ph, [128, 8, 128], F32, "h2T")
                ss = k.sb(ph, [128, 1], F32, "ss")
                sq = k.sb(ph, [128, 1], F32, "sq")
                rstd = k.sb(ph, [128, 1], F32, "rstd")
                mx = k.sb(ph, [128, 1], F32, "mx")
                nmx = k.sb(ph, [128, 1], F32, "nmx")
                ex = k.sb(ph, [128, NE], F32, "ex")
                sm = k.sb(ph, [128, 1], F32, "sm")
                rsm = k.sb(ph, [128, 1], F32, "rsm")
                af2 = [k.sb(ph, [128, NE], F32, "af") for _ in range(2)]
                tix = 0
                agroups = [gr for gr in groups if not (gr[2] == 1 and last)]
                GTv = GT.rearrange("(b o) p t -> o p b t", b=3)

                def p5_loadB(gi):
                    tok0, GW, v = agroups[gi]
                    Bt = Bt2[gi % 2]
                    load_f32(Bt[:, 0:8, 0:GW], Bt, AT[:, :, tok0:tok0 + GW].rearrange("h d t -> d h t"))
                    load_f32(Bt[:, 8:12, 0:GW], Bt, ST[:, :, tok0:tok0 + GW].rearrange("h d t -> d h t"))
                    load_f32(Bt[:, 12:16, 0:GW], Bt, FY[:, :, tok0:tok0 + GW].rearrange("h d t -> d h t"))

                gseq = [(gi, oc) for gi in range(len(agroups)) for oc in range(8)]

                def p5_loadG(qi_):
                    gi, oc = gseq[qi_]
                    tok0, GW, v = agroups[gi]
                    gt = Gt3[qi_ % 3]
                    load_f32(gt[:, :, 0:GW], gt, GTv[oc, :, :, tok0:tok0 + GW])

                def stageB(info):
                    h2f, af, r0 = info
                    for hf in range(2):
                        pt = PS[4 + hf]
                        for kk in range(4):
                            kc = hf * 4 + kk
                            op("tensor", lambda e, pt=pt, kk=kk, kc=kc: e.transpose(
                                pt[:, kk * 128:(kk + 1) * 128], h2f[:, kc * 128:(kc + 1) * 128], C["ident_f"][:]),
                               reads=[h2f, C["ident_f"]], writes=[pt])
                        op("scalar", lambda e, pt=pt, hf=hf: e.copy(
                            out=h2T[:, hf * 4:(hf + 1) * 4, :], in_=pt[:, :].rearrange("p (k t) -> p k t", k=4)),
                           reads=[pt], writes=[h2T])
                    pl = PS[6]
                    for kc in range(8):
                        op("tensor", lambda e, kc=kc: e.matmul(pl[:, 0:NE], h2T[:, kc, :], wr[:, kc, :],
                                                               start=(kc == 0), stop=(kc == 7)),
                           reads=[h2T, wr], writes=[pl])
                    op("vector", lambda e: e.reduce_max(out=mx[:], in_=pl[:, 0:NE], axis=AX.X), reads=[pl], writes=[mx])
                    op("vector", lambda e: e.tensor_scalar(out=nmx[:], in0=mx[:], scalar1=-1.0, scalar2=None,
                                                           op0=ALU.mult), reads=[mx], writes=[nmx])
                    op("scalar", lambda e: e.activation(out=ex[:], in_=pl[:, 0:NE], func=AF.Exp, bias=nmx[:],
                                                        accum_out=sm[:]), reads=[pl, nmx], writes=[ex, sm])
                    op("vector", lambda e: e.reciprocal(out=rsm[:], in_=sm[:]), reads=[sm], writes=[rsm])
                    op("vector", lambda e: e.tensor_scalar(out=af[:], in0=ex[:], scalar1=rsm[:, 0:1],
                                                           scalar2=None, op0=ALU.mult),
                       reads=[ex, rsm], writes=[af])
                    dma("sync", lambda e: e.dma_start(out=AFF[r0:r0 + 128, :], in_=af[:]), reads=[af])

                p5_loadB(0)
                p5_loadG(0)
                gq = 0
                for gi, (tok0, GW, v) in enumerate(agroups):
                    ntile = GW // 128
                    Bt = Bt2[gi % 2]
                    if gi + 1 < len(agroups):
                        p5_loadB(gi + 1)
                    for oc in range(8):
                        Gt = Gt3[gq % 3]
                        if gq + 1 < len(gseq):
                            p5_loadG(gq + 1)
                        gq += 1
                        osl = slice(oc * 128, (oc + 1) * 128)
                        pA, pS_, pF = PS[0 + (oc % 2) * 3], PS[1 + (oc % 2) * 3], PS[2 + (oc % 2) * 3]
                        for (pp, h0, nh) in ((pA, 0, 8), (pS_, 8, 4), (pF, 12, 4)):
                            for hh in range(nh):
                                op("tensor", lambda e, pp=pp, h0=h0, hh=hh, nh=nh, osl=osl: e.matmul(
                                    pp[:, 0:GW], wb[:, h0 + hh, osl], Bt[:, h0 + hh, 0:GW],
                                    start=(hh == 0), stop=(hh == nh - 1)), reads=[wb, Bt], writes=[pp])
                        op("vector", lambda e, pA=pA, Gt=Gt: e.tensor_tensor(out=m1[:, 0:GW], in0=pA[:, 0:GW],
                                                                             in1=Gt[:, 0, 0:GW], op=ALU.mult),
                           reads=[pA, Gt], writes=[m1])
                        op("vector", lambda e, pS_=pS_, Gt=Gt: e.tensor_tensor(out=m2[:, 0:GW], in0=pS_[:, 0:GW],
                                                                               in1=Gt[:, 1, 0:GW], op=ALU.mult),
                           reads=[pS_, Gt], writes=[m2])
                        op("vector", lambda e, pF=pF, Gt=Gt: e.tensor_tensor(out=m3[:, 0:GW], in0=pF[:, 0:GW],
                                                                             in1=Gt[:, 2, 0:GW], op=ALU.mult),
                           reads=[pF, Gt], writes=[m3])
                        op("gpsimd", lambda e: e.tensor_tensor(out=m1[:, 0:GW], in0=m1[:, 0:GW], in1=m2[:, 0:GW],
                                                               op=ALU.add), reads=[m1, m2], writes=[m1])
                        op("gpsimd", lambda e, oc=oc: e.tensor_tensor(out=mT[:, oc, 0:GW], in0=m1[:, 0:GW],
                                                                      in1=m3[:, 0:GW], op=ALU.add),
                           reads=[m1, m3], writes=[mT])
                    pend = None
                    for j in range(ntile):
                        r0 = tok0 + j * 128
                        xt = xt2[tix % 2]
                        xm = xm2[tix % 2]
                        h2b = h2b2[tix % 2]
                        af = af2[tix % 2]
                        h2f = h2f2[tix % 2]
                        pob = (tix % 2) * 2
                        tix += 1
                        load_f32(xt[:], xt, R[r0:r0 + 128, :])
                        for hf in range(2):
                            hs = slice(hf * 512, (hf + 1) * 512)
                            po = PS[pob + hf]
                            for kc in range(8):
                                op("tensor", lambda e, kc=kc, j=j, hs=hs, po=po: e.matmul(
                                    po[:, :], mT[:, kc, j * 128:(j + 1) * 128], wo[:, kc, hs],
                                    start=(kc == 0), stop=(kc == 7)), reads=[mT, wo], writes=[po])
                            op("vector", lambda e, po=po, hs=hs: e.tensor_tensor(out=tt[:, hs], in0=po[:, :],
                                                                                 in1=rw[(2, v)][:, hs], op=ALU.mult),
                               reads=[po, rw[(2, v)]], writes=[tt])
                        op("gpsimd", lambda e, xm=xm, xt=xt: e.tensor_tensor(out=xm[:], in0=tt[:], in1=xt[:], op=ALU.add),
                           reads=[tt, xt], writes=[xm])
                        dma("sync", lambda e, xm=xm, r0=r0: e.dma_start(out=R[r0:r0 + 128, :], in_=xm[:]), reads=[xm])
                        rms_rstd(ph, xm, ss, sq, rstd, junk)
                        op("vector", lambda e, xm=xm: e.scalar_tensor_tensor(
                            out=tt[:], in0=xm[:], scalar=rstd[:, 0:1], op0=ALU.mult, in1=rw[(3, v)][:], op1=ALU.mult),
                           reads=[xm, rstd, rw[(3, v)]], writes=[tt])
                        op("vector", lambda e, h2f=h2f: e.tensor_tensor(out=h2f[:], in0=tt[:], in1=rw[(4, v)][:],
                                                                        op=ALU.add),
                           reads=[tt, rw[(4, v)]], writes=[h2f])
                        op("gpsimd", lambda e, h2b=h2b, h2f=h2f: e.tensor_copy(out=h2b[:], in_=h2f[:]), reads=[h2f],
                           writes=[h2b])
                        dma("sync", lambda e, h2b=h2b, r0=r0: e.dma_start(out=H2[r0:r0 + 128, :], in_=h2b[:]), reads=[h2b])
                        if pend is not None:
                            stageB(pend)
                        pend = (h2f, af, r0)
                    if pend is not None:
                        stageB(pend)
                k.barrier()

            with Phase() as ph:
                if STOP < 5:
                    raise SkipPhase()
                def bisect(A3, np_, inner, cap, sumfn, tag):
                    lo = k.sb(ph, [np_, NE], F32, "lo" + tag)
                    hi = k.sb(ph, [np_, NE], F32, "hi" + tag)
                    mid = k.sb(ph, [np_, NE], F32, "mid" + tag)
                    Mk = k.sb(ph, [np_, inner, NE], F32, "Mk" + tag)
                    cnt = k.sb(ph, [np_, NE], F32, "cnt" + tag)
                    ge = k.sb(ph, [np_, NE], F32, "ge" + tag)
                    tmp = k.sb(ph, [np_, NE], F32, "tmp" + tag)
                    op("vector", lambda e: e.memset(lo[:], 0.0), writes=[lo])
                    op("vector", lambda e: e.memset(hi[:], 1.0), writes=[hi])
                    pc = PS[0]
                    for it in range(34):
                        op("vector", lambda e: e.tensor_tensor(out=mid[:], in0=lo[:], in1=hi[:], op=ALU.add),
                           reads=[lo, hi], writes=[mid])
                        op("vector", lambda e: e.tensor_scalar(out=mid[:], in0=mid[:], scalar1=0.5, scalar2=None,
                                                               op0=ALU.mult), reads=[mid], writes=[mid])
                        op("vector", lambda e: e.tensor_tensor(out=Mk[:], in0=A3,
                                                               in1=mid[:].unsqueeze(1).to_broadcast([np_, inner, NE]),
                                                               op=ALU.is_ge), reads=[mid, Abuf], writes=[Mk])
                        op("vector", lambda e: e.tensor_reduce(out=cnt[:], in_=Mk[:].rearrange("p i e -> p e i"),
                                                               axis=AX.X, op=ALU.add), reads=[Mk], writes=[cnt])
                        op("tensor", lambda e: e.matmul(pc[0:np_, 0:NE], C["ones_f"][0:np_, 0:np_], cnt[:],
                                                        start=True, stop=True), reads=[cnt, C["ones_f"]], writes=[pc])
                        op("vector", lambda e: e.tensor_scalar(out=ge[:], in0=pc[0:np_, 0:NE], scalar1=float(cap),
                                                               scalar2=None, op0=ALU.is_ge), reads=[pc], writes=[ge])
                        op("vector", lambda e: e.tensor_tensor(out=tmp[:], in0=ge[:], in1=mid[:], op=ALU.mult),
                           reads=[ge, mid], writes=[tmp])
                        op("vector", lambda e: e.tensor_tensor(out=lo[:], in0=lo[:], in1=tmp[:], op=ALU.max),
                           reads=[lo, tmp], writes=[lo])
                        op("vector", lambda e: e.scalar_tensor_tensor(out=tmp[:], in0=ge[:], scalar=2.0, op0=ALU.mult,
                                                                      in1=mid[:], op1=ALU.add),
                           reads=[ge, mid], writes=[tmp])
                        op("vector", lambda e: e.tensor_tensor(out=hi[:], in0=hi[:], in1=tmp[:], op=ALU.min),
                           reads=[hi, tmp], writes=[hi])
                    return lo, Mk

                A = k.sb(ph, [NB, 128, NE], F32, "A")
                Abuf = A
                load_f32(A[:], A, AFF[0:NL, :].rearrange("(t i) e -> t i e", i=128))
                thr, Mk = bisect(A[:], NB, 128, CAP, None, "l")
                op("vector", lambda e: e.tensor_tensor(out=Mk[:], in0=A[:],
                                                       in1=thr[:].unsqueeze(1).to_broadcast([NB, 128, NE]), op=ALU.is_ge),
                   reads=[A, thr], writes=[Mk])
                Mk2 = k.sb(ph, [NB, 128, NE], F32, "Mk2")
                cur, oth = Mk, Mk2
                dstep = 1
                while dstep < 128:
                    op("vector", lambda e, cur=cur, oth=oth, d=dstep: e.tensor_copy(out=oth[:, 0:d, :], in_=cur[:, 0:d, :]),
                       reads=[cur], writes=[oth])
                    op("vector", lambda e, cur=cur, oth=oth, d=dstep: e.tensor_tensor(
                        out=oth[:, d:128, :], in0=cur[:, d:128, :], in1=cur[:, 0:128 - d, :], op=ALU.add),
                       reads=[cur], writes=[oth])
                    cur, oth = oth, cur
                    dstep *= 2
                CL = cur
                CLe = k.sb(ph, [NB, NE, 128], F32, "CLe")
                op("vector", lambda e: e.tensor_copy(out=CLe[:], in_=CL[:].rearrange("p i e -> p e i")), reads=[CL],
                   writes=[CLe])
                cntl = k.sb(ph, [NB, NE], F32, "cntl")
                op("vector", lambda e: e.tensor_copy(out=cntl[:], in_=CL[:, 127, :]), reads=[CL], writes=[cntl])
                pe_ = PS[1]
                op("tensor", lambda e: e.matmul(pe_[0:NB, 0:NE], C["ustrict"][0:NB, 0:NB], cntl[:], start=True, stop=True),
                   reads=[cntl, C["ustrict"]], writes=[pe_])
                meta = k.sb(ph, [NB, NE, 2], F32, "meta")
                INC = k.sb(ph, [NB, NE], F32, "INC")
                op("vector", lambda e: e.tensor_copy(out=meta[:, :, 0], in_=pe_[0:NB, 0:NE]), reads=[pe_], writes=[meta])
                op("vector", lambda e: e.tensor_copy(out=meta[:, :, 1], in_=C["pcol"][0:NB, 0:1].to_broadcast([NB, NE])),
                   reads=[C["pcol"]], writes=[meta])
                op("vector", lambda e: e.tensor_tensor(out=INC[:], in0=meta[:, :, 0], in1=cntl[:], op=ALU.add),
                   reads=[meta, cntl], writes=[INC])
                oh1 = k.sb(ph, [NB, 128], F32, "oh1")
                oh2 = [k.sb(ph, [NB, 128], F32, "oh") for _ in range(2)]
                rr = k.sb(ph, [128, 1], F32, "rr")
                Vt = k.sb(ph, [NB, 128], F32, "Vt")
                il = k.sb(ph, [128, 1], F32, "il")
                idf = k.sb(ph, [128, 1], F32, "idf")
                jk = k.sb(ph, [128, 128], F32, "jk")
                def p6_front(ii, ex_, S):
                    oh = oh2[ii % 2]
                    pp = PS[2 + (ii % 2)]
                    io = C["iota"][0:NB, S * SP:S * SP + SP]
                    op("vector", lambda e: e.tensor_scalar(
                        out=oh1[:, 0:SP], in0=io, scalar1=meta[:, ex_, 0:1], scalar2=None, op0=ALU.is_ge),
                       reads=[C["iota"], meta], writes=[oh1])
                    op("vector", lambda e: e.scalar_tensor_tensor(
                        out=oh[:, 0:SP], in0=io, scalar=INC[:, ex_:ex_ + 1], op0=ALU.is_lt, in1=oh1[:, 0:SP],
                        op1=ALU.mult), reads=[C["iota"], INC, oh1], writes=[oh])
                    op("tensor", lambda e: e.matmul(
                        pp[0:SP, 0:128], oh[:, 0:SP], CLe[:, ex_, :], start=True, stop=True),
                       reads=[oh, CLe], writes=[pp])
                    op("tensor", lambda e: e.matmul(
                        pp[0:SP, 128:130], oh[:, 0:SP], meta[:, ex_, :], start=True, stop=True),
                       reads=[oh, meta], writes=[pp])
                    op("vector", lambda e: e.scalar_tensor_tensor(
                        out=Vt[:, 0:SP], in0=io, scalar=meta[:, ex_, 0:1], op0=ALU.subtract, in1=oh[:, 0:SP],
                        op1=ALU.mult), reads=[C["iota"], meta, oh], writes=[Vt])
                    op("tensor", lambda e: e.matmul(
                        pp[0:SP, 130:131], Vt[:, 0:SP], C["ones_f"][0:NB, 0:1], start=True, stop=True),
                       reads=[Vt, C["ones_f"]], writes=[pp])

                def p6_back(ii, ex_, S):
                    pp = PS[2 + (ii % 2)]
                    op("vector", lambda e: e.tensor_copy(out=rr[0:SP, :], in_=pp[0:SP, 130:131]),
                       reads=[pp], writes=[rr])
                    op("vector", lambda e: e.tensor_scalar(
                        out=jk[0:SP, :], in0=pp[0:SP, 0:128], scalar1=rr[0:SP, 0:1], scalar2=0.0, op0=ALU.is_le,
                        op1=ALU.add, accum_out=il[0:SP, :]), reads=[pp, rr], writes=[jk, il])
                    op("vector", lambda e: e.scalar_tensor_tensor(
                        out=idf[0:SP, :], in0=pp[0:SP, 129:130], scalar=128.0, op0=ALU.mult, in1=il[0:SP, :],
                        op1=ALU.add), reads=[pp, il], writes=[idf])
                    col = ex_ * NS + S
                    op("vector", lambda e: e.tensor_copy(out=idx_t[0:SP, col:col + 1], in_=idf[0:SP, :]),
                       reads=[idf], writes=[idx_t])

                p6items = [(ii, ex_, S) for ii, (ex_, S) in enumerate((a, b) for a in range(NE) for b in range(NS))]
                p6_front(*p6items[0])
                for n_, it_ in enumerate(p6items):
                    if n_ + 1 < len(p6items):
                        p6_front(*p6items[n_ + 1])
                    p6_back(*it_)
                if not last:
                    Ac = k.sb(ph, [128, 2, NE], F32, "Ac")
                    Abuf = Ac
                    load_f32(Ac[:], Ac, AFF[NL:NT, :].rearrange("(t i) e -> i t e", i=128))
                    thc, Mc = bisect(Ac[:], 128, 2, 32, None, "c")
                    op("vector", lambda e: e.tensor_tensor(out=Mc[:], in0=Ac[:],
                                                           in1=thc[:].unsqueeze(1).to_broadcast([128, 2, NE]),
                                                           op=ALU.is_ge), reads=[Ac, thc], writes=[Mc])
                    op("vector", lambda e: e.tensor_tensor(out=gwc[:], in0=Mc[:], in1=Ac[:], op=ALU.mult),
                       reads=[Mc, Ac], writes=[gwc])
                k.barrier()

            with Phase() as ph:
                if STOP < 6:
                    raise SkipPhase()
                cast_engs[:] = ["vector", "scalar"]
                wgh = [k.sb(ph, [128, 8, FF // 2], BF16, "wg") for _ in range(2)]
                wuh = [k.sb(ph, [128, 8, FF // 2], BF16, "wu") for _ in range(2)]
                wd2 = k.sb(ph, [128, 16, D], BF16, "wd")
                rw = load_rows(ph, [5])
                xe2 = [k.sb(ph, [128, D], BF16, "xe") for _ in range(2)]
                ae2 = [k.sb(ph, [128, NE], F32, "ae") for _ in range(8)]
                xeT = k.sb(ph, [128, 8, 512], BF16, "xeT")
                sa = k.sb(ph, [128, 512], F32, "sa")
                hTt = k.sb(ph, [128, 16, 512], BF16, "hTt")
                y2 = [k.sb(ph, [128, D], F32, "y") for _ in range(2)]
                cacc = k.sb(ph, [128, 2, D], F32, "cacc")
                op("gpsimd", lambda e: e.memset(cacc[:], 0.0), writes=[cacc])
                Rbuf = Buf("Rscatter")
                tgroups = []
                for s0 in range(0, NS, 4):
                    tgroups.append([("L", s) for s in range(s0, min(NS, s0 + 4))])
                if not last:
                    tgroups.append([("C", 0), ("C", 1)])
                yi = 0
                xi = 0
                for ex_ in range(NE):
                    for hh_ in range(2):
                        hsl = slice(hh_ * (FF // 2), (hh_ + 1) * (FF // 2))
                        for kp in range(4):
                            rsl = slice(kp * 256, (kp + 1) * 256)
                            load_cast(wgh[hh_][:, 2 * kp:2 * kp + 2, :], wgh[hh_],
                                      w_g[L, ex_, rsl, hsl].rearrange("(a p) n -> p a n", p=128), [128, 2, FF // 2])
                            load_cast(wuh[hh_][:, 2 * kp:2 * kp + 2, :], wuh[hh_],
                                      w_u[L, ex_, rsl, hsl].rearrange("(a p) n -> p a n", p=128), [128, 2, FF // 2])
                    for kp in range(8):
                        load_cast(wd2[:, 2 * kp:2 * kp + 2, :], wd2,
                                  w_d[L, ex_, kp * 256:(kp + 1) * 256, :].rearrange("(a p) n -> p a n", p=128),
                                  [128, 2, D])
                    for tg in tgroups:
                        GWt = 0
                        gates_ = []
                        for j, (kind, s) in enumerate(tg):
                            xe = xe2[xi % 2]
                            ae = ae2[xi % 8]
                            xi += 1
                            if kind == "L":
                                np_ = SP
                                col = ex_ * NS + s
                                off = IndirectOffsetOnAxis(ap=idx_t[0:SP, col:col + 1], axis=0)
                                dma("gpsimd", lambda e, xe=xe, off=off: e.indirect_dma_start(
                                    out=xe[0:SP, :], out_offset=None, in_=H2[:, :], in_offset=off),
                                    reads=[idx_t], writes=[xe])
                                dma("gpsimd", lambda e, ae=ae, off=off: e.indirect_dma_start(
                                    out=ae[0:SP, :], out_offset=None, in_=AFF[:, :], in_offset=off),
                                    reads=[idx_t], writes=[ae])
                                gates_.append((ae, ae[0:SP, ex_:ex_ + 1], np_, kind, col))
                            else:
                                np_ = 128
                                load_f32(xe[:], xe, H2[NL + s * 128:NL + (s + 1) * 128, :])
                                gates_.append((gwc, gwc[:, s, ex_:ex_ + 1], np_, kind, s))
                            pt = PS[j % 2]
                            ptv = pt[:, :].bitcast(BF16)
                            for kc in range(8):
                                op("tensor", lambda e, kc=kc, ptv=ptv, xe=xe, np_=np_: e.transpose(
                                    ptv[:, kc * 128:kc * 128 + np_], xe[0:np_, kc * 128:(kc + 1) * 128],
                                    C["ident_bf"][0:np_, 0:np_]), reads=[xe, C["ident_bf"]], writes=[pt])
                            op("scalar", lambda e, ptv=ptv, np_=np_, GWt=GWt: e.copy(
                                out=xeT[:, :, GWt:GWt + np_],
                                in_=ptv.rearrange("p (k t) -> p k t", k=8)[:, :, 0:np_]), reads=[pt], writes=[xeT])
                            gates_[-1] = gates_[-1] + (GWt,)
                            GWt += np_
                        for fc in range(16):
                            fsl = slice(fc * 128, (fc + 1) * 128)
                            fsl2 = slice((fc % 8) * 128, (fc % 8 + 1) * 128)
                            pa = PS[2 + (fc % 2) * 2]
                            pu = PS[3 + (fc % 2) * 2]
                            for kc in range(8):
                                op("tensor", lambda e, kc=kc, fsl=fsl, pa=pa: e.matmul(
                                    pa[:, 0:GWt], wgh[fc // 8][:, kc, fsl2], xeT[:, kc, 0:GWt], start=(kc == 0),
                                    stop=(kc == 7)), reads=[wgh[fc // 8], xeT], writes=[pa])
                            for kc in range(8):
                                op("tensor", lambda e, kc=kc, fsl=fsl, pu=pu: e.matmul(
                                    pu[:, 0:GWt], wuh[fc // 8][:, kc, fsl2], xeT[:, kc, 0:GWt], start=(kc == 0),
                                    stop=(kc == 7)), reads=[wuh[fc // 8], xeT], writes=[pu])
                            op("scalar", lambda e, pa=pa: e.activation(out=sa[:, 0:GWt], in_=pa[:, 0:GWt], func=AF.Silu),
                               reads=[pa], writes=[sa])
                            op("vector", lambda e, pu=pu, fc=fc: e.tensor_tensor(out=hTt[:, fc, 0:GWt], in0=pu[:, 0:GWt],
                                                                                 in1=sa[:, 0:GWt], op=ALU.mult),
                               reads=[pu, sa], writes=[hTt])
                        for (gt_, gap, np_, kind, ref_, g0) in gates_:
                            y = y2[yi % 2]
                            yi += 1
                            v = 0 if kind == "L" else 1
                            for hf in range(2):
                                hs = slice(hf * 512, (hf + 1) * 512)
                                py = PS[6 + hf]
                                for fc in range(16):
                                    op("tensor", lambda e, fc=fc, hs=hs, py=py, g0=g0, np_=np_: e.matmul(
                                        py[0:np_, :], hTt[:, fc, g0:g0 + np_], wd2[:, fc, hs],
                                        start=(fc == 0), stop=(fc == 15)), reads=[hTt, wd2], writes=[py])
                                op("vector", lambda e, py=py, hs=hs, y=y, gap=gap, np_=np_, v=v: e.scalar_tensor_tensor(
                                    out=y[0:np_, hs], in0=py[0:np_, :], scalar=gap, op0=ALU.mult,
                                    in1=rw[(5, v)][0:np_, hs], op1=ALU.mult), reads=[py, gt_, rw[(5, v)]], writes=[y])
                            if kind == "L":
                                off = IndirectOffsetOnAxis(ap=idx_t[0:SP, ref_:ref_ + 1], axis=0)
                                dma("gpsimd", lambda e, y=y, off=off: e.indirect_dma_start(
                                    out=R[:, :], out_offset=off, in_=y[0:SP, :], in_offset=None, compute_op=ALU.add),
                                    reads=[y, idx_t], writes=[Rbuf])
                            else:
                                op("gpsimd", lambda e, y=y, ref_=ref_: e.tensor_tensor(
                                    out=cacc[:, ref_, :], in0=cacc[:, ref_, :], in1=y[:], op=ALU.add),
                                   reads=[y, cacc], writes=[cacc])
                cast_engs[:] = ["gpsimd", "vector", "scalar"]
                if not last:
                    for s in range(2):
                        xt = y2[s]
                        load_f32(xt[:], xt, R[NL + s * 128:NL + (s + 1) * 128, :])
                        op("gpsimd", lambda e, xt=xt, s=s: e.tensor_tensor(out=xt[:], in0=xt[:], in1=cacc[:, s, :],
                                                                           op=ALU.add), reads=[xt, cacc], writes=[xt])
                        dma("sync", lambda e, xt=xt, s=s: e.dma_start(out=R[NL + s * 128:NL + (s + 1) * 128, :], in_=xt[:]),
                            reads=[xt])
                k.barrier()

        with contextlib.ExitStack() as ph:
            gfr = k.sb(ph, [128, D], F32, "gfr")
            load_f32(gfr[:], gfr, g_fin.partition_broadcast(128))
            xt2 = [k.sb(ph, [128, D], F32, "xt") for _ in range(2)]
            yo2 = [k.sb(ph, [128, D], F32, "yo") for _ in range(2)]
            junk = k.sb(ph, [128, D], F32, "junk")
            ss = k.sb(ph, [128, 1], F32, "ss")
            sq = k.sb(ph, [128, 1], F32, "sq")
            rstd = k.sb(ph, [128, 1], F32, "rstd")
            for t in range(NB):
                xt = xt2[t % 2]
                yo = yo2[t % 2]
                load_f32(xt[:], xt, R[t * 128:(t + 1) * 128, :])
                rms_rstd(ph, xt, ss, sq, rstd, junk)
                op("vector", lambda e, xt=xt, yo=yo: e.scalar_tensor_tensor(
                    out=yo[:], in0=xt[:], scalar=rstd[:, 0:1], op0=ALU.mult, in1=gfr[:], op1=ALU.mult),
                   reads=[xt, rstd, gfr], writes=[yo])
                dma("sync", lambda e, yo=yo, t=t: e.dma_start(out=y_out[t * 128:(t + 1) * 128, :], in_=yo[:]), reads=[yo])
            k.barrier()
    return nc, consts


def _prep_inputs(inp, NB, DEPTH):
    f = lambda a: np.ascontiguousarray(np.asarray(a, dtype=np.float32))
    qperm = []
    for c in range(4):
        qperm += list(range(c * 64, c * 64 + 64)) + list(range((4 + c) * 64, (4 + c) * 64 + 64))
    cols = qperm + list(range(512, 768)) + list(range(1024, 1280)) + list(range(1280, 1536)) + \
        list(range(768, 1024)) + list(range(1536, 4608))
    cols = np.asarray(cols)
    w_in = f(inp["w_in"])[:DEPTH][:, :, cols]
    fm = lambda v, n: f(np.asarray(v).reshape(v.shape[0], n, 128).transpose(0, 2, 1))
    shared = {
        "w_mod": f(inp["w_mod"])[:DEPTH],
        "b_modfm": fm(np.asarray(inp["b_mod"])[:DEPTH], 48),
        "g_mixfm": fm(np.asarray(inp["g_mix"])[:DEPTH], 8),
        "g_ffnfm": fm(np.asarray(inp["g_ffn"])[:DEPTH], 8),
        "w_in": f(w_in),
        "sink": f(inp["attn_sink"])[:DEPTH],
        "wsT": f(np.asarray(inp["w_spatial"])[:DEPTH].transpose(0, 3, 1, 2)),
        "b_sp": f(np.asarray(inp["b_spatial"])[:DEPTH].reshape(DEPTH, 512)),
        "w_ba": f(np.asarray(inp["w_branch_attn"])[:DEPTH].reshape(DEPTH, 8, 64, D).transpose(0, 2, 1, 3)),
        "w_bs": f(np.asarray(inp["w_branch_sgu"])[:DEPTH].reshape(DEPTH, 4, 64, D).transpose(0, 2, 1, 3)),
        "w_bf": f(np.asarray(inp["w_branch_fourier"])[:DEPTH].reshape(DEPTH, 4, 64, D).transpose(0, 2, 1, 3)),
        "w_o": f(inp["w_out"])[:DEPTH],
        "w_r": f(inp["w_router"])[:DEPTH],
        "w_g": f(inp["w_gate"])[:DEPTH],
        "w_u": f(inp["w_up"])[:DEPTH],
        "w_d": f(inp["w_down"])[:DEPTH],
        "g_fin": f(inp["g_final"]),
    }
    return shared


def run(inp, NB, DEPTH, dbg=False):
    nc, consts = build(NB, DEPTH, dbg)
    shared = _prep_inputs(inp, NB, DEPTH)
    if STOP < 6:
        for nm in ("w_g", "w_u", "w_d"):
            shared[nm] = np.zeros((1, 1, 8, 8), np.float32)
    for kname, v in consts.items():
        shared["c_" + kname] = np.ascontiguousarray(v, dtype=np.float32)
    x = np.asarray(inp["x"], dtype=np.float32)
    ctx = np.asarray(inp["ctx"], dtype=np.float32)
    c = np.asarray(inp["c"], dtype=np.float32)
    cc = np.asarray(inp["c_ctx"], dtype=np.float32)
    B = x.shape[0]
    in_maps = []
    for b in range(B):
        m = dict(shared)
        m["x"] = np.ascontiguousarray(x[b])
        m["ctx"] = np.ascontiguousarray(ctx[b])
        cv = np.stack([c[b].reshape(8, 128).T, cc.reshape(8, 128).T], axis=-1)
        m["cvec"] = np.ascontiguousarray(cv, dtype=np.float32)
        in_maps.append(m)
    res = run_bass_kernel_spmd(nc, in_maps, core_ids=list(range(B)))
    return res


def kernel(**inputs):
    NB = np.asarray(inputs["x"]).shape[1] // 128
    DEPTH = np.asarray(inputs["w_mod"]).shape[0]
    res = run(inputs, NB, DEPTH)
    out = np.stack([np.asarray(r["y"], dtype=np.float32) for r in res.results], axis=0)
    return out
```

```python
import contextlib
import numpy as np
import concourse.bass as bass
import concourse.mybir as mybir
from concourse.bass import IndirectOffsetOnAxis
from concourse.bass_utils import run_bass_kernel_spmd

F32 = mybir.dt.float32
BF16 = mybir.dt.bfloat16
I32 = mybir.dt.int32
U32 = mybir.dt.uint32
AF = mybir.ActivationFunctionType
ALU = mybir.AluOpType
AX = mybir.AxisListType

D = 1024
NCTX = 256
NE = 16
FF = 2048
EPS = 1e-6
Q0, K0, V0, VS0, F0, U0, G0 = 0, 512, 640, 768, 1024, 1280, 1536
SAME_ENG_SYNC = True
DEBUG_ALLOC = False
NDQ = 8


import os
STOP = int(os.environ.get("K_STOP", "99"))
SUB = int(os.environ.get("K_SUB", "99"))
SUB2 = int(os.environ.get("K_SUB2", "99"))


class SkipPhase(Exception):
    pass


class Phase(contextlib.ExitStack):
    def __exit__(self, et, ev, tb):
        r = super().__exit__(et, ev, tb)
        return bool(r) or (et is SkipPhase)


class Buf:
    __slots__ = ("name", "w", "r")

    def __init__(self, name):
        self.name = name
        self.w = None
        self.r = []


class Tile:
    def __init__(self, t, name):
        self.t = t
        self.b = Buf(name)

    def __getitem__(self, idx):
        return self.t[idx]


class Multi:
    def __init__(self, tiles, name):
        self.tiles = tiles
        self.b = Buf(name)

    def __getitem__(self, idx):
        p, kc, c = idx
        return self.tiles[kc][p, c]


class Eng:
    def __init__(self, k, name, obj):
        self.name = name
        self.obj = obj
        self.sem = k.new_sem()
        self.cnt = 0
        self.waited = {}


class DQ:
    def __init__(self, k, engname):
        self.engname = engname
        self.sems = [k.new_sem() for _ in range(NDQ)]
        self.n = 0


class K:
    def __init__(self, nc, stack):
        self.nc = nc
        self.stack = stack
        self.nsem = 0
        self.semid = {}
        self.engs = {}
        for n, o in (("tensor", nc.tensor), ("vector", nc.vector), ("scalar", nc.scalar),
                     ("gpsimd", nc.gpsimd), ("sync", nc.sync)):
            self.engs[n] = Eng(self, n, o)
        self.dq = {"sync": DQ(self, "sync"), "gpsimd": DQ(self, "gpsimd")}
        self.uid = 0

    def new_sem(self):
        s = self.stack.enter_context(self.nc.semaphore("s%d" % self.nsem))
        self.semid[id(s)] = self.nsem
        self.nsem += 1
        return s

    def sb(self, stack, shape, dt, name=None):
        self.uid += 1
        nm = "%s_%d" % (name or "t", self.uid)
        t = stack.enter_context(self.nc.sbuf_tensor(nm, list(shape), dt))
        if DEBUG_ALLOC:
            print("ALLOC", nm, shape, dt)
        return Tile(t, nm)

    def _bufs(self, lst):
        out = []
        for x in lst:
            if x is None:
                continue
            out.append(x.b if hasattr(x, 'b') else x)
        return out

    def _wait(self, E, dep):
        sem, val, src = dep
        if src == E.name and (E.name == "tensor" or not SAME_ENG_SYNC):
            return
        sid = self.semid[id(sem)]
        if E.waited.get(sid, 0) >= val:
            return
        E.obj.wait_ge(sem, val)
        E.waited[sid] = val

    def _wait_deps(self, E, reads, writes):
        for b in reads:
            if b.w is not None:
                self._wait(E, b.w)
        for b in writes:
            if b.w is not None:
                self._wait(E, b.w)
            for d in b.r:
                self._wait(E, d)

    def _record(self, dep, reads, writes):
        for b in writes:
            b.w = dep
            b.r = []
        for b in reads:
            b.r.append(dep)
            if len(b.r) > 64:
                b.r = b.r[-64:]

    def op(self, e, fn, reads=(), writes=()):
        E = self.engs[e]
        reads = self._bufs(reads)
        writes = self._bufs(writes)
        self._wait_deps(E, reads, writes)
        ins = fn(E.obj)
        if E.cnt >= 30000:
            E.sem = self.new_sem()
            E.cnt = 0
        E.cnt += 1
        ins.then_inc(E.sem, 1)
        self._record((E.sem, E.cnt, e), reads, writes)

    def dma(self, q, fn, reads=(), writes=()):
        Q = self.dq[q]
        E = self.engs[Q.engname]
        reads = self._bufs(reads)
        writes = self._bufs(writes)
        self._wait_deps(E, reads, writes)
        i = Q.n % NDQ
        rnd = Q.n // NDQ
        if rnd > 0:
            self._wait(E, (Q.sems[i], 16 * rnd, "dma"))
        ins = fn(E.obj)
        ins.then_inc(Q.sems[i], 16)
        Q.n += 1
        self._record((Q.sems[i], 16 * (rnd + 1), "dma"), reads, writes)

    def barrier(self):
        deps = []
        for n, E in self.engs.items():
            if E.cnt > 0:
                deps.append((E.sem, E.cnt, "x"))
        for Q in self.dq.values():
            for i in range(NDQ):
                cnt = (Q.n - i + NDQ - 1) // NDQ
                if cnt > 0:
                    deps.append((Q.sems[i], 16 * cnt, "dma"))
        for E in self.engs.values():
            for d in deps:
                self._wait(E, d)


def _consts(NB):
    NL = NB * 128
    NT = NL + NCTX
    c = {}
    c["ident_bf"] = np.eye(128, dtype=np.float32)
    c["ident_f"] = np.eye(128, dtype=np.float32)
    rot = np.zeros((128, 128), np.float32)
    for p in range(128):
        d = p % 64
        if d < 32:
            rot[p + 32, p] = -1.0
        else:
            rot[p - 32, p] = 1.0
    c["rotT"] = rot
    pos = np.arange(NL)
    row = (pos // 64).astype(np.float64)
    col = (pos % 64).astype(np.float64)
    inv = 10000.0 ** (-np.arange(16, dtype=np.float64) / 16)
    ang = np.concatenate([row[:, None] * inv, col[:, None] * inv], axis=-1)
    cosT = np.ones((128, NT), np.float32)
    sinT = np.zeros((128, NT), np.float32)
    for p in range(128):
        j = p % 32
        cosT[p, :NL] = np.cos(ang[:, j].astype(np.float32))
        sinT[p, :NL] = np.sin(ang[:, j].astype(np.float32))
    c["cosT"] = cosT
    c["sinT"] = sinT
    kk = np.arange(128)[:, None]
    ii = np.arange(128)[None, :]
    c["maskP"] = (kk >= ii).astype(np.float32)
    c["maskN"] = (kk <= ii).astype(np.float32)
    n1 = np.arange(NB)
    angA = 2 * np.pi * np.outer(n1, n1) / NB
    c["CA"] = np.cos(angA).astype(np.float32)
    c["nSA"] = (-np.sin(angA)).astype(np.float32)
    n2 = np.arange(128)
    angT = 2 * np.pi * np.outer(n2, n1) / NL
    c["Tr"] = np.cos(angT).astype(np.float32)
    c["Ti"] = (-np.sin(angT)).astype(np.float32)
    angC = 2 * np.pi * np.outer(n2, n2) / 128
    c["C128"] = np.cos(angC).astype(np.float32)
    c["S128"] = np.sin(angC).astype(np.float32)
    c["nS128"] = (-np.sin(angC)).astype(np.float32)
    dd = np.arange(64)
    angD = 2 * np.pi * np.outer(dd, dd) / 64
    c["CD"] = (np.cos(angD) / np.sqrt(64.0 * NL)).astype(np.float32)
    c["SD"] = (np.sin(angD) / np.sqrt(64.0 * NL)).astype(np.float32)
    c["CDc"] = (np.cos(angD) / np.sqrt(64.0 * NCTX)).astype(np.float32)
    c["SDc"] = (np.sin(angD) / np.sqrt(64.0 * NCTX)).astype(np.float32)
    nn = np.arange(NCTX)
    ang256 = 2 * np.pi * np.outer(nn, nn) / NCTX
    c["C256"] = np.cos(ang256).astype(np.float32).reshape(2, 128, NCTX).transpose(1, 0, 2).copy()
    c["nS256"] = (-np.sin(ang256)).astype(np.float32).reshape(2, 128, NCTX).transpose(1, 0, 2).copy()
    c["iota"] = np.tile(np.arange(2048, dtype=np.float32)[None, :], (128, 1))
    c["pcol"] = np.arange(128, dtype=np.float32)[:, None].copy()
    c["slotcol"] = (np.arange(128, dtype=np.float32)[:, None] + 128.0 * np.arange(16)[None, :]).astype(np.float32)
    c["ustrict"] = (np.arange(128)[:, None] < np.arange(128)[None, :]).astype(np.float32)
    c["ones_f"] = np.ones((128, 128), np.float32)
    return c


BF_CONSTS = ["ident_bf", "rotT", "maskP", "maskN", "CA", "nSA", "C128", "S128", "nS128",
             "CD", "SD", "CDc", "SDc", "C256", "nS256"]


def build(NB, DEPTH, dbg=False):
    NL = NB * 128
    NT = NL + NCTX
    CAP = NL // 8
    NS = max(1, CAP // 128)
    SP = min(128, CAP)
    assert CAP % SP == 0
    nc = bass.Bass("TRN2", target_bir_lowering=False)
    consts = _consts(NB)

    def din(name, shape, dt=F32):
        return nc.dram_tensor(name, list(shape), dt, kind="ExternalInput").ap()

    def dscr(name, shape, dt):
        kind = "ExternalOutput" if dbg else "Internal"
        return nc.dram_tensor(name, list(shape), dt, kind=kind).ap()

    x_in = din("x", [NL, D])
    ctx_in = din("ctx", [NCTX, D])
    cvec = din("cvec", [128, 8, 2])
    w_mod = din("w_mod", [DEPTH, D, 6 * D])
    b_modfm = din("b_modfm", [DEPTH, 128, 48])
    g_mixfm = din("g_mixfm", [DEPTH, 128, 8])
    g_ffnfm = din("g_ffnfm", [DEPTH, 128, 8])
    w_in = din("w_in", [DEPTH, D, 4608])
    sink = din("sink", [DEPTH, 8])
    wsT = din("wsT", [DEPTH, 128, 4, 128])
    b_sp = din("b_sp", [DEPTH, 4 * 128])
    w_ba = din("w_ba", [DEPTH, 64, 8, D])
    w_bs = din("w_bs", [DEPTH, 64, 4, D])
    w_bf = din("w_bf", [DEPTH, 64, 4, D])
    w_o = din("w_o", [DEPTH, D, D])
    w_r = din("w_r", [DEPTH, D, NE])
    if STOP < 6:
        w_g = din("w_g", [1, 1, 8, 8])
        w_u = din("w_u", [1, 1, 8, 8])
        w_d = din("w_d", [1, 1, 8, 8])
    else:
        w_g = din("w_g", [DEPTH, NE, D, FF])
        w_u = din("w_u", [DEPTH, NE, D, FF])
        w_d = din("w_d", [DEPTH, NE, FF, D])
    g_fin = din("g_fin", [D])
    cin = {k: din("c_" + k, list(v.shape)) for k, v in consts.items()}
    y_out = nc.dram_tensor("y", [NL, D], F32, kind="ExternalOutput").ap()

    R = dscr("R", [NT, D], F32)
    QT = dscr("QT", [4, 128, NT], BF16)
    KT = dscr("KT", [128, NT], BF16)
    Vd = dscr("Vd", [NT, 128], BF16)
    FX = dscr("FX", [4, NT, 64], BF16)
    GT = dscr("GT", [24, 128, NT], BF16)
    AT = dscr("AT", [8, 64, NT], BF16)
    ST = dscr("ST", [4, 64, NT], BF16)
    FY = dscr("FY", [4, 64, NT], BF16)
    H2 = dscr("H2", [NT, D], BF16)
    AFF = dscr("AFF", [NT, NE], F32)
    MODROW = dscr("MODROW", [12, D], F32)

    with contextlib.ExitStack() as top:
        k = K(nc, top)
        op = k.op
        dma = k.dma
        PS = []
        for i in range(8):
            t = top.enter_context(nc.psum_tensor("ps%d" % i, [128, 512], F32))
            PS.append(Tile(t, "ps%d" % i))

        C = {}
        stg = [k.sb(top, [128, 2048], F32, "stg") for _ in range(3)]
        cast_engs = ["gpsimd", "vector", "scalar"]
        stgi = [0]

        def load_cast(dst_ap, dst_tile, src_ap, shape):
            s = stg[stgi[0] % 3]
            ce = cast_engs[stgi[0] % len(cast_engs)]
            stgi[0] += 1
            p = shape[0]
            n = int(np.prod(shape[1:]))
            sv = s[0:p, 0:n]
            if len(shape) == 3:
                sv = sv.rearrange("p (a b) -> p a b", a=shape[1])
            dma("sync", lambda e: e.dma_start(out=sv, in_=src_ap), writes=[s])
            if ce == "scalar":
                op("scalar", lambda e: e.copy(out=dst_ap, in_=sv), reads=[s], writes=[dst_tile])
            else:
                op(ce, lambda e: e.tensor_copy(out=dst_ap, in_=sv), reads=[s], writes=[dst_tile])

        def load_f32(dst_ap, dst_tile, src_ap, q="sync", **kw):
            dma(q, lambda e: e.dma_start(out=dst_ap, in_=src_ap, **kw), writes=[dst_tile])

        for name, v in consts.items():
            shp = list(v.shape)
            if name in ("cosT", "sinT"):
                continue
            if name in BF_CONSTS:
                t = k.sb(top, shp, BF16, name)
                load_cast(t[:], t, cin[name], shp)
            else:
                t = k.sb(top, shp, F32, name)
                load_f32(t[:], t, cin[name])
            C[name] = t
        ones_bf = k.sb(top, [128, 64], BF16, "ones_bf")
        op("vector", lambda e: e.memset(ones_bf[:], 1.0), writes=[ones_bf])
        eps_t = k.sb(top, [128, 1], F32, "eps")
        op("vector", lambda e: e.memset(eps_t[:], EPS), writes=[eps_t])
        cv = k.sb(top, [128, 8, 2], F32, "cv")
        load_f32(cv[:], cv, cvec)
        csil = k.sb(top, [128, 8, 2], BF16, "csil")
        op("scalar", lambda e: e.activation(out=csil[:], in_=cv[:], func=AF.Silu), reads=[cv], writes=[csil])
        idx_t = k.sb(top, [128, NE * NS], I32, "idx")
        gwc = k.sb(top, [128, 2, NE], F32, "gwc")

        dma("sync", lambda e: e.dma_start(out=R[0:NL, :], in_=x_in[:, :]))
        dma("sync", lambda e: e.dma_start(out=R[NL:NT, :], in_=ctx_in[:, :]))
        k.barrier()

        def rms_rstd(st, xt, ss, sq, rstd, junk):
            op("scalar", lambda e: e.activation(out=junk[:], in_=xt[:], func=AF.Square, accum_out=ss[:]),
               reads=[xt], writes=[junk, ss])
            op("scalar", lambda e: e.activation(out=sq[:], in_=ss[:], func=AF.Sqrt, scale=1.0 / D, bias=eps_t[:]),
               reads=[ss, eps_t], writes=[sq])
            op("vector", lambda e: e.reciprocal(out=rstd[:], in_=sq[:]), reads=[sq], writes=[rstd])

        groups = [(g * 512, 512, 0) for g in range(NL // 512)] + [(NL, NCTX, 1)]

        for L in range(DEPTH):
            last = (L == DEPTH - 1)
            with Phase() as ph:
                if STOP < 0:
                    raise SkipPhase()
                modfm = k.sb(ph, [128, 48, 2], F32, "modfm")
                bm = k.sb(ph, [128, 48], F32, "bm")
                load_f32(bm[:], bm, b_modfm[L])
                gm = k.sb(ph, [128, 8], F32, "gm")
                gf = k.sb(ph, [128, 8], F32, "gf")
                load_f32(gm[:], gm, g_mixfm[L])
                load_f32(gf[:], gf, g_ffnfm[L])
                wm = [k.sb(ph, [128, 8, 1024], BF16, "wm") for _ in range(2)]
                pm = PS[0]
                pmv = pm[:, 0:96].rearrange("p (j v) -> p j v", v=2)
                for pc in range(6):
                    w = wm[pc % 2]
                    for kp in range(4):
                        src = w_mod[L, kp * 256:(kp + 1) * 256, pc * 1024:(pc + 1) * 1024].rearrange(
                            "(a p) n -> p a n", p=128)
                        load_cast(w[:, 2 * kp:2 * kp + 2, :], w, src, [128, 2, 1024])
                    for jj in range(8):
                        j = pc * 8 + jj
                        for kc in range(8):
                            op("tensor", lambda e, w=w, jj=jj, kc=kc, j=j: e.matmul(
                                pmv[:, j, :], w[:, kc, jj * 128:(jj + 1) * 128], csil[:, kc, :],
                                start=(kc == 0), stop=(kc == 7)), reads=[w, csil], writes=[pm])
                op("vector", lambda e: e.tensor_tensor(out=modfm[:], in0=pmv,
                                                       in1=bm[:].unsqueeze(2).to_broadcast([128, 48, 2]), op=ALU.add),
                   reads=[pm, bm], writes=[modfm])
                rows = k.sb(ph, [128, 6, 2, 8], F32, "rows")

                def mv(which):
                    return modfm[:, which * 8:(which + 1) * 8, :].rearrange("p k v -> p v k")

                for r, (sc_i, g_t) in ((0, (1, gm)), (3, (4, gf))):
                    op("vector", lambda e, r=r, sc_i=sc_i, g_t=g_t: e.scalar_tensor_tensor(
                        out=rows[:, r, :, :], in0=mv(sc_i), scalar=1.0, op0=ALU.add,
                        in1=g_t[:].unsqueeze(1).to_broadcast([128, 2, 8]), op1=ALU.mult),
                       reads=[modfm, g_t], writes=[rows])
                for r, wi in ((1, 0), (2, 2), (4, 3), (5, 5)):
                    op("vector", lambda e, r=r, wi=wi: e.tensor_copy(out=rows[:, r, :, :], in_=mv(wi)),
                       reads=[modfm], writes=[rows])
                pr = PS[1]
                op("tensor", lambda e: e.transpose(pr[0:96, 0:128], rows[:].rearrange("p r v k -> p (r v k)"),
                                                   C["ident_f"][:]), reads=[rows, C["ident_f"]], writes=[pr])
                rowsT = k.sb(ph, [96, 128], F32, "rowsT")
                op("vector", lambda e: e.tensor_copy(out=rowsT[:], in_=pr[0:96, 0:128]), reads=[pr], writes=[rowsT])
                dma("sync", lambda e: e.dma_start(out=MODROW.rearrange("r (kc p) -> (r kc) p", p=128), in_=rowsT[:]),
                    reads=[rowsT])
                k.barrier()

            def load_rows(ph, rlist):
                out = {}
                for r in rlist:
                    for v in range(2):
                        t = k.sb(ph, [128, D], F32, "row%d_%d" % (r, v))
                        load_f32(t[:], t, MODROW[r * 2 + v, :].partition_broadcast(128))
                        out[(r, v)] = t
                return out

            with Phase() as ph:
                if STOP < 1:
                    raise SkipPhase()
                win = Multi([k.sb(ph, [128, 4608], BF16, "win") for _ in range(8)], "win")
                for kc in range(8):
                    for c0 in (0, 2048, 4096):
                        cw = min(2048, 4608 - c0)
                        load_cast(win[:, kc, c0:c0 + cw], win, w_in[L, kc * 128:(kc + 1) * 128, c0:c0 + cw], [128, cw])
                rw = load_rows(ph, [0, 1])
                wst = k.sb(ph, [128, 4, 128], BF16, "wst")
                load_cast(wst[:], wst, wsT[L], [128, 4, 128])
                bsb = k.sb(ph, [64, 4, 128], F32, "bsb")
                load_f32(bsb[:].rearrange("p g i -> p (g i)"), bsb, b_sp[L, :].partition_broadcast(64))
                xt2 = [k.sb(ph, [128, D], F32, "xt") for _ in range(2)]
                t1 = k.sb(ph, [128, D], F32, "t1")
                junk = t1
                hb4 = [k.sb(ph, [128, D], BF16, "hb") for _ in range(4)]
                hT2 = [k.sb(ph, [128, 8, 512], BF16, "hT") for _ in range(2)]
                ss = k.sb(ph, [128, 1], F32, "ss")
                sq = k.sb(ph, [128, 1], F32, "sq")
                rstd = k.sb(ph, [128, 1], F32, "rstd")
                cs_t = [k.sb(ph, [128, 512], F32, "cos") for _ in range(2)]
                sn_t = [k.sb(ph, [128, 512], F32, "sin") for _ in range(2)]
                qf2 = [k.sb(ph, [128, 512], F32, "qf") for _ in range(2)]
                qb2 = [k.sb(ph, [128, 512], BF16, "qb") for _ in range(2)]
                r1 = k.sb(ph, [128, 512], F32, "r1")
                r2 = k.sb(ph, [128, 512], F32, "r2")
                qo2 = [k.sb(ph, [128, 512], BF16, "qo") for _ in range(3)]
                ug = k.sb(ph, [64, 4, 512], BF16, "ug")
                vb2 = [k.sb(ph, [128, 128], BF16, "vb") for _ in range(2)]
                fb2 = [k.sb(ph, [128, 256], BF16, "fb") for _ in range(2)]
                gv = k.sb(ph, [128, 4, 64], F32, "gv")
                gsq = k.sb(ph, [128, 4, 64], F32, "gsq")
                ms = k.sb(ph, [128, 4], F32, "ms")
                ms2 = k.sb(ph, [128, 4], F32, "ms2")
                rs = k.sb(ph, [128, 4], F32, "rs")
                vn2 = [k.sb(ph, [128, 4, 64], BF16, "vn") for _ in range(2)]
                tz = k.sb(ph, [64, 4, 128], F32, "tz")
                so2 = [k.sb(ph, [64, 4, 128], BF16, "so") for _ in range(2)]
                qoi = [0]

                def prepA(gi):
                    tok0, GW, v = groups[gi]
                    cs = cs_t[gi % 2]
                    sn = sn_t[gi % 2]
                    load_f32(cs[:, 0:GW], cs, cin["cosT"][:, tok0:tok0 + GW])
                    load_f32(sn[:, 0:GW], sn, cin["sinT"][:, tok0:tok0 + GW])
                    for j in range(GW // 128):
                        xt = xt2[j % 2]
                        hb = hb4[j]
                        r0 = tok0 + j * 128
                        load_f32(xt[:], xt, R[r0:r0 + 128, :])
                        rms_rstd(ph, xt, ss, sq, rstd, junk)
                        op("vector", lambda e, xt=xt, v=v: e.scalar_tensor_tensor(
                            out=t1[:], in0=xt[:], scalar=rstd[:, 0:1], op0=ALU.mult, in1=rw[(0, v)][:], op1=ALU.mult),
                           reads=[xt, rstd, rw[(0, v)]], writes=[t1])
                        op("vector", lambda e, hb=hb, v=v: e.tensor_tensor(out=hb[:], in0=t1[:], in1=rw[(1, v)][:],
                                                                          op=ALU.add),
                           reads=[t1, rw[(1, v)]], writes=[hb])

                def prepB(gi):
                    tok0, GW, v = groups[gi]
                    hT = hT2[gi % 2]
                    for j in range(GW // 128):
                        hb = hb4[j]
                        pt = PS[j % 2]
                        ptv = pt[:, :].bitcast(BF16)
                        for kc in range(8):
                            op("tensor", lambda e, kc=kc, ptv=ptv, hb=hb: e.transpose(
                                ptv[:, kc * 128:(kc + 1) * 128], hb[:, kc * 128:(kc + 1) * 128], C["ident_bf"][:]),
                               reads=[hb, C["ident_bf"]], writes=[pt])
                        op("scalar", lambda e, ptv=ptv, j=j, hT=hT: e.copy(
                            out=hT[:, :, j * 128:(j + 1) * 128], in_=ptv.rearrange("p (k t) -> p k t", k=8)),
                           reads=[pt], writes=[hT])

                prepA(0)
                prepB(0)
                for gi, (tok0, GW, v) in enumerate(groups):
                    ntile = GW // 128
                    hT = hT2[gi % 2]
                    cs = cs_t[gi % 2]
                    sn = sn_t[gi % 2]
                    if gi + 1 < len(groups):
                        prepA(gi + 1)
                    fmc = [0]

                    def fm_mm(col0, ncol, pq):
                        for kc in range(8):
                            op("tensor", lambda e, kc=kc: e.matmul(
                                pq[0:ncol, 0:GW], win[:, kc, col0:col0 + ncol], hT[:, kc, 0:GW],
                                start=(kc == 0), stop=(kc == 7)), reads=[win, hT], writes=[pq])

                    def next_pq():
                        pq = PS[2 + (fmc[0] % 2)]
                        fmc[0] += 1
                        return pq

                    def job_rope(ci):
                        pq = next_pq()
                        fm_mm(Q0 + ci * 128, 128, pq)
                        qf = qf2[ci % 2]
                        qb = qb2[ci % 2]
                        op("scalar", lambda e: e.copy(out=qf[:, 0:GW], in_=pq[:, 0:GW]), reads=[pq], writes=[qf])
                        op("vector", lambda e: e.tensor_copy(out=qb[:, 0:GW], in_=qf[:, 0:GW]), reads=[qf], writes=[qb])

                        def follow():
                            prr = PS[4]
                            op("tensor", lambda e: e.matmul(prr[:, 0:GW], C["rotT"][:], qb[:, 0:GW], start=True,
                                                            stop=True), reads=[C["rotT"], qb], writes=[prr])
                            op("vector", lambda e: e.tensor_tensor(out=r1[:, 0:GW], in0=qf[:, 0:GW], in1=cs[:, 0:GW],
                                                                   op=ALU.mult), reads=[qf, cs], writes=[r1])
                            op("vector", lambda e: e.tensor_tensor(out=r2[:, 0:GW], in0=prr[:, 0:GW], in1=sn[:, 0:GW],
                                                                   op=ALU.mult), reads=[prr, sn], writes=[r2])
                            qo = qo2[qoi[0] % 3]
                            qoi[0] += 1
                            op("vector", lambda e: e.tensor_tensor(out=qo[:, 0:GW], in0=r1[:, 0:GW], in1=r2[:, 0:GW],
                                                                   op=ALU.add), reads=[r1, r2], writes=[qo])
                            dst = QT[ci, :, tok0:tok0 + GW] if ci < 4 else KT[:, tok0:tok0 + GW]
                            dma("sync", lambda e: e.dma_start(out=dst, in_=qo[:, 0:GW]), reads=[qo])
                        return follow, 1

                    def job_gate(j):
                        pq = next_pq()
                        fm_mm(G0 + j * 128, 128, pq)
                        qo = qo2[qoi[0] % 3]
                        qoi[0] += 1
                        op("scalar", lambda e: e.activation(out=qo[:, 0:GW], in_=pq[:, 0:GW], func=AF.Sigmoid),
                           reads=[pq], writes=[qo])
                        dma("sync", lambda e: e.dma_start(out=GT[j, :, tok0:tok0 + GW], in_=qo[:, 0:GW]), reads=[qo])
                        return None, 0

                    def job_u(g):
                        pq = next_pq()
                        fm_mm(U0 + g * 64, 64, pq)
                        op("scalar", lambda e: e.activation(out=ug[:, g, 0:GW], in_=pq[0:64, 0:GW],
                                                            func=AF.Gelu_apprx_tanh), reads=[pq], writes=[ug])
                        return None, 0

                    def job_tm(j):
                        r0 = tok0 + j * 128
                        pa = PS[5]
                        pb = PS[6]
                        for kc in range(8):
                            op("tensor", lambda e, kc=kc: e.matmul(
                                pa[:, 0:384], hT[:, kc, j * 128:(j + 1) * 128], win[:, kc, V0:V0 + 384],
                                start=(kc == 0), stop=(kc == 7)), reads=[win, hT], writes=[pa])
                        for kc in range(8):
                            op("tensor", lambda e, kc=kc: e.matmul(
                                pb[:, 0:256], hT[:, kc, j * 128:(j + 1) * 128], win[:, kc, F0:F0 + 256],
                                start=(kc == 0), stop=(kc == 7)), reads=[win, hT], writes=[pb])
                        vb = vb2[j % 2]
                        fb = fb2[j % 2]
                        vn = vn2[j % 2]
                        op("scalar", lambda e: e.copy(out=vb[:], in_=pa[:, 0:128]), reads=[pa], writes=[vb])
                        dma("sync", lambda e: e.dma_start(out=Vd[r0:r0 + 128, :], in_=vb[:]), reads=[vb])
                        op("scalar", lambda e: e.copy(out=fb[:], in_=pb[:, 0:256]), reads=[pb], writes=[fb])
                        dma("sync", lambda e: e.dma_start(
                            out=FX[:, r0:r0 + 128, :].rearrange("g t d -> t g d"),
                            in_=fb[:].rearrange("p (g d) -> p g d", g=4)), reads=[fb])
                        op("scalar", lambda e: e.activation(out=gv[:].rearrange("p g d -> p (g d)"), in_=pa[:, 128:384],
                                                            func=AF.Gelu_apprx_tanh), reads=[pa], writes=[gv])
                        op("vector", lambda e: e.tensor_tensor(out=gsq[:], in0=gv[:], in1=gv[:], op=ALU.mult),
                           reads=[gv], writes=[gsq])
                        op("vector", lambda e: e.tensor_reduce(out=ms[:], in_=gsq[:], axis=AX.X, op=ALU.add),
                           reads=[gsq], writes=[ms])
                        op("scalar", lambda e: e.activation(out=ms2[:], in_=ms[:], func=AF.Sqrt, scale=1.0 / 64,
                                                            bias=eps_t[:]), reads=[ms, eps_t], writes=[ms2])
                        op("vector", lambda e: e.reciprocal(out=rs[:], in_=ms2[:]), reads=[ms2], writes=[rs])
                        op("vector", lambda e: e.tensor_tensor(out=vn[:], in0=gv[:],
                                                               in1=rs[:].unsqueeze(2).to_broadcast([128, 4, 64]),
                                                               op=ALU.mult), reads=[gv, rs], writes=[vn])

                        def follow():
                            pz = PS[7]
                            for g in range(4):
                                op("tensor", lambda e, g=g: e.matmul(pz[0:64, g * 128:(g + 1) * 128], vn[:, g, :],
                                                                     wst[:, g, :], start=True, stop=True),
                                   reads=[vn, wst], writes=[pz])
                            op("vector", lambda e: e.tensor_tensor(
                                out=tz[:], in0=pz[0:64, :].rearrange("p (g i) -> p g i", g=4), in1=bsb[:], op=ALU.add),
                               reads=[pz, bsb], writes=[tz])
                            so = so2[j % 2]
                            op("vector", lambda e: e.tensor_tensor(out=so[:], in0=tz[:],
                                                                   in1=ug[:, :, j * 128:(j + 1) * 128], op=ALU.mult),
                               reads=[tz, ug], writes=[so])
                            dma("sync", lambda e: e.dma_start(
                                out=ST[:, :, r0:r0 + 128].rearrange("g d t -> d g t"), in_=so[:]), reads=[so])
                        return follow, 2

                    jobs = [(job_rope, ci) for ci in range(5)] + [(job_u, g) for g in range(4)]
                    gper = 24 // ntile
                    for j in range(ntile):
                        jobs += [(job_gate, jj) for jj in range(j * gper, (j + 1) * gper)]
                        jobs.append((job_tm, j))
                    pendf = []
                    for n_, (fn_, arg_) in enumerate(jobs):
                        fo, dl = fn_(arg_)
                        due = [p for p in pendf if p[0] <= n_]
                        pendf = [p for p in pendf if p[0] > n_]
                        for p in due:
                            p[1]()
                        if fo is not None:
                            pendf.append((n_ + dl, fo))
                    for p in pendf:
                        p[1]()
                    if gi + 1 < len(groups):
                        prepB(gi + 1)
                k.barrier()

            with Phase() as ph:
                if STOP < 2:
                    raise SkipPhase()
                sk = k.sb(ph, [64, 8], F32, "sk")
                load_f32(sk[:], sk, sink[L, :].partition_broadcast(64))
                se = k.sb(ph, [64, 8], F32, "se")
                op("scalar", lambda e: e.activation(out=se[:], in_=sk[:], func=AF.Exp), reads=[sk], writes=[se])
                sexp = k.sb(ph, [64, 8, 128], F32, "sexp")
                op("vector", lambda e: e.tensor_copy(out=sexp[:], in_=se[:].unsqueeze(2).to_broadcast([64, 8, 128])),
                   reads=[se], writes=[sexp])
                kc_t = k.sb(ph, [128, NCTX], BF16, "kctx")
                vc_t = k.sb(ph, [128, 2, 128], BF16, "vctx")
                load_f32(kc_t[:], kc_t, KT[:, NL:NT])
                load_f32(vc_t[:], vc_t, Vd[NL:NT, :].rearrange("(b p) d -> p b d", p=128))
                q2 = [k.sb(ph, [128, 4, 512], BF16, "q4") for _ in range(2)]
                k2 = [k.sb(ph, [128, 768], BF16, "k6") for _ in range(2)]
                v2 = [k.sb(ph, [128, 6, 128], BF16, "v6") for _ in range(2)]
                pt2 = [k.sb(ph, [128, 512], BF16, "PT") for _ in range(4)]
                den = k.sb(ph, [64, 512], F32, "den")
                rden = k.sb(ph, [64, 512], F32, "rden")
                ao2 = [k.sb(ph, [64, 4, 128], BF16, "ao") for _ in range(2)]
                pti = 0
                aoi = [0]
                psi = 0
                agroups = [gr for gr in groups if not (gr[2] == 1 and last)]

                def p2_loads(gi):
                    tok0, GW, v = agroups[gi]
                    q4 = q2[gi % 2]
                    load_f32(q4[:, :, 0:GW], q4, QT[:, :, tok0:tok0 + GW].rearrange("c p t -> p c t"))
                    if v == 0:
                        k6 = k2[gi % 2]
                        v6 = v2[gi % 2]
                        lo = max(tok0 - 128, 0)
                        hi = min(tok0 + GW + 128, NL)
                        off = lo - (tok0 - 128)
                        load_f32(k6[:, off:off + hi - lo], k6, KT[:, lo:hi])
                        load_f32(v6[:, off // 128:(off + hi - lo) // 128, :], v6,
                                 Vd[lo:hi, :].rearrange("(b p) d -> p b d", p=128))

                p2_loads(0)
                pend = []
                jobc = 0

                def emit_pv(it):
                    (bi, nb_, g, par, kb, PT, tok0_, qi_) = it
                    kt_, kcol, vt_, vblk, msk = kb
                    po = PS[3 + 2 * par]
                    pd = PS[4 + 2 * par]
                    st = (bi == 0)
                    sp = (bi == nb_ - 1)
                    op("tensor", lambda e: e.matmul(
                        po[0:64, :], vt_[:, vblk, 64 * g:64 * g + 64], PT[:], start=st, stop=sp),
                       reads=[vt_, PT], writes=[po])
                    op("tensor", lambda e: e.matmul(
                        pd[0:64, :], ones_bf[:, 0:64], PT[:], start=st, stop=sp),
                       reads=[ones_bf, PT], writes=[pd])
                    if sp:
                        op("vector", lambda e: e.tensor_tensor(
                            out=den[:], in0=pd[0:64, :],
                            in1=sexp[:, 4 * g:4 * g + 4, :].rearrange("p c q -> p (c q)"), op=ALU.add),
                           reads=[pd, sexp], writes=[den])
                        op("vector", lambda e: e.reciprocal(out=rden[:], in_=den[:]), reads=[den], writes=[rden])
                        ao = ao2[aoi[0] % 2]
                        aoi[0] += 1
                        op("vector", lambda e: e.tensor_tensor(
                            out=ao[:].rearrange("p c q -> p (c q)"), in0=po[0:64, :], in1=rden[:], op=ALU.mult),
                           reads=[po, rden], writes=[ao])
                        r0 = tok0_ + qi_ * 128
                        dma("sync", lambda e: e.dma_start(
                            out=AT[4 * g:4 * g + 4, :, r0:r0 + 128].rearrange("h d t -> d h t"), in_=ao[:]),
                            reads=[ao])

                for gi, (tok0, GW, v) in enumerate(agroups):
                    nq = GW // 128
                    q4 = q2[gi % 2]
                    k6 = k2[gi % 2]
                    v6 = v2[gi % 2]
                    while pend:
                        emit_pv(pend.pop(0))
                    if gi + 1 < len(agroups):
                        p2_loads(gi + 1)
                    for g in range(2):
                        for qi in range(nq):
                            nblk = tok0 // 128 + qi
                            par = jobc % 2
                            jobc += 1
                            kbs = []
                            if v == 0:
                                if nblk > 0:
                                    kbs.append((k6, qi * 128, v6, qi, "P"))
                                kbs.append((k6, (qi + 1) * 128, v6, qi + 1, None))
                                if nblk < NB - 1:
                                    kbs.append((k6, (qi + 2) * 128, v6, qi + 2, "N"))
                            kbs.append((kc_t, 0, vc_t, 0, None))
                            kbs.append((kc_t, 128, vc_t, 1, None))
                            for bi, kb in enumerate(kbs):
                                kt_, kcol, vt_, vblk, msk = kb
                                psS = PS[psi % 3]
                                psi += 1
                                op("tensor", lambda e, kt_=kt_, kcol=kcol, psS=psS, g=g, qi=qi: e.matmul(
                                    psS[:, :].rearrange("p (c q) -> p c q", c=4),
                                    kt_[64 * g:64 * g + 64, kcol:kcol + 128],
                                    q4[64 * g:64 * g + 64, :, qi * 128:(qi + 1) * 128], start=True, stop=True),
                                   reads=[kt_, q4], writes=[psS])
                                PT = pt2[pti % 4]
                                pti += 1
                                op("scalar", lambda e, psS=psS, PT=PT: e.activation(out=PT[:], in_=psS[:, :], func=AF.Exp,
                                                                                    scale=0.125), reads=[psS], writes=[PT])
                                if msk is not None:
                                    mt = C["maskP"] if msk == "P" else C["maskN"]
                                    op("vector", lambda e, PT=PT, mt=mt: e.tensor_tensor(
                                        out=PT[:].rearrange("p (c q) -> p c q", c=4),
                                        in0=PT[:].rearrange("p (c q) -> p c q", c=4),
                                        in1=mt[:].unsqueeze(1).to_broadcast([128, 4, 128]), op=ALU.mult),
                                       reads=[PT, mt], writes=[PT])
                                pend.append((bi, len(kbs), g, par, kb, PT, tok0, qi))
                                if len(pend) > 2:
                                    emit_pv(pend.pop(0))
                while pend:
                    emit_pv(pend.pop(0))
                k.barrier()

            with Phase() as ph:
                if STOP < 3:
                    raise SkipPhase()
                Xg = k.sb(ph, [NB, 128, 64], BF16, "Xg")
                Bre = k.sb(ph, [128, 64, NB], BF16, "Bre")
                Bim = k.sb(ph, [128, 64, NB], BF16, "Bim")
                ZrT = k.sb(ph, [64, NL], BF16, "ZrT")
                ZiT = k.sb(ph, [64, NL], BF16, "ZiT")
                tw = [k.sb(ph, [128, 4, NB], F32, "tw") for _ in range(4)]
                yo2 = [k.sb(ph, [64, 512], BF16, "yo") for _ in range(2)]
                Trb = C["Tr"][:].unsqueeze(1).to_broadcast([128, 4, NB])
                Tib = C["Ti"][:].unsqueeze(1).to_broadcast([128, 4, NB])
                KG = max(1, min(4, NB))
                for g in range(4):
                    dma("sync", lambda e, g=g: e.dma_start(out=Xg[:], in_=FX[g, 0:NL, :].rearrange(
                        "(a b) c -> a b c", b=128)), writes=[Xg])
                    for cq in range(16):
                        par = PS[0 + (cq % 2) * 2]
                        pai = PS[1 + (cq % 2) * 2]
                        parv = par[:, 0:4 * NB].rearrange("p (c k) -> p c k", c=4)
                        paiv = pai[:, 0:4 * NB].rearrange("p (c k) -> p c k", c=4)
                        for cc in range(4):
                            ch = cq * 4 + cc
                            op("tensor", lambda e, ch=ch, cc=cc, parv=parv, par=par: e.matmul(
                                parv[:, cc, :], Xg[0:NB, :, ch], C["CA"][0:NB, 0:NB], start=True, stop=True),
                               reads=[Xg, C["CA"]], writes=[par])
                            op("tensor", lambda e, ch=ch, cc=cc, paiv=paiv, pai=pai: e.matmul(
                                paiv[:, cc, :], Xg[0:NB, :, ch], C["nSA"][0:NB, 0:NB], start=True, stop=True),
                               reads=[Xg, C["nSA"]], writes=[pai])
                        c0 = cq * 4
                        op("vector", lambda e, parv=parv, par=par: e.tensor_tensor(out=tw[0][:], in0=parv, in1=Trb,
                                                                                   op=ALU.mult),
                           reads=[par, C["Tr"]], writes=[tw[0]])
                        op("vector", lambda e, paiv=paiv, pai=pai: e.tensor_tensor(out=tw[1][:], in0=paiv, in1=Tib,
                                                                                   op=ALU.mult),
                           reads=[pai, C["Ti"]], writes=[tw[1]])
                        op("gpsimd", lambda e, c0=c0: e.tensor_tensor(out=Bre[:, c0:c0 + 4, :], in0=tw[0][:], in1=tw[1][:],
                                                                      op=ALU.subtract),
                           reads=[tw[0], tw[1]], writes=[Bre])
                        op("vector", lambda e, parv=parv, par=par: e.tensor_tensor(out=tw[2][:], in0=parv, in1=Tib,
                                                                                   op=ALU.mult),
                           reads=[par, C["Ti"]], writes=[tw[2]])
                        op("vector", lambda e, paiv=paiv, pai=pai: e.tensor_tensor(out=tw[3][:], in0=paiv, in1=Trb,
                                                                                   op=ALU.mult),
                           reads=[pai, C["Tr"]], writes=[tw[3]])
                        op("gpsimd", lambda e, c0=c0: e.tensor_tensor(out=Bim[:, c0:c0 + 4, :], in0=tw[2][:], in1=tw[3][:],
                                                                      op=ALU.add),
                           reads=[tw[2], tw[3]], writes=[Bim])
                    Zrv = ZrT[:].rearrange("d (k2 k1) -> d k1 k2", k1=NB)
                    Ziv = ZiT[:].rearrange("d (k2 k1) -> d k1 k2", k1=NB)
                    for kg in range(NB // KG):
                        pzr = PS[4 + (kg % 2) * 2]
                        pzi = PS[5 + (kg % 2) * 2]
                        for j in range(KG):
                            k1 = kg * KG + j
                            sl = slice(j * 128, (j + 1) * 128)
                            op("tensor", lambda e, k1=k1, sl=sl, pzr=pzr: e.matmul(
                                pzr[0:64, sl], Bre[:, :, k1], C["C128"][:], start=True, stop=False),
                               reads=[Bre, C["C128"]], writes=[pzr])
                            op("tensor", lambda e, k1=k1, sl=sl, pzr=pzr: e.matmul(
                                pzr[0:64, sl], Bim[:, :, k1], C["S128"][:], start=False, stop=True),
                               reads=[Bim, C["S128"]], writes=[pzr])
                            op("tensor", lambda e, k1=k1, sl=sl, pzi=pzi: e.matmul(
                                pzi[0:64, sl], Bim[:, :, k1], C["C128"][:], start=True, stop=False),
                               reads=[Bim, C["C128"]], writes=[pzi])
                            op("tensor", lambda e, k1=k1, sl=sl, pzi=pzi: e.matmul(
                                pzi[0:64, sl], Bre[:, :, k1], C["nS128"][:], start=False, stop=True),
                               reads=[Bre, C["nS128"]], writes=[pzi])
                        op("scalar", lambda e, kg=kg, pzr=pzr: e.copy(
                            out=Zrv[:, kg * KG:(kg + 1) * KG, :],
                            in_=pzr[0:64, 0:KG * 128].rearrange("p (j q) -> p j q", j=KG)), reads=[pzr], writes=[ZrT])
                        op("scalar", lambda e, kg=kg, pzi=pzi: e.copy(
                            out=Ziv[:, kg * KG:(kg + 1) * KG, :],
                            in_=pzi[0:64, 0:KG * 128].rearrange("p (j q) -> p j q", j=KG)), reads=[pzi], writes=[ZiT])
                    for ti in range(NL // 512):
                        py = PS[0 + (ti % 2)]
                        sl = slice(ti * 512, (ti + 1) * 512)
                        op("tensor", lambda e, py=py, sl=sl: e.matmul(py[0:64, :], C["CD"][:], ZrT[:, sl], start=True,
                                                                      stop=False), reads=[C["CD"], ZrT], writes=[py])
                        op("tensor", lambda e, py=py, sl=sl: e.matmul(py[0:64, :], C["SD"][:], ZiT[:, sl], start=False,
                                                                      stop=True), reads=[C["SD"], ZiT], writes=[py])
                        yo = yo2[ti % 2]
                        op("scalar", lambda e, py=py, yo=yo: e.copy(out=yo[:], in_=py[0:64, :]), reads=[py], writes=[yo])
                        dma("sync", lambda e, yo=yo, sl=sl, g=g: e.dma_start(out=FY[g, :, sl], in_=yo[:]), reads=[yo])
                if not last:
                    Xc = k.sb(ph, [128, 2, 4, 64], BF16, "Xc")
                    for j in range(2):
                        dma("sync", lambda e, j=j: e.dma_start(
                            out=Xc[:, j, :, :], in_=FX[:, NL + j * 128:NL + (j + 1) * 128, :].rearrange("g t d -> t g d")),
                            writes=[Xc])
                    zc = [k.sb(ph, [64, NCTX], BF16, "zc") for _ in range(2)]
                    for g in range(4):
                        pzr = PS[2]
                        pzi = PS[3]
                        for j in range(2):
                            op("tensor", lambda e, j=j, g=g: e.matmul(pzr[0:64, 0:NCTX], Xc[:, j, g, :], C["C256"][:, j, :],
                                                                      start=(j == 0), stop=(j == 1)),
                               reads=[Xc, C["C256"]], writes=[pzr])
                            op("tensor", lambda e, j=j, g=g: e.matmul(pzi[0:64, 0:NCTX], Xc[:, j, g, :], C["nS256"][:, j, :],
                                                                      start=(j == 0), stop=(j == 1)),
                               reads=[Xc, C["nS256"]], writes=[pzi])
                        op("scalar", lambda e: e.copy(out=zc[0][:], in_=pzr[0:64, 0:NCTX]), reads=[pzr], writes=[zc[0]])
                        op("scalar", lambda e: e.copy(out=zc[1][:], in_=pzi[0:64, 0:NCTX]), reads=[pzi],
                           writes=[zc[1]])
                        py = PS[0]
                        op("tensor", lambda e: e.matmul(py[0:64, 0:NCTX], C["CDc"][:], zc[0][:], start=True, stop=False),
                           reads=[C["CDc"], zc[0]], writes=[py])
                        op("tensor", lambda e: e.matmul(py[0:64, 0:NCTX], C["SDc"][:], zc[1][:], start=False, stop=True),
                           reads=[C["SDc"], zc[1]], writes=[py])
                        yo = yo2[g % 2]
                        op("scalar", lambda e, yo=yo: e.copy(out=yo[:, 0:NCTX], in_=py[0:64, 0:NCTX]), reads=[py],
                           writes=[yo])
                        dma("sync", lambda e, yo=yo, g=g: e.dma_start(out=FY[g, :, NL:NT], in_=yo[:, 0:NCTX]), reads=[yo])
                k.barrier()

            with Phase() as ph:
                if STOP < 4:
                    raise SkipPhase()
                wb = k.sb(ph, [64, 16, D], BF16, "wb")
                for h0 in (0, 2, 4, 6):
                    load_cast(wb[:, h0:h0 + 2, :], wb, w_ba[L, :, h0:h0 + 2, :], [64, 2, D])
                for h0 in (0, 2):
                    load_cast(wb[:, 8 + h0:10 + h0, :], wb, w_bs[L, :, h0:h0 + 2, :], [64, 2, D])
                    load_cast(wb[:, 12 + h0:14 + h0, :], wb, w_bf[L, :, h0:h0 + 2, :], [64, 2, D])
                wo = k.sb(ph, [128, 8, D], BF16, "wo")
                for kp in range(4):
                    load_cast(wo[:, 2 * kp:2 * kp + 2, :], wo,
                              w_o[L, kp * 256:(kp + 1) * 256, :].rearrange("(a p) n -> p a n", p=128), [128, 2, D])
                wr = k.sb(ph, [128, 8, NE], F32, "wr")
                load_f32(wr[:], wr, w_r[L].rearrange("(a p) n -> p a n", p=128))
                rw = load_rows(ph, [2, 3, 4])
                Bt2 = [k.sb(ph, [64, 16, 512], BF16, "Bt") for _ in range(2)]
                Gt3 = [k.sb(ph, [128, 3, 512], BF16, "Gt") for _ in range(3)]
                m1 = k.sb(ph, [128, 512], F32, "m1")
                m2 = k.sb(ph, [128, 512], F32, "m2")
                m3 = k.sb(ph, [128, 512], F32, "m3")
                mT = k.sb(ph, [128, 8, 512], BF16, "mT")
                xt2 = [k.sb(ph, [128, D], F32, "xt") for _ in range(2)]
                xm2 = [k.sb(ph, [128, D], F32, "xm") for _ in range(2)]
                tt = k.sb(ph, [128, D], F32, "tt")
                junk = k.sb(ph, [128, D], BF16, "junk")
                h2f2 = [k.sb(ph, [128, D], F32, "h2f") for _ in range(2)]
                h2b2 = [k.sb(ph, [128, D], BF16, "h2b")] * 2
                h2T = k.sb(ph, [128, 8, 128], F32, "h2T")
                ss = k.sb(ph, [128, 1], F32, "ss")
                sq = k.sb(ph, [128, 1], F32, "sq")
                rstd = k.sb(ph, [128, 1], F32, "rstd")
                mx = k.sb(ph, [128, 1], F32, "mx")
                nmx = k.sb(ph, [128, 1], F32, "nmx")
                ex = k.sb(ph, [128, NE], F32, "ex")
                sm = k.sb(ph, [128, 1], F32, "sm")
                rsm = k.sb(ph, [128, 1], F32, "rsm")
                af2 = [k.sb(ph, [128, NE], F32, "af") for _ in range(2)]
                tix = 0
                agroups = [gr for gr in groups if not (gr[2] == 1 and last)]
                GTv = GT.rearrange("(b o) p t -> o p b t", b=3)

                def p5_loadB(gi):
                    tok0, GW, v = agroups[gi]
                    Bt = Bt2[gi % 2]
                    load_f32(Bt[:, 0:8, 0:GW], Bt, AT[:, :, tok0:tok0 + GW].rearrange("h d t -> d h t"))
                    load_f32(Bt[:, 8:12, 0:GW], Bt, ST[:, :, tok0:tok0 + GW].rearrange("h d t -> d h t"))
                    load_f32(Bt[:, 12:16, 0:GW], Bt, FY[:, :, tok0:tok0 + GW].rearrange("h d t -> d h t"))

                gseq = [(gi, oc) for gi in range(len(agroups)) for oc in range(8)]

                def p5_loadG(qi_):
                    gi, oc = gseq[qi_]
                    tok0, GW, v = agroups[gi]
                    gt = Gt3[qi_ % 3]
                    load_f32(gt[:, :, 0:GW], gt, GTv[oc, :, :, tok0:tok0 + GW])

                def stageB(info):
                    h2f, af, r0 = info
                    for hf in range(2):
                        pt = PS[4 + hf]
                        for kk in range(4):
                            kc = hf * 4 + kk
                            op("tensor", lambda e, pt=pt, kk=kk, kc=kc: e.transpose(
                                pt[:, kk * 128:(kk + 1) * 128], h2f[:, kc * 128:(kc + 1) * 128], C["ident_f"][:]),
                               reads=[h2f, C["ident_f"]], writes=[pt])
                        op("scalar", lambda e, pt=pt, hf=hf: e.copy(
                            out=h2T[:, hf * 4:(hf + 1) * 4, :], in_=pt[:, :].rearrange("p (k t) -> p k t", k=4)),
                           reads=[pt], writes=[h2T])
                    pl = PS[6]
                    for kc in range(8):
                        op("tensor", lambda e, kc=kc: e.matmul(pl[:, 0:NE], h2T[:, kc, :], wr[:, kc, :],
                                                               start=(kc == 0), stop=(kc == 7)),
                           reads=[h2T, wr], writes=[pl])
                    op("vector", lambda e: e.reduce_max(out=mx[:], in_=pl[:, 0:NE], axis=AX.X), reads=[pl], writes=[mx])
                    op("vector", lambda e: e.tensor_scalar(out=nmx[:], in0=mx[:], scalar1=-1.0, scalar2=None,
                                                           op0=ALU.mult), reads=[mx], writes=[nmx])
                    op("scalar", lambda e: e.activation(out=ex[:], in_=pl[:, 0:NE], func=AF.Exp, bias=nmx[:],
                                                        accum_out=sm[:]), reads=[pl, nmx], writes=[ex, sm])
                    op("vector", lambda e: e.reciprocal(out=rsm[:], in_=sm[:]), reads=[sm], writes=[rsm])
                    op("vector", lambda e: e.tensor_scalar(out=af[:], in0=ex[:], scalar1=rsm[:, 0:1],
                                                           scalar2=None, op0=ALU.mult),
                       reads=[ex, rsm], writes=[af])
                    dma("sync", lambda e: e.dma_start(out=AFF[r0:r0 + 128, :], in_=af[:]), reads=[af])

                p5_loadB(0)
                p5_loadG(0)
                gq = 0
                for gi, (tok0, GW, v) in enumerate(agroups):
                    ntile = GW // 128
                    Bt = Bt2[gi % 2]
                    if gi + 1 < len(agroups):
                        p5_loadB(gi + 1)
                    for oc in range(8):
                        Gt = Gt3[gq % 3]
                        if gq + 1 < len(gseq):
                            p5_loadG(gq + 1)
                        gq += 1
                        osl = slice(oc * 128, (oc + 1) * 128)
                        pA, pS_, pF = PS[0 + (oc % 2) * 3], PS[1 + (oc % 2) * 3], PS[2 + (oc % 2) * 3]
                        for (pp, h0, nh) in ((pA, 0, 8), (pS_, 8, 4), (pF, 12, 4)):
                            for hh in range(nh):
                                op("tensor", lambda e, pp=pp, h0=h0, hh=hh, nh=nh, osl=osl: e.matmul(
                                    pp[:, 0:GW], wb[:, h0 + hh, osl], Bt[:, h0 + hh, 0:GW],
                                    start=(hh == 0), stop=(hh == nh - 1)), reads=[wb, Bt], writes=[pp])
                        op("vector", lambda e, pA=pA, Gt=Gt: e.tensor_tensor(out=m1[:, 0:GW], in0=pA[:, 0:GW],
                                                                             in1=Gt[:, 0, 0:GW], op=ALU.mult),
                           reads=[pA, Gt], writes=[m1])
                        op("vector", lambda e, pS_=pS_, Gt=Gt: e.tensor_tensor(out=m2[:, 0:GW], in0=pS_[:, 0:GW],
                                                                               in1=Gt[:, 1, 0:GW], op=ALU.mult),
                           reads=[pS_, Gt], writes=[m2])
                        op("vector", lambda e, pF=pF, Gt=Gt: e.tensor_tensor(out=m3[:, 0:GW], in0=pF[:, 0:GW],
                                                                             in1=Gt[:, 2, 0:GW], op=ALU.mult),
                           reads=[pF, Gt], writes=[m3])
                        op("gpsimd", lambda e: e.tensor_tensor(out=m1[:, 0:GW], in0=m1[:, 0:GW], in1=m2[:, 0:GW],
                                                               op=ALU.add), reads=[m1, m2], writes=[m1])
                        op("gpsimd", lambda e, oc=oc: e.tensor_tensor(out=mT[:, oc, 0:GW], in0=m1[:, 0:GW],
                                                                      in1=m3[:, 0:GW], op=ALU.add),
                           reads=[m1, m3], writes=[mT])
                    pend = None
                    for j in range(ntile):
                        r0 = tok0 + j * 128
                        xt = xt2[tix % 2]
                        xm = xm2[tix % 2]
                        h2b = h2b2[tix % 2]
                        af = af2[tix % 2]
                        h2f = h2f2[tix % 2]
                        pob = (tix % 2) * 2
                        tix += 1
                        load_f32(xt[:], xt, R[r0:r0 + 128, :])
                        for hf in range(2):
                            hs = slice(hf * 512, (hf + 1) * 512)
                            po = PS[pob + hf]
                            for kc in range(8):
                                op("tensor", lambda e, kc=kc, j=j, hs=hs, po=po: e.matmul(
                                    po[:, :], mT[:, kc, j * 128:(j + 1) * 128], wo[:, kc, hs],
                                    start=(kc == 0), stop=(kc == 7)), reads=[mT, wo], writes=[po])
                            op("vector", lambda e, po=po, hs=hs: e.tensor_tensor(out=tt[:, hs], in0=po[:, :],
                                                                                 in1=rw[(2, v)][:, hs], op=ALU.mult),
                               reads=[po, rw[(2, v)]], writes=[tt])
                        op("gpsimd", lambda e, xm=xm, xt=xt: e.tensor_tensor(out=xm[:], in0=tt[:], in1=xt[:], op=ALU.add),
                           reads=[tt, xt], writes=[xm])
                        dma("sync", lambda e, xm=xm, r0=r0: e.dma_start(out=R[r0:r0 + 128, :], in_=xm[:]), reads=[xm])
                        rms_rstd(ph, xm, ss, sq, rstd, junk)
                        op("vector", lambda e, xm=xm: e.scalar_tensor_tensor(
                            out=tt[:], in0=xm[:], scalar=rstd[:, 0:1], op0=ALU.mult, in1=rw[(3, v)][:], op1=ALU.mult),
                           reads=[xm, rstd, rw[(3, v)]], writes=[tt])
                        op("vector", lambda e, h2f=h2f: e.tensor_tensor(out=h2f[:], in0=tt[:], in1=rw[(4, v)][:],
                                                                        op=ALU.add),
                           reads=[tt, rw[(4, v)]], writes=[h2f])
                        op("gpsimd", lambda e, h2b=h2b, h2f=h2f: e.tensor_copy(out=h2b[:], in_=h2f[:]), reads=[h2f],
                           writes=[h2b])
                        dma("sync", lambda e, h2b=h2b, r0=r0: e.dma_start(out=H2[r0:r0 + 128, :], in_=h2b[:]), reads=[h2b])
                        if pend is not None:
                            stageB(pend)
                        pend = (h2f, af, r0)
                    if pend is not None:
                        stageB(pend)
                k.barrier()

            with Phase() as ph:
                if STOP < 5:
                    raise SkipPhase()
                def bisect(A3, np_, inner, cap, sumfn, tag):
                    lo = k.sb(ph, [np_, NE], F32, "lo" + tag)
                    hi = k.sb(ph, [np_, NE], F32, "hi" + tag)
                    mid = k.sb(ph, [np_, NE], F32, "mid" + tag)
                    Mk = k.sb(ph, [np_, inner, NE], F32, "Mk" + tag)
                    cnt = k.sb(ph, [np_, NE], F32, "cnt" + tag)
                    ge = k.sb(ph, [np_, NE], F32, "ge" + tag)
                    tmp = k.sb(ph, [np_, NE], F32, "tmp" + tag)
                    op("vector", lambda e: e.memset(lo[:], 0.0), writes=[lo])
                    op("vector", lambda e: e.memset(hi[:], 1.0), writes=[hi])
                    pc = PS[0]
                    for it in range(34):
                        op("vector", lambda e: e.tensor_tensor(out=mid[:], in0=lo[:], in1=hi[:], op=ALU.add),
                           reads=[lo, hi], writes=[mid])
                        op("vector", lambda e: e.tensor_scalar(out=mid[:], in0=mid[:], scalar1=0.5, scalar2=None,
                                                               op0=ALU.mult), reads=[mid], writes=[mid])
                        op("vector", lambda e: e.tensor_tensor(out=Mk[:], in0=A3,
                                                               in1=mid[:].unsqueeze(1).to_broadcast([np_, inner, NE]),
                                                               op=ALU.is_ge), reads=[mid, Abuf], writes=[Mk])
                        op("vector", lambda e: e.tensor_reduce(out=cnt[:], in_=Mk[:].rearrange("p i e -> p e i"),
                                                               axis=AX.X, op=ALU.add), reads=[Mk], writes=[cnt])
                        op("tensor", lambda e: e.matmul(pc[0:np_, 0:NE], C["ones_f"][0:np_, 0:np_], cnt[:],
                                                        start=True, stop=True), reads=[cnt, C["ones_f"]], writes=[pc])
                        op("vector", lambda e: e.tensor_scalar(out=ge[:], in0=pc[0:np_, 0:NE], scalar1=float(cap),
                                                               scalar2=None, op0=ALU.is_ge), reads=[pc], writes=[ge])
                        op("vector", lambda e: e.tensor_tensor(out=tmp[:], in0=ge[:], in1=mid[:], op=ALU.mult),
                           reads=[ge, mid], writes=[tmp])
                        op("vector", lambda e: e.tensor_tensor(out=lo[:], in0=lo[:], in1=tmp[:], op=ALU.max),
                           reads=[lo, tmp], writes=[lo])
                        op("vector", lambda e: e.scalar_tensor_tensor(out=tmp[:], in0=ge[:], scalar=2.0, op0=ALU.mult,
                                                                      in1=mid[:], op1=ALU.add),
                           reads=[ge, mid], writes=[tmp])
                        op("vector", lambda e: e.tensor_tensor(out=hi[:], in0=hi[:], in1=tmp[:], op=ALU.min),
                           reads=[hi, tmp], writes=[hi])
                    return lo, Mk

                A = k.sb(ph, [NB, 128, NE], F32, "A")
                Abuf = A
                load_f32(A[:], A, AFF[0:NL, :].rearrange("(t i) e -> t i e", i=128))
                thr, Mk = bisect(A[:], NB, 128, CAP, None, "l")
                op("vector", lambda e: e.tensor_tensor(out=Mk[:], in0=A[:],
                                                       in1=thr[:].unsqueeze(1).to_broadcast([NB, 128, NE]), op=ALU.is_ge),
                   reads=[A, thr], writes=[Mk])
                Mk2 = k.sb(ph, [NB, 128, NE], F32, "Mk2")
                cur, oth = Mk, Mk2
                dstep = 1
                while dstep < 128:
                    op("vector", lambda e, cur=cur, oth=oth, d=dstep: e.tensor_copy(out=oth[:, 0:d, :], in_=cur[:, 0:d, :]),
                       reads=[cur], writes=[oth])
                    op("vector", lambda e, cur=cur, oth=oth, d=dstep: e.tensor_tensor(
                        out=oth[:, d:128, :], in0=cur[:, d:128, :], in1=cur[:, 0:128 - d, :], op=ALU.add),
                       reads=[cur], writes=[oth])
                    cur, oth = oth, cur
                    dstep *= 2
                CL = cur
                CLe = k.sb(ph, [NB, NE, 128], F32, "CLe")
                op("vector", lambda e: e.tensor_copy(out=CLe[:], in_=CL[:].rearrange("p i e -> p e i")), reads=[CL],
                   writes=[CLe])
                cntl = k.sb(ph, [NB, NE], F32, "cntl")
                op("vector", lambda e: e.tensor_copy(out=cntl[:], in_=CL[:, 127, :]), reads=[CL], writes=[cntl])
                pe_ = PS[1]
                op("tensor", lambda e: e.matmul(pe_[0:NB, 0:NE], C["ustrict"][0:NB, 0:NB], cntl[:], start=True, stop=True),
                   reads=[cntl, C["ustrict"]], writes=[pe_])
                meta = k.sb(ph, [NB, NE, 2], F32, "meta")
                INC = k.sb(ph, [NB, NE], F32, "INC")
                op("vector", lambda e: e.tensor_copy(out=meta[:, :, 0], in_=pe_[0:NB, 0:NE]), reads=[pe_], writes=[meta])
                op("vector", lambda e: e.tensor_copy(out=meta[:, :, 1], in_=C["pcol"][0:NB, 0:1].to_broadcast([NB, NE])),
                   reads=[C["pcol"]], writes=[meta])
                op("vector", lambda e: e.tensor_tensor(out=INC[:], in0=meta[:, :, 0], in1=cntl[:], op=ALU.add),
                   reads=[meta, cntl], writes=[INC])
                oh1 = k.sb(ph, [NB, 128], F32, "oh1")
                oh2 = [k.sb(ph, [NB, 128], F32, "oh") for _ in range(2)]
                rr = k.sb(ph, [128, 1], F32, "rr")
                Vt = k.sb(ph, [NB, 128], F32, "Vt")
                il = k.sb(ph, [128, 1], F32, "il")
                idf = k.sb(ph, [128, 1], F32, "idf")
                jk = k.sb(ph, [128, 128], F32, "jk")
                def p6_front(ii, ex_, S):
                    oh = oh2[ii % 2]
                    pp = PS[2 + (ii % 2)]
                    io = C["iota"][0:NB, S * SP:S * SP + SP]
                    op("vector", lambda e: e.tensor_scalar(
                        out=oh1[:, 0:SP], in0=io, scalar1=meta[:, ex_, 0:1], scalar2=None, op0=ALU.is_ge),
                       reads=[C["iota"], meta], writes=[oh1])
                    op("vector", lambda e: e.scalar_tensor_tensor(
                        out=oh[:, 0:SP], in0=io, scalar=INC[:, ex_:ex_ + 1], op0=ALU.is_lt, in1=oh1[:, 0:SP],
                        op1=ALU.mult), reads=[C["iota"], INC, oh1], writes=[oh])
                    op("tensor", lambda e: e.matmul(
                        pp[0:SP, 0:128], oh[:, 0:SP], CLe[:, ex_, :], start=True, stop=True),
                       reads=[oh, CLe], writes=[pp])
                    op("tensor", lambda e: e.matmul(
                        pp[0:SP, 128:130], oh[:, 0:SP], meta[:, ex_, :], start=True, stop=True),
                       reads=[oh, meta], writes=[pp])
                    op("vector", lambda e: e.scalar_tensor_tensor(
                        out=Vt[:, 0:SP], in0=io, scalar=meta[:, ex_, 0:1], op0=ALU.subtract, in1=oh[:, 0:SP],
                        op1=ALU.mult), reads=[C["iota"], meta, oh], writes=[Vt])
                    op("tensor", lambda e: e.matmul(
                        pp[0:SP, 130:131], Vt[:, 0:SP], C["ones_f"][0:NB, 0:1], start=True, stop=True),
                       reads=[Vt, C["ones_f"]], writes=[pp])

                def p6_back(ii, ex_, S):
                    pp = PS[2 + (ii % 2)]
                    op("vector", lambda e: e.tensor_copy(out=rr[0:SP, :], in_=pp[0:SP, 130:131]),
                       reads=[pp], writes=[rr])
                    op("vector", lambda e: e.tensor_scalar(
                        out=jk[0:SP, :], in0=pp[0:SP, 0:128], scalar1=rr[0:SP, 0:1], scalar2=0.0, op0=ALU.is_le,
                        op1=ALU.add, accum_out=il[0:SP, :]), reads=[pp, rr], writes=[jk, il])
                    op("vector", lambda e: e.scalar_tensor_tensor(
                        out=idf[0:SP, :], in0=pp[0:SP, 129:130], scalar=128.0, op0=ALU.mult, in1=il[0:SP, :],
                        op1=ALU.add), reads=[pp, il], writes=[idf])
                    col = ex_ * NS + S
                    op("vector", lambda e: e.tensor_copy(out=idx_t[0:SP, col:col + 1], in_=idf[0:SP, :]),
                       reads=[idf], writes=[idx_t])

                p6items = [(ii, ex_, S) for ii, (ex_, S) in enumerate((a, b) for a in range(NE) for b in range(NS))]
                p6_front(*p6items[0])
                for n_, it_ in enumerate(p6items):
                    if n_ + 1 < len(p6items):
                        p6_front(*p6items[n_ + 1])
                    p6_back(*it_)
                if not last:
                    Ac = k.sb(ph, [128, 2, NE], F32, "Ac")
                    Abuf = Ac
                    load_f32(Ac[:], Ac, AFF[NL:NT, :].rearrange("(t i) e -> i t e", i=128))
                    thc, Mc = bisect(Ac[:], 128, 2, 32, None, "c")
                    op("vector", lambda e: e.tensor_tensor(out=Mc[:], in0=Ac[:],
                                                           in1=thc[:].unsqueeze(1).to_broadcast([128, 2, NE]),
                                                           op=ALU.is_ge), reads=[Ac, thc], writes=[Mc])
                    op("vector", lambda e: e.tensor_tensor(out=gwc[:], in0=Mc[:], in1=Ac[:], op=ALU.mult),
                       reads=[Mc, Ac], writes=[gwc])
                k.barrier()

            with Phase() as ph:
                if STOP < 6:
                    raise SkipPhase()
                cast_engs[:] = ["vector", "scalar"]
                wg2 = k.sb(ph, [128, 8, FF], BF16, "wg")
                wu2 = k.sb(ph, [128, 8, FF], BF16, "wu")
                wd2 = k.sb(ph, [128, 16, D], BF16, "wd")
                rw = load_rows(ph, [5])
                xe2 = [k.sb(ph, [128, D], BF16, "xe") for _ in range(4)]
                ae2 = [k.sb(ph, [128, NE], F32, "ae") for _ in range(8)]
                xeT = k.sb(ph, [128, 8, 512], BF16, "xeT")
                sa = k.sb(ph, [128, 512], F32, "sa")
                hTt = k.sb(ph, [128, 16, 512], BF16, "hTt")
                y2 = [k.sb(ph, [128, D], F32, "y") for _ in range(2)]
                cacc = k.sb(ph, [128, 2, D], F32, "cacc")
                op("gpsimd", lambda e: e.memset(cacc[:], 0.0), writes=[cacc])
                Rbuf = Buf("Rscatter")
                tgroups = []
                for s0 in range(0, NS, 4):
                    tgroups.append([("L", s) for s in range(s0, min(NS, s0 + 4))])
                if not last:
                    tgroups.append([("C", 0), ("C", 1)])
                yi = 0
                work = [(e_, tg_) for e_ in range(NE) for tg_ in tgroups]
                fetched = {}

                def fetch(wi):
                    e_, tg_ = work[wi]
                    out = []
                    for j, (kind, s_) in enumerate(tg_):
                        xe = xe2[j % 4]
                        ae = ae2[(wi % 2) * 4 + (j % 4)]
                        if kind == "L":
                            col = e_ * NS + s_
                            off = IndirectOffsetOnAxis(ap=idx_t[0:SP, col:col + 1], axis=0)
                            dma("gpsimd", lambda e, xe=xe, off=off: e.indirect_dma_start(
                                out=xe[0:SP, :], out_offset=None, in_=H2[:, :], in_offset=off),
                                reads=[idx_t], writes=[xe])
                            dma("gpsimd", lambda e, ae=ae, off=off: e.indirect_dma_start(
                                out=ae[0:SP, :], out_offset=None, in_=AFF[:, :], in_offset=off),
                                reads=[idx_t], writes=[ae])
                        else:
                            load_f32(xe[:], xe, H2[NL + s_ * 128:NL + (s_ + 1) * 128, :])
                        out.append((xe, ae))
                    fetched[wi] = out

                fetch(0)
                wi_ = -1
                for ex_ in range(NE):
                    for kc in range(8):
                        load_cast(wg2[:, kc, :], wg2, w_g[L, ex_, kc * 128:(kc + 1) * 128, :], [128, FF])
                        load_cast(wu2[:, kc, :], wu2, w_u[L, ex_, kc * 128:(kc + 1) * 128, :], [128, FF])
                    for kp in range(8):
                        load_cast(wd2[:, 2 * kp:2 * kp + 2, :], wd2,
                                  w_d[L, ex_, kp * 256:(kp + 1) * 256, :].rearrange("(a p) n -> p a n", p=128),
                                  [128, 2, D])
                    for tg in tgroups:
                        wi_ += 1
                        GWt = 0
                        gates_ = []
                        for j, (kind, s) in enumerate(tg):
                            xe, ae = fetched[wi_][j]
                            if kind == "L":
                                np_ = SP
                                col = ex_ * NS + s
                                gates_.append((ae, ae[0:SP, ex_:ex_ + 1], np_, kind, col))
                            else:
                                np_ = 128
                                gates_.append((gwc, gwc[:, s, ex_:ex_ + 1], np_, kind, s))
                            pt = PS[j % 2]
                            ptv = pt[:, :].bitcast(BF16)
                            for kc in range(8):
                                op("tensor", lambda e, kc=kc, ptv=ptv, xe=xe, np_=np_: e.transpose(
                                    ptv[:, kc * 128:kc * 128 + np_], xe[0:np_, kc * 128:(kc + 1) * 128],
                                    C["ident_bf"][0:np_, 0:np_]), reads=[xe, C["ident_bf"]], writes=[pt])
                            op("scalar", lambda e, ptv=ptv, np_=np_, GWt=GWt: e.copy(
                                out=xeT[:, :, GWt:GWt + np_],
                                in_=ptv.rearrange("p (k t) -> p k t", k=8)[:, :, 0:np_]), reads=[pt], writes=[xeT])
                            gates_[-1] = gates_[-1] + (GWt,)
                            GWt += np_
                        for fc in range(16):
                            fsl = slice(fc * 128, (fc + 1) * 128)
                            pa = PS[2 + (fc % 2) * 2]
                            pu = PS[3 + (fc % 2) * 2]
                            for kc in range(8):
                                op("tensor", lambda e, kc=kc, fsl=fsl, pa=pa: e.matmul(
                                    pa[:, 0:GWt], wg2[:, kc, fsl], xeT[:, kc, 0:GWt], start=(kc == 0), stop=(kc == 7)),
                                   reads=[wg2, xeT], writes=[pa])
                            for kc in range(8):
                                op("tensor", lambda e, kc=kc, fsl=fsl, pu=pu: e.matmul(
                                    pu[:, 0:GWt], wu2[:, kc, fsl], xeT[:, kc, 0:GWt], start=(kc == 0), stop=(kc == 7)),
                                   reads=[wu2, xeT], writes=[pu])
                            op("scalar", lambda e, pa=pa: e.activation(out=sa[:, 0:GWt], in_=pa[:, 0:GWt], func=AF.Silu),
                               reads=[pa], writes=[sa])
                            op("vector", lambda e, pu=pu, fc=fc: e.tensor_tensor(out=hTt[:, fc, 0:GWt], in0=pu[:, 0:GWt],
                                                                                 in1=sa[:, 0:GWt], op=ALU.mult),
                               reads=[pu, sa], writes=[hTt])
                        if wi_ + 1 < len(work):
                            fetch(wi_ + 1)
                        for (gt_, gap, np_, kind, ref_, g0) in gates_:
                            y = y2[yi % 2]
                            yi += 1
                            v = 0 if kind == "L" else 1
                            for hf in range(2):
                                hs = slice(hf * 512, (hf + 1) * 512)
                                py = PS[6 + hf]
                                for fc in range(16):
                                    op("tensor", lambda e, fc=fc, hs=hs, py=py, g0=g0, np_=np_: e.matmul(
                                        py[0:np_, :], hTt[:, fc, g0:g0 + np_], wd2[:, fc, hs],
                                        start=(fc == 0), stop=(fc == 15)), reads=[hTt, wd2], writes=[py])
                                op("vector", lambda e, py=py, hs=hs, y=y, gap=gap, np_=np_, v=v: e.scalar_tensor_tensor(
                                    out=y[0:np_, hs], in0=py[0:np_, :], scalar=gap, op0=ALU.mult,
                                    in1=rw[(5, v)][0:np_, hs], op1=ALU.mult), reads=[py, gt_, rw[(5, v)]], writes=[y])
                            if kind == "L":
                                off = IndirectOffsetOnAxis(ap=idx_t[0:SP, ref_:ref_ + 1], axis=0)
                                dma("gpsimd", lambda e, y=y, off=off: e.indirect_dma_start(
                                    out=R[:, :], out_offset=off, in_=y[0:SP, :], in_offset=None, compute_op=ALU.add),
                                    reads=[y, idx_t], writes=[Rbuf])
                            else:
                                op("gpsimd", lambda e, y=y, ref_=ref_: e.tensor_tensor(
                                    out=cacc[:, ref_, :], in0=cacc[:, ref_, :], in1=y[:], op=ALU.add),
                                   reads=[y, cacc], writes=[cacc])
                cast_engs[:] = ["gpsimd", "vector", "scalar"]
                if not last:
                    for s in range(2):
                        xt = y2[s]
                        load_f32(xt[:], xt, R[NL + s * 128:NL + (s + 1) * 128, :])
                        op("gpsimd", lambda e, xt=xt, s=s: e.tensor_tensor(out=xt[:], in0=xt[:], in1=cacc[:, s, :],
                                                                           op=ALU.add), reads=[xt, cacc], writes=[xt])
                        dma("sync", lambda e, xt=xt, s=s: e.dma_start(out=R[NL + s * 128:NL + (s + 1) * 128, :], in_=xt[:]),
                            reads=[xt])
                k.barrier()

        with contextlib.ExitStack() as ph:
            gfr = k.sb(ph, [128, D], F32, "gfr")
            load_f32(gfr[:], gfr, g_fin.partition_broadcast(128))
            xt2 = [k.sb(ph, [128, D], F32, "xt") for _ in range(2)]
            yo2 = [k.sb(ph, [128, D], F32, "yo") for _ in range(2)]
            junk = k.sb(ph, [128, D], F32, "junk")
            ss = k.sb(ph, [128, 1], F32, "ss")
            sq = k.sb(ph, [128, 1], F32, "sq")
            rstd = k.sb(ph, [128, 1], F32, "rstd")
            for t in range(NB):
                xt = xt2[t % 2]
                yo = yo2[t % 2]
                load_f32(xt[:], xt, R[t * 128:(t + 1) * 128, :])
                rms_rstd(ph, xt, ss, sq, rstd, junk)
                op("vector", lambda e, xt=xt, yo=yo: e.scalar_tensor_tensor(
                    out=yo[:], in0=xt[:], scalar=rstd[:, 0:1], op0=ALU.mult, in1=gfr[:], op1=ALU.mult),
                   reads=[xt, rstd, gfr], writes=[yo])
                dma("sync", lambda e, yo=yo, t=t: e.dma_start(out=y_out[t * 128:(t + 1) * 128, :], in_=yo[:]), reads=[yo])
            k.barrier()
    return nc, consts


def _prep_inputs(inp, NB, DEPTH):
    f = lambda a: np.ascontiguousarray(np.asarray(a, dtype=np.float32))
    qperm = []
    for c in range(4):
        qperm += list(range(c * 64, c * 64 + 64)) + list(range((4 + c) * 64, (4 + c) * 64 + 64))
    cols = qperm + list(range(512, 768)) + list(range(1024, 1280)) + list(range(1280, 1536)) + \
        list(range(768, 1024)) + list(range(1536, 4608))
    cols = np.asarray(cols)
    w_in = f(inp["w_in"])[:DEPTH][:, :, cols]
    fm = lambda v, n: f(np.asarray(v).reshape(v.shape[0], n, 128).transpose(0, 2, 1))
    shared = {
        "w_mod": f(inp["w_mod"])[:DEPTH],
        "b_modfm": fm(np.asarray(inp["b_mod"])[:DEPTH], 48),
        "g_mixfm": fm(np.asarray(inp["g_mix"])[:DEPTH], 8),
        "g_ffnfm": fm(np.asarray(inp["g_ffn"])[:DEPTH], 8),
        "w_in": f(w_in),
        "sink": f(inp["attn_sink"])[:DEPTH],
        "wsT": f(np.asarray(inp["w_spatial"])[:DEPTH].transpose(0, 3, 1, 2)),
        "b_sp": f(np.asarray(inp["b_spatial"])[:DEPTH].reshape(DEPTH, 512)),
        "w_ba": f(np.asarray(inp["w_branch_attn"])[:DEPTH].reshape(DEPTH, 8, 64, D).transpose(0, 2, 1, 3)),
        "w_bs": f(np.asarray(inp["w_branch_sgu"])[:DEPTH].reshape(DEPTH, 4, 64, D).transpose(0, 2, 1, 3)),
        "w_bf": f(np.asarray(inp["w_branch_fourier"])[:DEPTH].reshape(DEPTH, 4, 64, D).transpose(0, 2, 1, 3)),
        "w_o": f(inp["w_out"])[:DEPTH],
        "w_r": f(inp["w_router"])[:DEPTH],
        "w_g": f(inp["w_gate"])[:DEPTH],
        "w_u": f(inp["w_up"])[:DEPTH],
        "w_d": f(inp["w_down"])[:DEPTH],
        "g_fin": f(inp["g_final"]),
    }
    return shared


def run(inp, NB, DEPTH, dbg=False):
    nc, consts = build(NB, DEPTH, dbg)
    shared = _prep_inputs(inp, NB, DEPTH)
    if STOP < 6:
        for nm in ("w_g", "w_u", "w_d"):
            shared[nm] = np.zeros((1, 1, 8, 8), np.float32)
    for kname, v in consts.items():
        shared["c_" + kname] = np.ascontiguousarray(v, dtype=np.float32)
    x = np.asarray(inp["x"], dtype=np.float32)
    ctx = np.asarray(inp["ctx"], dtype=np.float32)
    c = np.asarray(inp["c"], dtype=np.float32)
    cc = np.asarray(inp["c_ctx"], dtype=np.float32)
    B = x.shape[0]
    in_maps = []
    for b in range(B):
        m = dict(shared)
        m["x"] = np.ascontiguousarray(x[b])
        m["ctx"] = np.ascontiguousarray(ctx[b])
        cv = np.stack([c[b].reshape(8, 128).T, cc.reshape(8, 128).T], axis=-1)
        m["cvec"] = np.ascontiguousarray(cv, dtype=np.float32)
        in_maps.append(m)
    res = run_bass_kernel_spmd(nc, in_maps, core_ids=list(range(B)))
    return res


def kernel(**inputs):
    NB = np.asarray(inputs["x"]).shape[1] // 128
    DEPTH = np.asarray(inputs["w_mod"]).shape[0]
    res = run(inputs, NB, DEPTH)
    out = np.stack([np.asarray(r["y"], dtype=np.float32) for r in res.results], axis=0)
    return out
```

```python
import contextlib
import numpy as np
import concourse.bass as bass
import concourse.mybir as mybir
from concourse.bass import IndirectOffsetOnAxis
from concourse.bass_utils import run_bass_kernel_spmd

F32 = mybir.dt.float32
BF16 = mybir.dt.bfloat16
I32 = mybir.dt.int32
U32 = mybir.dt.uint32
AF = mybir.ActivationFunctionType
ALU = mybir.AluOpType
AX = mybir.AxisListType

D = 1024
NCTX = 256
NE = 16
FF = 2048
EPS = 1e-6
Q0, K0, V0, VS0, F0, U0, G0 = 0, 512, 640, 768, 1024, 1280, 1536
SAME_ENG_SYNC = True
DEBUG_ALLOC = False
NDQ = 8


import os
STOP = int(os.environ.get("K_STOP", "99"))
SUB = int(os.environ.get("K_SUB", "99"))
SUB2 = int(os.environ.get("K_SUB2", "99"))


class SkipPhase(Exception):
    pass


class Phase(contextlib.ExitStack):
    def __exit__(self, et, ev, tb):
        r = super().__exit__(et, ev, tb)
        return bool(r) or (et is SkipPhase)


class Buf:
    __slots__ = ("name", "w", "r")

    def __init__(self, name):
        self.name = name
        self.w = None
        self.r = []


class Tile:
    def __init__(self, t, name):
        self.t = t
        self.b = Buf(name)

    def __getitem__(self, idx):
        return self.t[idx]


class Multi:
    def __init__(self, tiles, name):
        self.tiles = tiles
        self.b = Buf(name)

    def __getitem__(self, idx):
        p, kc, c = idx
        return self.tiles[kc][p, c]


class Eng:
    def __init__(self, k, name, obj):
        self.name = name
        self.obj = obj
        self.sem = k.new_sem()
        self.cnt = 0
        self.waited = {}


class DQ:
    def __init__(self, k, engname):
        self.engname = engname
        self.sems = [k.new_sem() for _ in range(NDQ)]
        self.n = 0


class K:
    def __init__(self, nc, stack):
        self.nc = nc
        self.stack = stack
        self.nsem = 0
        self.semid = {}
        self.engs = {}
        for n, o in (("tensor", nc.tensor), ("vector", nc.vector), ("scalar", nc.scalar),
                     ("gpsimd", nc.gpsimd), ("sync", nc.sync)):
            self.engs[n] = Eng(self, n, o)
        self.dq = {"sync": DQ(self, "sync"), "gpsimd": DQ(self, "gpsimd")}
        self.uid = 0

    def new_sem(self):
        s = self.stack.enter_context(self.nc.semaphore("s%d" % self.nsem))
        self.semid[id(s)] = self.nsem
        self.nsem += 1
        return s

    def sb(self, stack, shape, dt, name=None):
        self.uid += 1
        nm = "%s_%d" % (name or "t", self.uid)
        t = stack.enter_context(self.nc.sbuf_tensor(nm, list(shape), dt))
        if DEBUG_ALLOC:
            print("ALLOC", nm, shape, dt)
        return Tile(t, nm)

    def _bufs(self, lst):
        out = []
        for x in lst:
            if x is None:
                continue
            out.append(x.b if hasattr(x, 'b') else x)
        return out

    def _wait(self, E, dep):
        sem, val, src = dep
        if src == E.name and (E.name == "tensor" or not SAME_ENG_SYNC):
            return
        sid = self.semid[id(sem)]
        if E.waited.get(sid, 0) >= val:
            return
        E.obj.wait_ge(sem, val)
        E.waited[sid] = val

    def _wait_deps(self, E, reads, writes):
        for b in reads:
            if b.w is not None:
                self._wait(E, b.w)
        for b in writes:
            if b.w is not None:
                self._wait(E, b.w)
            for d in b.r:
                self._wait(E, d)

    def _record(self, dep, reads, writes):
        for b in writes:
            b.w = dep
            b.r = []
        for b in reads:
            b.r.append(dep)
            if len(b.r) > 64:
                b.r = b.r[-64:]

    def op(self, e, fn, reads=(), writes=()):
        E = self.engs[e]
        reads = self._bufs(reads)
        writes = self._bufs(writes)
        self._wait_deps(E, reads, writes)
        ins = fn(E.obj)
        if E.cnt >= 30000:
            E.sem = self.new_sem()
            E.cnt = 0
        E.cnt += 1
        ins.then_inc(E.sem, 1)
        self._record((E.sem, E.cnt, e), reads, writes)

    def dma(self, q, fn, reads=(), writes=()):
        Q = self.dq[q]
        E = self.engs[Q.engname]
        reads = self._bufs(reads)
        writes = self._bufs(writes)
        self._wait_deps(E, reads, writes)
        i = Q.n % NDQ
        rnd = Q.n // NDQ
        if rnd > 0:
            self._wait(E, (Q.sems[i], 16 * rnd, "dma"))
        ins = fn(E.obj)
        ins.then_inc(Q.sems[i], 16)
        Q.n += 1
        self._record((Q.sems[i], 16 * (rnd + 1), "dma"), reads, writes)

    def barrier(self):
        deps = []
        for n, E in self.engs.items():
            if E.cnt > 0:
                deps.append((E.sem, E.cnt, "x"))
        for Q in self.dq.values():
            for i in range(NDQ):
                cnt = (Q.n - i + NDQ - 1) // NDQ
                if cnt > 0:
                    deps.append((Q.sems[i], 16 * cnt, "dma"))
        for E in self.engs.values():
            for d in deps:
                self._wait(E, d)


def _consts(NB):
    NL = NB * 128
    NT = NL + NCTX
    c = {}
    c["ident_bf"] = np.eye(128, dtype=np.float32)
    c["ident_f"] = np.eye(128, dtype=np.float32)
    rot = np.zeros((128, 128), np.float32)
    for p in range(128):
        d = p % 64
        if d < 32:
            rot[p + 32, p] = -1.0
        else:
            rot[p - 32, p] = 1.0
    c["rotT"] = rot
    pos = np.arange(NL)
    row = (pos // 64).astype(np.float64)
    col = (pos % 64).astype(np.float64)
    inv = 10000.0 ** (-np.arange(16, dtype=np.float64) / 16)
    ang = np.concatenate([row[:, None] * inv, col[:, None] * inv], axis=-1)
    cosT = np.ones((128, NT), np.float32)
    sinT = np.zeros((128, NT), np.float32)
    for p in range(128):
        j = p % 32
        cosT[p, :NL] = np.cos(ang[:, j].astype(np.float32))
        sinT[p, :NL] = np.sin(ang[:, j].astype(np.float32))
    c["cosT"] = cosT
    c["sinT"] = sinT
    kk = np.arange(128)[:, None]
    ii = np.arange(128)[None, :]
    c["maskP"] = (kk >= ii).astype(np.float32)
    c["maskN"] = (kk <= ii).astype(np.float32)
    n1 = np.arange(NB)
    angA = 2 * np.pi * np.outer(n1, n1) / NB
    c["CA"] = np.cos(angA).astype(np.float32)
    c["nSA"] = (-np.sin(angA)).astype(np.float32)
    n2 = np.arange(128)
    angT = 2 * np.pi * np.outer(n2, n1) / NL
    c["Tr"] = np.cos(angT).astype(np.float32)
    c["Ti"] = (-np.sin(angT)).astype(np.float32)
    angC = 2 * np.pi * np.outer(n2, n2) / 128
    c["C128"] = np.cos(angC).astype(np.float32)
    c["S128"] = np.sin(angC).astype(np.float32)
    c["nS128"] = (-np.sin(angC)).astype(np.float32)
    dd = np.arange(64)
    angD = 2 * np.pi * np.outer(dd, dd) / 64
    c["CD"] = (np.cos(angD) / np.sqrt(64.0 * NL)).astype(np.float32)
    c["SD"] = (np.sin(angD) / np.sqrt(64.0 * NL)).astype(np.float32)
    c["CDc"] = (np.cos(angD) / np.sqrt(64.0 * NCTX)).astype(np.float32)
    c["SDc"] = (np.sin(angD) / np.sqrt(64.0 * NCTX)).astype(np.float32)
    nn = np.arange(NCTX)
    ang256 = 2 * np.pi * np.outer(nn, nn) / NCTX
    c["C256"] = np.cos(ang256).astype(np.float32).reshape(2, 128, NCTX).transpose(1, 0, 2).copy()
    c["nS256"] = (-np.sin(ang256)).astype(np.float32).reshape(2, 128, NCTX).transpose(1, 0, 2).copy()
    c["iota"] = np.tile(np.arange(2048, dtype=np.float32)[None, :], (128, 1))
    c["pcol"] = np.arange(128, dtype=np.float32)[:, None].copy()
    c["slotcol"] = (np.arange(128, dtype=np.float32)[:, None] + 128.0 * np.arange(16)[None, :]).astype(np.float32)
    c["ustrict"] = (np.arange(128)[:, None] < np.arange(128)[None, :]).astype(np.float32)
    c["ones_f"] = np.ones((128, 128), np.float32)
    return c


BF_CONSTS = ["ident_bf", "rotT", "maskP", "maskN", "CA", "nSA", "C128", "S128", "nS128",
             "CD", "SD", "CDc", "SDc", "C256", "nS256"]


def build(NB, DEPTH, dbg=False):
    NL = NB * 128
    NT = NL + NCTX
    CAP = NL // 8
    NS = max(1, CAP // 128)
    SP = min(128, CAP)
    assert CAP % SP == 0
    nc = bass.Bass("TRN2", target_bir_lowering=False)
    consts = _consts(NB)

    def din(name, shape, dt=F32):
        return nc.dram_tensor(name, list(shape), dt, kind="ExternalInput").ap()

    def dscr(name, shape, dt):
        kind = "ExternalOutput" if dbg else "Internal"
        return nc.dram_tensor(name, list(shape), dt, kind=kind).ap()

    x_in = din("x", [NL, D])
    ctx_in = din("ctx", [NCTX, D])
    cvec = din("cvec", [128, 8, 2])
    w_mod = din("w_mod", [DEPTH, D, 6 * D])
    b_modfm = din("b_modfm", [DEPTH, 128, 48])
    g_mixfm = din("g_mixfm", [DEPTH, 128, 8])
    g_ffnfm = din("g_ffnfm", [DEPTH, 128, 8])
    w_in = din("w_in", [DEPTH, D, 4608])
    sink = din("sink", [DEPTH, 8])
    wsT = din("wsT", [DEPTH, 128, 4, 128])
    b_sp = din("b_sp", [DEPTH, 4 * 128])
    w_ba = din("w_ba", [DEPTH, 64, 8, D])
    w_bs = din("w_bs", [DEPTH, 64, 4, D])
    w_bf = din("w_bf", [DEPTH, 64, 4, D])
    w_o = din("w_o", [DEPTH, D, D])
    w_r = din("w_r", [DEPTH, D, NE])
    if STOP < 6:
        w_g = din("w_g", [1, 1, 8, 8])
        w_u = din("w_u", [1, 1, 8, 8])
        w_d = din("w_d", [1, 1, 8, 8])
    else:
        w_g = din("w_g", [DEPTH, NE, D, FF])
        w_u = din("w_u", [DEPTH, NE, D, FF])
        w_d = din("w_d", [DEPTH, NE, FF, D])
    g_fin = din("g_fin", [D])
    cin = {k: din("c_" + k, list(v.shape)) for k, v in consts.items()}
    y_out = nc.dram_tensor("y", [NL, D], F32, kind="ExternalOutput").ap()

    R = dscr("R", [NT, D], F32)
    QT = dscr("QT", [4, 128, NT], BF16)
    KT = dscr("KT", [128, NT], BF16)
    Vd = dscr("Vd", [NT, 128], BF16)
    FX = dscr("FX", [4, NT, 64], BF16)
    GT = dscr("GT", [24, 128, NT], BF16)
    AT = dscr("AT", [8, 64, NT], BF16)
    ST = dscr("ST", [4, 64, NT], BF16)
    FY = dscr("FY", [4, 64, NT], BF16)
    H2 = dscr("H2", [NT, D], BF16)
    AFF = dscr("AFF", [NT, NE], F32)
    MODROW = dscr("MODROW", [12, D], F32)

    with contextlib.ExitStack() as top:
        k = K(nc, top)
        op = k.op
        dma = k.dma
        PS = []
        for i in range(8):
            t = top.enter_context(nc.psum_tensor("ps%d" % i, [128, 512], F32))
            PS.append(Tile(t, "ps%d" % i))

        C = {}
        stg = [k.sb(top, [128, 2048], F32, "stg") for _ in range(3)]
        cast_engs = ["gpsimd", "vector", "scalar"]
        stgi = [0]

        def load_cast(dst_ap, dst_tile, src_ap, shape):
            s = stg[stgi[0] % 3]
            ce = cast_engs[stgi[0] % len(cast_engs)]
            stgi[0] += 1
            p = shape[0]
            n = int(np.prod(shape[1:]))
            sv = s[0:p, 0:n]
            if len(shape) == 3:
                sv = sv.rearrange("p (a b) -> p a b", a=shape[1])
            dma("sync", lambda e: e.dma_start(out=sv, in_=src_ap), writes=[s])
            if ce == "scalar":
                op("scalar", lambda e: e.copy(out=dst_ap, in_=sv), reads=[s], writes=[dst_tile])
            else:
                op(ce, lambda e: e.tensor_copy(out=dst_ap, in_=sv), reads=[s], writes=[dst_tile])

        def load_f32(dst_ap, dst_tile, src_ap, q="sync", **kw):
            dma(q, lambda e: e.dma_start(out=dst_ap, in_=src_ap, **kw), writes=[dst_tile])

        for name, v in consts.items():
            shp = list(v.shape)
            if name in ("cosT", "sinT"):
                continue
            if name in BF_CONSTS:
                t = k.sb(top, shp, BF16, name)
                load_cast(t[:], t, cin[name], shp)
            else:
                t = k.sb(top, shp, F32, name)
                load_f32(t[:], t, cin[name])
            C[name] = t
        ones_bf = k.sb(top, [128, 64], BF16, "ones_bf")
        op("vector", lambda e: e.memset(ones_bf[:], 1.0), writes=[ones_bf])
        eps_t = k.sb(top, [128, 1], F32, "eps")
        op("vector", lambda e: e.memset(eps_t[:], EPS), writes=[eps_t])
        cv = k.sb(top, [128, 8, 2], F32, "cv")
        load_f32(cv[:], cv, cvec)
        csil = k.sb(top, [128, 8, 2], BF16, "csil")
        op("scalar", lambda e: e.activation(out=csil[:], in_=cv[:], func=AF.Silu), reads=[cv], writes=[csil])
        idx_t = k.sb(top, [128, NE * NS], I32, "idx")
        gwc = k.sb(top, [128, 2, NE], F32, "gwc")

        dma("sync", lambda e: e.dma_start(out=R[0:NL, :], in_=x_in[:, :]))
        dma("sync", lambda e: e.dma_start(out=R[NL:NT, :], in_=ctx_in[:, :]))
        k.barrier()

        def rms_rstd(st, xt, ss, sq, rstd, junk):
            op("scalar", lambda e: e.activation(out=junk[:], in_=xt[:], func=AF.Square, accum_out=ss[:]),
               reads=[xt], writes=[junk, ss])
            op("scalar", lambda e: e.activation(out=sq[:], in_=ss[:], func=AF.Sqrt, scale=1.0 / D, bias=eps_t[:]),
               reads=[ss, eps_t], writes=[sq])
            op("vector", lambda e: e.reciprocal(out=rstd[:], in_=sq[:]), reads=[sq], writes=[rstd])

        groups = [(g * 512, 512, 0) for g in range(NL // 512)] + [(NL, NCTX, 1)]

        for L in range(DEPTH):
            last = (L == DEPTH - 1)
            with Phase() as ph:
                if STOP < 0:
                    raise SkipPhase()
                modfm = k.sb(ph, [128, 48, 2], F32, "modfm")
                bm = k.sb(ph, [128, 48], F32, "bm")
                load_f32(bm[:], bm, b_modfm[L])
                gm = k.sb(ph, [128, 8], F32, "gm")
                gf = k.sb(ph, [128, 8], F32, "gf")
                load_f32(gm[:], gm, g_mixfm[L])
                load_f32(gf[:], gf, g_ffnfm[L])
                wm = [k.sb(ph, [128, 8, 1024], BF16, "wm") for _ in range(2)]
                pm = PS[0]
                pmv = pm[:, 0:96].rearrange("p (j v) -> p j v", v=2)
                for pc in range(6):
                    w = wm[pc % 2]
                    for kp in range(4):
                        src = w_mod[L, kp * 256:(kp + 1) * 256, pc * 1024:(pc + 1) * 1024].rearrange(
                            "(a p) n -> p a n", p=128)
                        load_cast(w[:, 2 * kp:2 * kp + 2, :], w, src, [128, 2, 1024])
                    for jj in range(8):
                        j = pc * 8 + jj
                        for kc in range(8):
                            op("tensor", lambda e, w=w, jj=jj, kc=kc, j=j: e.matmul(
                                pmv[:, j, :], w[:, kc, jj * 128:(jj + 1) * 128], csil[:, kc, :],
                                start=(kc == 0), stop=(kc == 7)), reads=[w, csil], writes=[pm])
                op("vector", lambda e: e.tensor_tensor(out=modfm[:], in0=pmv,
                                                       in1=bm[:].unsqueeze(2).to_broadcast([128, 48, 2]), op=ALU.add),
                   reads=[pm, bm], writes=[modfm])
                rows = k.sb(ph, [128, 6, 2, 8], F32, "rows")

                def mv(which):
                    return modfm[:, which * 8:(which + 1) * 8, :].rearrange("p k v -> p v k")

                for r, (sc_i, g_t) in ((0, (1, gm)), (3, (4, gf))):
                    op("vector", lambda e, r=r, sc_i=sc_i, g_t=g_t: e.scalar_tensor_tensor(
                        out=rows[:, r, :, :], in0=mv(sc_i), scalar=1.0, op0=ALU.add,
                        in1=g_t[:].unsqueeze(1).to_broadcast([128, 2, 8]), op1=ALU.mult),
                       reads=[modfm, g_t], writes=[rows])
                for r, wi in ((1, 0), (2, 2), (4, 3), (5, 5)):
                    op("vector", lambda e, r=r, wi=wi: e.tensor_copy(out=rows[:, r, :, :], in_=mv(wi)),
                       reads=[modfm], writes=[rows])
                pr = PS[1]
                op("tensor", lambda e: e.transpose(pr[0:96, 0:128], rows[:].rearrange("p r v k -> p (r v k)"),
                                                   C["ident_f"][:]), reads=[rows, C["ident_f"]], writes=[pr])
                rowsT = k.sb(ph, [96, 128], F32, "rowsT")
                op("vector", lambda e: e.tensor_copy(out=rowsT[:], in_=pr[0:96, 0:128]), reads=[pr], writes=[rowsT])
                dma("sync", lambda e: e.dma_start(out=MODROW.rearrange("r (kc p) -> (r kc) p", p=128), in_=rowsT[:]),
                    reads=[rowsT])
                k.barrier()

            def load_rows(ph, rlist):
                out = {}
                for r in rlist:
                    for v in range(2):
                        t = k.sb(ph, [128, D], F32, "row%d_%d" % (r, v))
                        load_f32(t[:], t, MODROW[r * 2 + v, :].partition_broadcast(128))
                        out[(r, v)] = t
                return out

            with Phase() as ph:
                if STOP < 1:
                    raise SkipPhase()
                win = Multi([k.sb(ph, [128, 4608], BF16, "win") for _ in range(8)], "win")
                for kc in range(8):
                    for c0 in (0, 2048, 4096):
                        cw = min(2048, 4608 - c0)
                        load_cast(win[:, kc, c0:c0 + cw], win, w_in[L, kc * 128:(kc + 1) * 128, c0:c0 + cw], [128, cw])
                rw = load_rows(ph, [0, 1])
                wst = k.sb(ph, [128, 4, 128], BF16, "wst")
                load_cast(wst[:], wst, wsT[L], [128, 4, 128])
                bsb = k.sb(ph, [64, 4, 128], F32, "bsb")
                load_f32(bsb[:].rearrange("p g i -> p (g i)"), bsb, b_sp[L, :].partition_broadcast(64))
                xt2 = [k.sb(ph, [128, D], F32, "xt") for _ in range(2)]
                t1 = k.sb(ph, [128, D], F32, "t1")
                junk = t1
                hb4 = [k.sb(ph, [128, D], BF16, "hb") for _ in range(4)]
                hT2 = [k.sb(ph, [128, 8, 512], BF16, "hT") for _ in range(2)]
                ss = k.sb(ph, [128, 1], F32, "ss")
                sq = k.sb(ph, [128, 1], F32, "sq")
                rstd = k.sb(ph, [128, 1], F32, "rstd")
                cs_t = [k.sb(ph, [128, 512], F32, "cos") for _ in range(2)]
                sn_t = [k.sb(ph, [128, 512], F32, "sin") for _ in range(2)]
                qf2 = [k.sb(ph, [128, 512], F32, "qf") for _ in range(2)]
                qb2 = [k.sb(ph, [128, 512], BF16, "qb") for _ in range(2)]
                r1 = k.sb(ph, [128, 512], F32, "r1")
                r2 = k.sb(ph, [128, 512], F32, "r2")
                qo2 = [k.sb(ph, [128, 512], BF16, "qo") for _ in range(3)]
                ug = k.sb(ph, [64, 4, 512], BF16, "ug")
                vb2 = [k.sb(ph, [128, 128], BF16, "vb") for _ in range(2)]
                fb2 = [k.sb(ph, [128, 256], BF16, "fb") for _ in range(2)]
                gv = k.sb(ph, [128, 4, 64], F32, "gv")
                gsq = k.sb(ph, [128, 4, 64], F32, "gsq")
                ms = k.sb(ph, [128, 4], F32, "ms")
                ms2 = k.sb(ph, [128, 4], F32, "ms2")
                rs = k.sb(ph, [128, 4], F32, "rs")
                vn2 = [k.sb(ph, [128, 4, 64], BF16, "vn") for _ in range(2)]
                tz = k.sb(ph, [64, 4, 128], F32, "tz")
                so2 = [k.sb(ph, [64, 4, 128], BF16, "so") for _ in range(2)]
                qoi = [0]

                def prepA(gi):
                    tok0, GW, v = groups[gi]
                    cs = cs_t[gi % 2]
                    sn = sn_t[gi % 2]
                    load_f32(cs[:, 0:GW], cs, cin["cosT"][:, tok0:tok0 + GW])
                    load_f32(sn[:, 0:GW], sn, cin["sinT"][:, tok0:tok0 + GW])
                    for j in range(GW // 128):
                        xt = xt2[j % 2]
                        hb = hb4[j]
                        r0 = tok0 + j * 128
                        load_f32(xt[:], xt, R[r0:r0 + 128, :])
                        rms_rstd(ph, xt, ss, sq, rstd, junk)
                        op("vector", lambda e, xt=xt, v=v: e.scalar_tensor_tensor(
                            out=t1[:], in0=xt[:], scalar=rstd[:, 0:1], op0=ALU.mult, in1=rw[(0, v)][:], op1=ALU.mult),
                           reads=[xt, rstd, rw[(0, v)]], writes=[t1])
                        op("vector", lambda e, hb=hb, v=v: e.tensor_tensor(out=hb[:], in0=t1[:], in1=rw[(1, v)][:],
                                                                          op=ALU.add),
                           reads=[t1, rw[(1, v)]], writes=[hb])

                def prepB(gi):
                    tok0, GW, v = groups[gi]
                    hT = hT2[gi % 2]
                    for j in range(GW // 128):
                        hb = hb4[j]
                        pt = PS[j % 2]
                        ptv = pt[:, :].bitcast(BF16)
                        for kc in range(8):
                            op("tensor", lambda e, kc=kc, ptv=ptv, hb=hb: e.transpose(
                                ptv[:, kc * 128:(kc + 1) * 128], hb[:, kc * 128:(kc + 1) * 128], C["ident_bf"][:]),
                               reads=[hb, C["ident_bf"]], writes=[pt])
                        op("scalar", lambda e, ptv=ptv, j=j, hT=hT: e.copy(
                            out=hT[:, :, j * 128:(j + 1) * 128], in_=ptv.rearrange("p (k t) -> p k t", k=8)),
                           reads=[pt], writes=[hT])

                prepA(0)
                prepB(0)
                for gi, (tok0, GW, v) in enumerate(groups):
                    ntile = GW // 128
                    hT = hT2[gi % 2]
                    cs = cs_t[gi % 2]
                    sn = sn_t[gi % 2]
                    if gi + 1 < len(groups):
                        prepA(gi + 1)
                    fmc = [0]

                    def fm_mm(col0, ncol, pq):
                        for kc in range(8):
                            op("tensor", lambda e, kc=kc: e.matmul(
                                pq[0:ncol, 0:GW], win[:, kc, col0:col0 + ncol], hT[:, kc, 0:GW],
                                start=(kc == 0), stop=(kc == 7)), reads=[win, hT], writes=[pq])

                    def next_pq():
                        pq = PS[2 + (fmc[0] % 2)]
                        fmc[0] += 1
                        return pq

                    def job_rope(ci):
                        pq = next_pq()
                        fm_mm(Q0 + ci * 128, 128, pq)
                        qf = qf2[ci % 2]
                        qb = qb2[ci % 2]
                        op("scalar", lambda e: e.copy(out=qf[:, 0:GW], in_=pq[:, 0:GW]), reads=[pq], writes=[qf])
                        op("vector", lambda e: e.tensor_copy(out=qb[:, 0:GW], in_=qf[:, 0:GW]), reads=[qf], writes=[qb])

                        def follow():
                            prr = PS[4]
                            op("tensor", lambda e: e.matmul(prr[:, 0:GW], C["rotT"][:], qb[:, 0:GW], start=True,
                                                            stop=True), reads=[C["rotT"], qb], writes=[prr])
                            op("vector", lambda e: e.tensor_tensor(out=r1[:, 0:GW], in0=qf[:, 0:GW], in1=cs[:, 0:GW],
                                                                   op=ALU.mult), reads=[qf, cs], writes=[r1])
                            op("vector", lambda e: e.tensor_tensor(out=r2[:, 0:GW], in0=prr[:, 0:GW], in1=sn[:, 0:GW],
                                                                   op=ALU.mult), reads=[prr, sn], writes=[r2])
                            qo = qo2[qoi[0] % 3]
                            qoi[0] += 1
                            op("vector", lambda e: e.tensor_tensor(out=qo[:, 0:GW], in0=r1[:, 0:GW], in1=r2[:, 0:GW],
                                                                   op=ALU.add), reads=[r1, r2], writes=[qo])
                            dst = QT[ci, :, tok0:tok0 + GW] if ci < 4 else KT[:, tok0:tok0 + GW]
                            dma("sync", lambda e: e.dma_start(out=dst, in_=qo[:, 0:GW]), reads=[qo])
                        return follow, 1

                    def job_gate(j):
                        pq = next_pq()
                        fm_mm(G0 + j * 128, 128, pq)
                        qo = qo2[qoi[0] % 3]
                        qoi[0] += 1
                        op("scalar", lambda e: e.activation(out=qo[:, 0:GW], in_=pq[:, 0:GW], func=AF.Sigmoid),
                           reads=[pq], writes=[qo])
                        dma("sync", lambda e: e.dma_start(out=GT[j, :, tok0:tok0 + GW], in_=qo[:, 0:GW]), reads=[qo])
                        return None, 0

                    def job_u(g):
                        pq = next_pq()
                        fm_mm(U0 + g * 64, 64, pq)
                        op("scalar", lambda e: e.activation(out=ug[:, g, 0:GW], in_=pq[0:64, 0:GW],
                                                            func=AF.Gelu_apprx_tanh), reads=[pq], writes=[ug])
                        return None, 0

                    def job_tm(j):
                        r0 = tok0 + j * 128
                        pa = PS[5]
                        pb = PS[6]
                        for kc in range(8):
                            op("tensor", lambda e, kc=kc: e.matmul(
                                pa[:, 0:384], hT[:, kc, j * 128:(j + 1) * 128], win[:, kc, V0:V0 + 384],
                                start=(kc == 0), stop=(kc == 7)), reads=[win, hT], writes=[pa])
                        for kc in range(8):
                            op("tensor", lambda e, kc=kc: e.matmul(
                                pb[:, 0:256], hT[:, kc, j * 128:(j + 1) * 128], win[:, kc, F0:F0 + 256],
                                start=(kc == 0), stop=(kc == 7)), reads=[win, hT], writes=[pb])
                        vb = vb2[j % 2]
                        fb = fb2[j % 2]
                        vn = vn2[j % 2]
                        op("scalar", lambda e: e.copy(out=vb[:], in_=pa[:, 0:128]), reads=[pa], writes=[vb])
                        dma("sync", lambda e: e.dma_start(out=Vd[r0:r0 + 128, :], in_=vb[:]), reads=[vb])
                        op("scalar", lambda e: e.copy(out=fb[:], in_=pb[:, 0:256]), reads=[pb], writes=[fb])
                        dma("sync", lambda e: e.dma_start(
                            out=FX[:, r0:r0 + 128, :].rearrange("g t d -> t g d"),
                            in_=fb[:].rearrange("p (g d) -> p g d", g=4)), reads=[fb])
                        op("scalar", lambda e: e.activation(out=gv[:].rearrange("p g d -> p (g d)"), in_=pa[:, 128:384],
                                                            func=AF.Gelu_apprx_tanh), reads=[pa], writes=[gv])
                        op("vector", lambda e: e.tensor_tensor(out=gsq[:], in0=gv[:], in1=gv[:], op=ALU.mult),
                           reads=[gv], writes=[gsq])
                        op("vector", lambda e: e.tensor_reduce(out=ms[:], in_=gsq[:], axis=AX.X, op=ALU.add),
                           reads=[gsq], writes=[ms])
                        op("scalar", lambda e: e.activation(out=ms2[:], in_=ms[:], func=AF.Sqrt, scale=1.0 / 64,
                                                            bias=eps_t[:]), reads=[ms, eps_t], writes=[ms2])
                        op("vector", lambda e: e.reciprocal(out=rs[:], in_=ms2[:]), reads=[ms2], writes=[rs])
                        op("vector", lambda e: e.tensor_tensor(out=vn[:], in0=gv[:],
                                                               in1=rs[:].unsqueeze(2).to_broadcast([128, 4, 64]),
                                                               op=ALU.mult), reads=[gv, rs], writes=[vn])

                        def follow():
                            pz = PS[7]
                            for g in range(4):
                                op("tensor", lambda e, g=g: e.matmul(pz[0:64, g * 128:(g + 1) * 128], vn[:, g, :],
                                                                     wst[:, g, :], start=True, stop=True),
                                   reads=[vn, wst], writes=[pz])
                            op("vector", lambda e: e.tensor_tensor(
                                out=tz[:], in0=pz[0:64, :].rearrange("p (g i) -> p g i", g=4), in1=bsb[:], op=ALU.add),
                               reads=[pz, bsb], writes=[tz])
                            so = so2[j % 2]
                            op("vector", lambda e: e.tensor_tensor(out=so[:], in0=tz[:],
                                                                   in1=ug[:, :, j * 128:(j + 1) * 128], op=ALU.mult),
                               reads=[tz, ug], writes=[so])
                            dma("sync", lambda e: e.dma_start(
                                out=ST[:, :, r0:r0 + 128].rearrange("g d t -> d g t"), in_=so[:]), reads=[so])
                        return follow, 2

                    jobs = [(job_rope, ci) for ci in range(5)] + [(job_u, g) for g in range(4)]
                    gper = 24 // ntile
                    for j in range(ntile):
                        jobs += [(job_gate, jj) for jj in range(j * gper, (j + 1) * gper)]
                        jobs.append((job_tm, j))
                    pendf = []
                    for n_, (fn_, arg_) in enumerate(jobs):
                        fo, dl = fn_(arg_)
                        due = [p for p in pendf if p[0] <= n_]
                        pendf = [p for p in pendf if p[0] > n_]
                        for p in due:
                            p[1]()
                        if fo is not None:
                            pendf.append((n_ + dl, fo))
                    for p in pendf:
                        p[1]()
                    if gi + 1 < len(groups):
                        prepB(gi + 1)
                k.barrier()

            with Phase() as ph:
                if STOP < 2:
                    raise SkipPhase()
                sk = k.sb(ph, [64, 8], F32, "sk")
                load_f32(sk[:], sk, sink[L, :].partition_broadcast(64))
                se = k.sb(ph, [64, 8], F32, "se")
                op("scalar", lambda e: e.activation(out=se[:], in_=sk[:], func=AF.Exp), reads=[sk], writes=[se])
                sexp = k.sb(ph, [64, 8, 128], F32, "sexp")
                op("vector", lambda e: e.tensor_copy(out=sexp[:], in_=se[:].unsqueeze(2).to_broadcast([64, 8, 128])),
                   reads=[se], writes=[sexp])
                kc_t = k.sb(ph, [128, NCTX], BF16, "kctx")
                vc_t = k.sb(ph, [128, 2, 128], BF16, "vctx")
                load_f32(kc_t[:], kc_t, KT[:, NL:NT])
                load_f32(vc_t[:], vc_t, Vd[NL:NT, :].rearrange("(b p) d -> p b d", p=128))
                q2 = [k.sb(ph, [128, 4, 512], BF16, "q4") for _ in range(2)]
                k2 = [k.sb(ph, [128, 768], BF16, "k6") for _ in range(2)]
                v2 = [k.sb(ph, [128, 6, 128], BF16, "v6") for _ in range(2)]
                pt2 = [k.sb(ph, [128, 512], BF16, "PT") for _ in range(6)]
                den = k.sb(ph, [64, 512], F32, "den")
                rden = k.sb(ph, [64, 512], F32, "rden")
                ao2 = [k.sb(ph, [64, 4, 128], BF16, "ao") for _ in range(2)]
                pti = 0
                aoi = [0]
                psi = 0
                agroups = [gr for gr in groups if not (gr[2] == 1 and last)]

                def p2_loads(gi):
                    tok0, GW, v = agroups[gi]
                    q4 = q2[gi % 2]
                    load_f32(q4[:, :, 0:GW], q4, QT[:, :, tok0:tok0 + GW].rearrange("c p t -> p c t"))
                    if v == 0:
                        k6 = k2[gi % 2]
                        v6 = v2[gi % 2]
                        lo = max(tok0 - 128, 0)
                        hi = min(tok0 + GW + 128, NL)
                        off = lo - (tok0 - 128)
                        load_f32(k6[:, off:off + hi - lo], k6, KT[:, lo:hi])
                        load_f32(v6[:, off // 128:(off + hi - lo) // 128, :], v6,
                                 Vd[lo:hi, :].rearrange("(b p) d -> p b d", p=128))

                p2_loads(0)
                pend = []
                jobc = 0

                def emit_pv(it):
                    (bi, nb_, g, par, kb, PT, tok0_, qi_) = it
                    kt_, kcol, vt_, vblk, msk = kb
                    po = PS[3 + 2 * par]
                    pd = PS[4 + 2 * par]
                    st = (bi == 0)
                    sp = (bi == nb_ - 1)
                    op("tensor", lambda e: e.matmul(
                        po[0:64, :], vt_[:, vblk, 64 * g:64 * g + 64], PT[:], start=st, stop=sp),
                       reads=[vt_, PT], writes=[po])
                    op("tensor", lambda e: e.matmul(
                        pd[0:64, :], ones_bf[:, 0:64], PT[:], start=st, stop=sp),
                       reads=[ones_bf, PT], writes=[pd])
                    if sp:
                        op("vector", lambda e: e.tensor_tensor(
                            out=den[:], in0=pd[0:64, :],
                            in1=sexp[:, 4 * g:4 * g + 4, :].rearrange("p c q -> p (c q)"), op=ALU.add),
                           reads=[pd, sexp], writes=[den])
                        op("vector", lambda e: e.reciprocal(out=rden[:], in_=den[:]), reads=[den], writes=[rden])
                        ao = ao2[aoi[0] % 2]
                        aoi[0] += 1
                        op("vector", lambda e: e.tensor_tensor(
                            out=ao[:].rearrange("p c q -> p (c q)"), in0=po[0:64, :], in1=rden[:], op=ALU.mult),
                           reads=[po, rden], writes=[ao])
                        r0 = tok0_ + qi_ * 128
                        dma("sync", lambda e: e.dma_start(
                            out=AT[4 * g:4 * g + 4, :, r0:r0 + 128].rearrange("h d t -> d h t"), in_=ao[:]),
                            reads=[ao])

                for gi, (tok0, GW, v) in enumerate(agroups):
                    nq = GW // 128
                    q4 = q2[gi % 2]
                    k6 = k2[gi % 2]
                    v6 = v2[gi % 2]
                    while pend:
                        emit_pv(pend.pop(0))
                    if gi + 1 < len(agroups):
                        p2_loads(gi + 1)
                    for g in range(2):
                        for qi in range(nq):
                            nblk = tok0 // 128 + qi
                            par = jobc % 2
                            jobc += 1
                            kbs = []
                            if v == 0:
                                if nblk > 0:
                                    kbs.append((k6, qi * 128, v6, qi, "P"))
                                kbs.append((k6, (qi + 1) * 128, v6, qi + 1, None))
                                if nblk < NB - 1:
                                    kbs.append((k6, (qi + 2) * 128, v6, qi + 2, "N"))
                            kbs.append((kc_t, 0, vc_t, 0, None))
                            kbs.append((kc_t, 128, vc_t, 1, None))
                            for bi, kb in enumerate(kbs):
                                kt_, kcol, vt_, vblk, msk = kb
                                psS = PS[psi % 3]
                                psi += 1
                                op("tensor", lambda e, kt_=kt_, kcol=kcol, psS=psS, g=g, qi=qi: e.matmul(
                                    psS[:, :].rearrange("p (c q) -> p c q", c=4),
                                    kt_[64 * g:64 * g + 64, kcol:kcol + 128],
                                    q4[64 * g:64 * g + 64, :, qi * 128:(qi + 1) * 128], start=True, stop=True),
                                   reads=[kt_, q4], writes=[psS])
                                PT = pt2[pti % 6]
                                pti += 1
                                op("scalar", lambda e, psS=psS, PT=PT: e.activation(out=PT[:], in_=psS[:, :], func=AF.Exp,
                                                                                    scale=0.125), reads=[psS], writes=[PT])
                                if msk is not None:
                                    mt = C["maskP"] if msk == "P" else C["maskN"]
                                    op("vector", lambda e, PT=PT, mt=mt: e.tensor_tensor(
                                        out=PT[:].rearrange("p (c q) -> p c q", c=4),
                                        in0=PT[:].rearrange("p (c q) -> p c q", c=4),
                                        in1=mt[:].unsqueeze(1).to_broadcast([128, 4, 128]), op=ALU.mult),
                                       reads=[PT, mt], writes=[PT])
                                pend.append((bi, len(kbs), g, par, kb, PT, tok0, qi))
                                if len(pend) > 3:
                                    emit_pv(pend.pop(0))
                while pend:
                    emit_pv(pend.pop(0))
                k.barrier()

            with Phase() as ph:
                if STOP < 3:
                    raise SkipPhase()
                Xg = k.sb(ph, [NB, 128, 64], BF16, "Xg")
                Bre = k.sb(ph, [128, 64, NB], BF16, "Bre")
                Bim = k.sb(ph, [128, 64, NB], BF16, "Bim")
                ZrT = k.sb(ph, [64, NL], BF16, "ZrT")
                ZiT = k.sb(ph, [64, NL], BF16, "ZiT")
                tw = [k.sb(ph, [128, 4, NB], F32, "tw") for _ in range(4)]
                yo2 = [k.sb(ph, [64, 512], BF16, "yo") for _ in range(2)]
                Trb = C["Tr"][:].unsqueeze(1).to_broadcast([128, 4, NB])
                Tib = C["Ti"][:].unsqueeze(1).to_broadcast([128, 4, NB])
                KG = max(1, min(4, NB))
                for g in range(4):
                    dma("sync", lambda e, g=g: e.dma_start(out=Xg[:], in_=FX[g, 0:NL, :].rearrange(
                        "(a b) c -> a b c", b=128)), writes=[Xg])
                    for cq in range(16):
                        par = PS[0 + (cq % 2) * 2]
                        pai = PS[1 + (cq % 2) * 2]
                        parv = par[:, 0:4 * NB].rearrange("p (c k) -> p c k", c=4)
                        paiv = pai[:, 0:4 * NB].rearrange("p (c k) -> p c k", c=4)
                        for cc in range(4):
                            ch = cq * 4 + cc
                            op("tensor", lambda e, ch=ch, cc=cc, parv=parv, par=par: e.matmul(
                                parv[:, cc, :], Xg[0:NB, :, ch], C["CA"][0:NB, 0:NB], start=True, stop=True),
                               reads=[Xg, C["CA"]], writes=[par])
                            op("tensor", lambda e, ch=ch, cc=cc, paiv=paiv, pai=pai: e.matmul(
                                paiv[:, cc, :], Xg[0:NB, :, ch], C["nSA"][0:NB, 0:NB], start=True, stop=True),
                               reads=[Xg, C["nSA"]], writes=[pai])
                        c0 = cq * 4
                        op("vector", lambda e, parv=parv, par=par: e.tensor_tensor(out=tw[0][:], in0=parv, in1=Trb,
                                                                                   op=ALU.mult),
                           reads=[par, C["Tr"]], writes=[tw[0]])
                        op("vector", lambda e, paiv=paiv, pai=pai: e.tensor_tensor(out=tw[1][:], in0=paiv, in1=Tib,
                                                                                   op=ALU.mult),
                           reads=[pai, C["Ti"]], writes=[tw[1]])
                        op("gpsimd", lambda e, c0=c0: e.tensor_tensor(out=Bre[:, c0:c0 + 4, :], in0=tw[0][:], in1=tw[1][:],
                                                                      op=ALU.subtract),
                           reads=[tw[0], tw[1]], writes=[Bre])
                        op("vector", lambda e, parv=parv, par=par: e.tensor_tensor(out=tw[2][:], in0=parv, in1=Tib,
                                                                                   op=ALU.mult),
                           reads=[par, C["Ti"]], writes=[tw[2]])
                        op("vector", lambda e, paiv=paiv, pai=pai: e.tensor_tensor(out=tw[3][:], in0=paiv, in1=Trb,
                                                                                   op=ALU.mult),
                           reads=[pai, C["Tr"]], writes=[tw[3]])
                        op("gpsimd", lambda e, c0=c0: e.tensor_tensor(out=Bim[:, c0:c0 + 4, :], in0=tw[2][:], in1=tw[3][:],
                                                                      op=ALU.add),
                           reads=[tw[2], tw[3]], writes=[Bim])
                    Zrv = ZrT[:].rearrange("d (k2 k1) -> d k1 k2", k1=NB)
                    Ziv = ZiT[:].rearrange("d (k2 k1) -> d k1 k2", k1=NB)
                    for kg in range(NB // KG):
                        pzr = PS[4 + (kg % 2) * 2]
                        pzi = PS[5 + (kg % 2) * 2]
                        for j in range(KG):
                            k1 = kg * KG + j
                            sl = slice(j * 128, (j + 1) * 128)
                            op("tensor", lambda e, k1=k1, sl=sl, pzr=pzr: e.matmul(
                                pzr[0:64, sl], Bre[:, :, k1], C["C128"][:], start=True, stop=False),
                               reads=[Bre, C["C128"]], writes=[pzr])
                            op("tensor", lambda e, k1=k1, sl=sl, pzr=pzr: e.matmul(
                                pzr[0:64, sl], Bim[:, :, k1], C["S128"][:], start=False, stop=True),
                               reads=[Bim, C["S128"]], writes=[pzr])
                            op("tensor", lambda e, k1=k1, sl=sl, pzi=pzi: e.matmul(
                                pzi[0:64, sl], Bim[:, :, k1], C["C128"][:], start=True, stop=False),
                               reads=[Bim, C["C128"]], writes=[pzi])
                            op("tensor", lambda e, k1=k1, sl=sl, pzi=pzi: e.matmul(
                                pzi[0:64, sl], Bre[:, :, k1], C["nS128"][:], start=False, stop=True),
                               reads=[Bre, C["nS128"]], writes=[pzi])
                        op("scalar", lambda e, kg=kg, pzr=pzr: e.copy(
                            out=Zrv[:, kg * KG:(kg + 1) * KG, :],
                            in_=pzr[0:64, 0:KG * 128].rearrange("p (j q) -> p j q", j=KG)), reads=[pzr], writes=[ZrT])
                        op("scalar", lambda e, kg=kg, pzi=pzi: e.copy(
                            out=Ziv[:, kg * KG:(kg + 1) * KG, :],
                            in_=pzi[0:64, 0:KG * 128].rearrange("p (j q) -> p j q", j=KG)), reads=[pzi], writes=[ZiT])
                    for ti in range(NL // 512):
                        py = PS[0 + (ti % 2)]
                        sl = slice(ti * 512, (ti + 1) * 512)
                        op("tensor", lambda e, py=py, sl=sl: e.matmul(py[0:64, :], C["CD"][:], ZrT[:, sl], start=True,
                                                                      stop=False), reads=[C["CD"], ZrT], writes=[py])
                        op("tensor", lambda e, py=py, sl=sl: e.matmul(py[0:64, :], C["SD"][:], ZiT[:, sl], start=False,
                                                                      stop=True), reads=[C["SD"], ZiT], writes=[py])
                        yo = yo2[ti % 2]
                        op("scalar", lambda e, py=py, yo=yo: e.copy(out=yo[:], in_=py[0:64, :]), reads=[py], writes=[yo])
                        dma("sync", lambda e, yo=yo, sl=sl, g=g: e.dma_start(out=FY[g, :, sl], in_=yo[:]), reads=[yo])
                if not last:
                    Xc = k.sb(ph, [128, 2, 4, 64], BF16, "Xc")
                    for j in range(2):
                        dma("sync", lambda e, j=j: e.dma_start(
                            out=Xc[:, j, :, :], in_=FX[:, NL + j * 128:NL + (j + 1) * 128, :].rearrange("g t d -> t g d")),
                            writes=[Xc])
                    zc = [k.sb(ph, [64, NCTX], BF16, "zc") for _ in range(2)]
                    for g in range(4):
                        pzr = PS[2]
                        pzi = PS[3]
                        for j in range(2):
                            op("tensor", lambda e, j=j, g=g: e.matmul(pzr[0:64, 0:NCTX], Xc[:, j, g, :], C["C256"][:, j, :],
                                                                      start=(j == 0), stop=(j == 1)),
                               reads=[Xc, C["C256"]], writes=[pzr])
                            op("tensor", lambda e, j=j, g=g: e.matmul(pzi[0:64, 0:NCTX], Xc[:, j, g, :], C["nS256"][:, j, :],
                                                                      start=(j == 0), stop=(j == 1)),
                               reads=[Xc, C["nS256"]], writes=[pzi])
                        op("scalar", lambda e: e.copy(out=zc[0][:], in_=pzr[0:64, 0:NCTX]), reads=[pzr], writes=[zc[0]])
                        op("scalar", lambda e: e.copy(out=zc[1][:], in_=pzi[0:64, 0:NCTX]), reads=[pzi],
                           writes=[zc[1]])
                        py = PS[0]
                        op("tensor", lambda e: e.matmul(py[0:64, 0:NCTX], C["CDc"][:], zc[0][:], start=True, stop=False),
                           reads=[C["CDc"], zc[0]], writes=[py])
                        op("tensor", lambda e: e.matmul(py[0:64, 0:NCTX], C["SDc"][:], zc[1][:], start=False, stop=True),
                           reads=[C["SDc"], zc[1]], writes=[py])
                        yo = yo2[g % 2]
                        op("scalar", lambda e, yo=yo: e.copy(out=yo[:, 0:NCTX], in_=py[0:64, 0:NCTX]), reads=[py],
                           writes=[yo])
                        dma("sync", lambda e, yo=yo, g=g: e.dma_start(out=FY[g, :, NL:NT], in_=yo[:, 0:NCTX]), reads=[yo])
                k.barrier()

            with Phase() as ph:
                if STOP < 4:
                    raise SkipPhase()
                wb = k.sb(ph, [64, 16, D], BF16, "wb")
                for h0 in (0, 2, 4, 6):
                    load_cast(wb[:, h0:h0 + 2, :], wb, w_ba[L, :, h0:h0 + 2, :], [64, 2, D])
                for h0 in (0, 2):
                    load_cast(wb[:, 8 + h0:10 + h0, :], wb, w_bs[L, :, h0:h0 + 2, :], [64, 2, D])
                    load_cast(wb[:, 12 + h0:14 + h0, :], wb, w_bf[L, :, h0:h0 + 2, :], [64, 2, D])
                wo = k.sb(ph, [128, 8, D], BF16, "wo")
                for kp in range(4):
                    load_cast(wo[:, 2 * kp:2 * kp + 2, :], wo,
                              w_o[L, kp * 256:(kp + 1) * 256, :].rearrange("(a p) n -> p a n", p=128), [128, 2, D])
                wr = k.sb(ph, [128, 8, NE], F32, "wr")
                load_f32(wr[:], wr, w_r[L].rearrange("(a p) n -> p a n", p=128))
                rw = load_rows(ph, [2, 3, 4])
                Bt2 = [k.sb(ph, [64, 16, 512], BF16, "Bt") for _ in range(2)]
                Gt3 = [k.sb(ph, [128, 3, 512], BF16, "Gt") for _ in range(3)]
                m1 = k.sb(ph, [128, 512], F32, "m1")
                m2 = k.sb(ph, [128, 512], F32, "m2")
                m3 = k.sb(ph, [128, 512], F32, "m3")
                mT = k.sb(ph, [128, 8, 512], BF16, "mT")
                xt2 = [k.sb(ph, [128, D], F32, "xt") for _ in range(2)]
                xm2 = [k.sb(ph, [128, D], F32, "xm") for _ in range(2)]
                tt = k.sb(ph, [128, D], F32, "tt")
                junk = k.sb(ph, [128, D], BF16, "junk")
                h2f2 = [k.sb(ph, [128, D], F32, "h2f") for _ in range(2)]
                h2b2 = [k.sb(ph, [128, D], BF16, "h2b")] * 2
                h2T = k.sb(ph, [128, 8, 128], F32, "h2T")
                ss = k.sb(ph, [128, 1], F32, "ss")
                sq = k.sb(ph, [128, 1], F32, "sq")
                rstd = k.sb(ph, [128, 1], F32, "rstd")
                mx = k.sb(ph, [128, 1], F32, "mx")
                nmx = k.sb(ph, [128, 1], F32, "nmx")
                ex = k.sb(ph, [128, NE], F32, "ex")
                sm = k.sb(ph, [128, 1], F32, "sm")
                rsm = k.sb(ph, [128, 1], F32, "rsm")
                af2 = [k.sb(ph, [128, NE], F32, "af") for _ in range(2)]
                tix = 0
                agroups = [gr for gr in groups if not (gr[2] == 1 and last)]
                GTv = GT.rearrange("(b o) p t -> o p b t", b=3)

                def p5_loadB(gi):
                    tok0, GW, v = agroups[gi]
                    Bt = Bt2[gi % 2]
                    load_f32(Bt[:, 0:8, 0:GW], Bt, AT[:, :, tok0:tok0 + GW].rearrange("h d t -> d h t"))
                    load_f32(Bt[:, 8:12, 0:GW], Bt, ST[:, :, tok0:tok0 + GW].rearrange("h d t -> d h t"))
                    load_f32(Bt[:, 12:16, 0:GW], Bt, FY[:, :, tok0:tok0 + GW].rearrange("h d t -> d h t"))

                gseq = [(gi, oc) for gi in range(len(agroups)) for oc in range(8)]

                def p5_loadG(qi_):
                    gi, oc = gseq[qi_]
                    tok0, GW, v = agroups[gi]
                    gt = Gt3[qi_ % 3]
                    load_f32(gt[:, :, 0:GW], gt, GTv[oc, :, :, tok0:tok0 + GW])

                def stageB(info):
                    h2f, af, r0 = info
                    for hf in range(2):
                        pt = PS[4 + hf]
                        for kk in range(4):
                            kc = hf * 4 + kk
                            op("tensor", lambda e, pt=pt, kk=kk, kc=kc: e.transpose(
                                pt[:, kk * 128:(kk + 1) * 128], h2f[:, kc * 128:(kc + 1) * 128], C["ident_f"][:]),
                               reads=[h2f, C["ident_f"]], writes=[pt])
                        op("scalar", lambda e, pt=pt, hf=hf: e.copy(
                            out=h2T[:, hf * 4:(hf + 1) * 4, :], in_=pt[:, :].rearrange("p (k t) -> p k t", k=4)),
                           reads=[pt], writes=[h2T])
                    pl = PS[6]
                    for kc in range(8):
                        op("tensor", lambda e, kc=kc: e.matmul(pl[:, 0:NE], h2T[:, kc, :], wr[:, kc, :],
                                                               start=(kc == 0), stop=(kc == 7)),
                           reads=[h2T, wr], writes=[pl])
                    op("vector", lambda e: e.reduce_max(out=mx[:], in_=pl[:, 0:NE], axis=AX.X), reads=[pl], writes=[mx])
                    op("vector", lambda e: e.tensor_scalar(out=nmx[:], in0=mx[:], scalar1=-1.0, scalar2=None,
                                                           op0=ALU.mult), reads=[mx], writes=[nmx])
                    op("scalar", lambda e: e.activation(out=ex[:], in_=pl[:, 0:NE], func=AF.Exp, bias=nmx[:],
                                                        accum_out=sm[:]), reads=[pl, nmx], writes=[ex, sm])
                    op("vector", lambda e: e.reciprocal(out=rsm[:], in_=sm[:]), reads=[sm], writes=[rsm])
                    op("vector", lambda e: e.tensor_scalar(out=af[:], in0=ex[:], scalar1=rsm[:, 0:1],
                                                           scalar2=None, op0=ALU.mult),
                       reads=[ex, rsm], writes=[af])
                    dma("sync", lambda e: e.dma_start(out=AFF[r0:r0 + 128, :], in_=af[:]), reads=[af])

                p5_loadB(0)
                p5_loadG(0)
                gq = 0
                for gi, (tok0, GW, v) in enumerate(agroups):
                    ntile = GW // 128
                    Bt = Bt2[gi % 2]
                    if gi + 1 < len(agroups):
                        p5_loadB(gi + 1)
                    for oc in range(8):
                        Gt = Gt3[gq % 3]
                        if gq + 1 < len(gseq):
                            p5_loadG(gq + 1)
                        gq += 1
                        osl = slice(oc * 128, (oc + 1) * 128)
                        pA, pS_, pF = PS[0 + (oc % 2) * 3], PS[1 + (oc % 2) * 3], PS[2 + (oc % 2) * 3]
                        for (pp, h0, nh) in ((pA, 0, 8), (pS_, 8, 4), (pF, 12, 4)):
                            for hh in range(nh):
                                op("tensor", lambda e, pp=pp, h0=h0, hh=hh, nh=nh, osl=osl: e.matmul(
                                    pp[:, 0:GW], wb[:, h0 + hh, osl], Bt[:, h0 + hh, 0:GW],
                                    start=(hh == 0), stop=(hh == nh - 1)), reads=[wb, Bt], writes=[pp])
                        op("vector", lambda e, pA=pA, Gt=Gt: e.tensor_tensor(out=m1[:, 0:GW], in0=pA[:, 0:GW],
                                                                             in1=Gt[:, 0, 0:GW], op=ALU.mult),
                           reads=[pA, Gt], writes=[m1])
                        op("vector", lambda e, pS_=pS_, Gt=Gt: e.tensor_tensor(out=m2[:, 0:GW], in0=pS_[:, 0:GW],
                                                                               in1=Gt[:, 1, 0:GW], op=ALU.mult),
                           reads=[pS_, Gt], writes=[m2])
                        op("vector", lambda e, pF=pF, Gt=Gt: e.tensor_tensor(out=m3[:, 0:GW], in0=pF[:, 0:GW],
                                                                             in1=Gt[:, 2, 0:GW], op=ALU.mult),
                           reads=[pF, Gt], writes=[m3])
                        op("gpsimd", lambda e: e.tensor_tensor(out=m1[:, 0:GW], in0=m1[:, 0:GW], in1=m2[:, 0:GW],
                                                               op=ALU.add), reads=[m1, m2], writes=[m1])
                        op("gpsimd", lambda e, oc=oc: e.tensor_tensor(out=mT[:, oc, 0:GW], in0=m1[:, 0:GW],
                                                                      in1=m3[:, 0:GW], op=ALU.add),
                           reads=[m1, m3], writes=[mT])
                    pend = None
                    for j in range(ntile):
                        r0 = tok0 + j * 128
                        xt = xt2[tix % 2]
                        xm = xm2[tix % 2]
                        h2b = h2b2[tix % 2]
                        af = af2[tix % 2]
                        h2f = h2f2[tix % 2]
                        pob = (tix % 2) * 2
                        tix += 1
                        load_f32(xt[:], xt, R[r0:r0 + 128, :])
                        for hf in range(2):
                            hs = slice(hf * 512, (hf + 1) * 512)
                            po = PS[pob + hf]
                            for kc in range(8):
                                op("tensor", lambda e, kc=kc, j=j, hs=hs, po=po: e.matmul(
                                    po[:, :], mT[:, kc, j * 128:(j + 1) * 128], wo[:, kc, hs],
                                    start=(kc == 0), stop=(kc == 7)), reads=[mT, wo], writes=[po])
                            op("vector", lambda e, po=po, hs=hs: e.tensor_tensor(out=tt[:, hs], in0=po[:, :],
                                                                                 in1=rw[(2, v)][:, hs], op=ALU.mult),
                               reads=[po, rw[(2, v)]], writes=[tt])
                        op("gpsimd", lambda e, xm=xm, xt=xt: e.tensor_tensor(out=xm[:], in0=tt[:], in1=xt[:], op=ALU.add),
                           reads=[tt, xt], writes=[xm])
                        dma("sync", lambda e, xm=xm, r0=r0: e.dma_start(out=R[r0:r0 + 128, :], in_=xm[:]), reads=[xm])
                        rms_rstd(ph, xm, ss, sq, rstd, junk)
                        op("vector", lambda e, xm=xm: e.scalar_tensor_tensor(
                            out=tt[:], in0=xm[:], scalar=rstd[:, 0:1], op0=ALU.mult, in1=rw[(3, v)][:], op1=ALU.mult),
                           reads=[xm, rstd, rw[(3, v)]], writes=[tt])
                        op("vector", lambda e, h2f=h2f: e.tensor_tensor(out=h2f[:], in0=tt[:], in1=rw[(4, v)][:],
                                                                        op=ALU.add),
                           reads=[tt, rw[(4, v)]], writes=[h2f])
                        op("gpsimd", lambda e, h2b=h2b, h2f=h2f: e.tensor_copy(out=h2b[:], in_=h2f[:]), reads=[h2f],
                           writes=[h2b])
                        dma("sync", lambda e, h2b=h2b, r0=r0: e.dma_start(out=H2[r0:r0 + 128, :], in_=h2b[:]), reads=[h2b])
                        if pend is not None:
                            stageB(pend)
                        pend = (h2f, af, r0)
                    if pend is not None:
                        stageB(pend)
                k.barrier()

            with Phase() as ph:
                if STOP < 5:
                    raise SkipPhase()
                def bisect(A3, np_, inner, cap, sumfn, tag):
                    lo = k.sb(ph, [np_, NE], F32, "lo" + tag)
                    hi = k.sb(ph, [np_, NE], F32, "hi" + tag)
                    mid = k.sb(ph, [np_, NE], F32, "mid" + tag)
                    Mk = k.sb(ph, [np_, inner, NE], F32, "Mk" + tag)
                    cnt = k.sb(ph, [np_, NE], F32, "cnt" + tag)
                    ge = k.sb(ph, [np_, NE], F32, "ge" + tag)
                    tmp = k.sb(ph, [np_, NE], F32, "tmp" + tag)
                    op("vector", lambda e: e.memset(lo[:], 0.0), writes=[lo])
                    op("vector", lambda e: e.memset(hi[:], 1.0), writes=[hi])
                    pc = PS[0]
                    for it in range(34):
                        op("vector", lambda e: e.tensor_tensor(out=mid[:], in0=lo[:], in1=hi[:], op=ALU.add),
                           reads=[lo, hi], writes=[mid])
                        op("vector", lambda e: e.tensor_scalar(out=mid[:], in0=mid[:], scalar1=0.5, scalar2=None,
                                                               op0=ALU.mult), reads=[mid], writes=[mid])
                        op("vector", lambda e: e.tensor_tensor(out=Mk[:], in0=A3,
                                                               in1=mid[:].unsqueeze(1).to_broadcast([np_, inner, NE]),
                                                               op=ALU.is_ge), reads=[mid, Abuf], writes=[Mk])
                        op("vector", lambda e: e.tensor_reduce(out=cnt[:], in_=Mk[:].rearrange("p i e -> p e i"),
                                                               axis=AX.X, op=ALU.add), reads=[Mk], writes=[cnt])
                        op("tensor", lambda e: e.matmul(pc[0:np_, 0:NE], C["ones_f"][0:np_, 0:np_], cnt[:],
                                                        start=True, stop=True), reads=[cnt, C["ones_f"]], writes=[pc])
                        op("vector", lambda e: e.tensor_scalar(out=ge[:], in0=pc[0:np_, 0:NE], scalar1=float(cap),
                                                               scalar2=None, op0=ALU.is_ge), reads=[pc], writes=[ge])
                        op("vector", lambda e: e.tensor_tensor(out=tmp[:], in0=ge[:], in1=mid[:], op=ALU.mult),
                           reads=[ge, mid], writes=[tmp])
                        op("vector", lambda e: e.tensor_tensor(out=lo[:], in0=lo[:], in1=tmp[:], op=ALU.max),
                           reads=[lo, tmp], writes=[lo])
                        op("vector", lambda e: e.scalar_tensor_tensor(out=tmp[:], in0=ge[:], scalar=2.0, op0=ALU.mult,
                                                                      in1=mid[:], op1=ALU.add),
                           reads=[ge, mid], writes=[tmp])
                        op("vector", lambda e: e.tensor_tensor(out=hi[:], in0=hi[:], in1=tmp[:], op=ALU.min),
                           reads=[hi, tmp], writes=[hi])
                    return lo, Mk

                A = k.sb(ph, [NB, 128, NE], F32, "A")
                Abuf = A
                load_f32(A[:], A, AFF[0:NL, :].rearrange("(t i) e -> t i e", i=128))
                thr, Mk = bisect(A[:], NB, 128, CAP, None, "l")
                op("vector", lambda e: e.tensor_tensor(out=Mk[:], in0=A[:],
                                                       in1=thr[:].unsqueeze(1).to_broadcast([NB, 128, NE]), op=ALU.is_ge),
                   reads=[A, thr], writes=[Mk])
                Mk2 = k.sb(ph, [NB, 128, NE], F32, "Mk2")
                cur, oth = Mk, Mk2
                dstep = 1
                while dstep < 128:
                    op("vector", lambda e, cur=cur, oth=oth, d=dstep: e.tensor_copy(out=oth[:, 0:d, :], in_=cur[:, 0:d, :]),
                       reads=[cur], writes=[oth])
                    op("vector", lambda e, cur=cur, oth=oth, d=dstep: e.tensor_tensor(
                        out=oth[:, d:128, :], in0=cur[:, d:128, :], in1=cur[:, 0:128 - d, :], op=ALU.add),
                       reads=[cur], writes=[oth])
                    cur, oth = oth, cur
                    dstep *= 2
                CL = cur
                CLe = k.sb(ph, [NB, NE, 128], F32, "CLe")
                op("vector", lambda e: e.tensor_copy(out=CLe[:], in_=CL[:].rearrange("p i e -> p e i")), reads=[CL],
                   writes=[CLe])
                cntl = k.sb(ph, [NB, NE], F32, "cntl")
                op("vector", lambda e: e.tensor_copy(out=cntl[:], in_=CL[:, 127, :]), reads=[CL], writes=[cntl])
                pe_ = PS[1]
                op("tensor", lambda e: e.matmul(pe_[0:NB, 0:NE], C["ustrict"][0:NB, 0:NB], cntl[:], start=True, stop=True),
                   reads=[cntl, C["ustrict"]], writes=[pe_])
                meta = k.sb(ph, [NB, NE, 2], F32, "meta")
                INC = k.sb(ph, [NB, NE], F32, "INC")
                op("vector", lambda e: e.tensor_copy(out=meta[:, :, 0], in_=pe_[0:NB, 0:NE]), reads=[pe_], writes=[meta])
                op("vector", lambda e: e.tensor_copy(out=meta[:, :, 1], in_=C["pcol"][0:NB, 0:1].to_broadcast([NB, NE])),
                   reads=[C["pcol"]], writes=[meta])
                op("vector", lambda e: e.tensor_tensor(out=INC[:], in0=meta[:, :, 0], in1=cntl[:], op=ALU.add),
                   reads=[meta, cntl], writes=[INC])
                oh1 = k.sb(ph, [NB, 128], F32, "oh1")
                oh2 = [k.sb(ph, [NB, 128], F32, "oh") for _ in range(2)]
                rr = k.sb(ph, [128, 1], F32, "rr")
                Vt = k.sb(ph, [NB, 128], F32, "Vt")
                il = k.sb(ph, [128, 1], F32, "il")
                idf = k.sb(ph, [128, 1], F32, "idf")
                jk = k.sb(ph, [128, 128], F32, "jk")
                def p6_front(ii, ex_, S):
                    oh = oh2[ii % 2]
                    pp = PS[2 + (ii % 2)]
                    io = C["iota"][0:NB, S * SP:S * SP + SP]
                    op("vector", lambda e: e.tensor_scalar(
                        out=oh1[:, 0:SP], in0=io, scalar1=meta[:, ex_, 0:1], scalar2=None, op0=ALU.is_ge),
                       reads=[C["iota"], meta], writes=[oh1])
                    op("vector", lambda e: e.scalar_tensor_tensor(
                        out=oh[:, 0:SP], in0=io, scalar=INC[:, ex_:ex_ + 1], op0=ALU.is_lt, in1=oh1[:, 0:SP],
                        op1=ALU.mult), reads=[C["iota"], INC, oh1], writes=[oh])
                    op("tensor", lambda e: e.matmul(
                        pp[0:SP, 0:128], oh[:, 0:SP], CLe[:, ex_, :], start=True, stop=True),
                       reads=[oh, CLe], writes=[pp])
                    op("tensor", lambda e: e.matmul(
                        pp[0:SP, 128:130], oh[:, 0:SP], meta[:, ex_, :], start=True, stop=True),
                       reads=[oh, meta], writes=[pp])
                    op("vector", lambda e: e.scalar_tensor_tensor(
                        out=Vt[:, 0:SP], in0=io, scalar=meta[:, ex_, 0:1], op0=ALU.subtract, in1=oh[:, 0:SP],
                        op1=ALU.mult), reads=[C["iota"], meta, oh], writes=[Vt])
                    op("tensor", lambda e: e.matmul(
                        pp[0:SP, 130:131], Vt[:, 0:SP], C["ones_f"][0:NB, 0:1], start=True, stop=True),
                       reads=[Vt, C["ones_f"]], writes=[pp])

                def p6_back(ii, ex_, S):
                    pp = PS[2 + (ii % 2)]
                    op("vector", lambda e: e.tensor_copy(out=rr[0:SP, :], in_=pp[0:SP, 130:131]),
                       reads=[pp], writes=[rr])
                    op("vector", lambda e: e.tensor_scalar(
                        out=jk[0:SP, :], in0=pp[0:SP, 0:128], scalar1=rr[0:SP, 0:1], scalar2=0.0, op0=ALU.is_le,
                        op1=ALU.add, accum_out=il[0:SP, :]), reads=[pp, rr], writes=[jk, il])
                    op("vector", lambda e: e.scalar_tensor_tensor(
                        out=idf[0:SP, :], in0=pp[0:SP, 129:130], scalar=128.0, op0=ALU.mult, in1=il[0:SP, :],
                        op1=ALU.add), reads=[pp, il], writes=[idf])
                    col = ex_ * NS + S
                    op("vector", lambda e: e.tensor_copy(out=idx_t[0:SP, col:col + 1], in_=idf[0:SP, :]),
                       reads=[idf], writes=[idx_t])

                p6items = [(ii, ex_, S) for ii, (ex_, S) in enumerate((a, b) for a in range(NE) for b in range(NS))]
                p6_front(*p6items[0])
                for n_, it_ in enumerate(p6items):
                    if n_ + 1 < len(p6items):
                        p6_front(*p6items[n_ + 1])
                    p6_back(*it_)
                if not last:
                    Ac = k.sb(ph, [128, 2, NE], F32, "Ac")
                    Abuf = Ac
                    load_f32(Ac[:], Ac, AFF[NL:NT, :].rearrange("(t i) e -> i t e", i=128))
                    thc, Mc = bisect(Ac[:], 128, 2, 32, None, "c")
                    op("vector", lambda e: e.tensor_tensor(out=Mc[:], in0=Ac[:],
                                                           in1=thc[:].unsqueeze(1).to_broadcast([128, 2, NE]),
                                                           op=ALU.is_ge), reads=[Ac, thc], writes=[Mc])
                    op("vector", lambda e: e.tensor_tensor(out=gwc[:], in0=Mc[:], in1=Ac[:], op=ALU.mult),
                       reads=[Mc, Ac], writes=[gwc])
                k.barrier()

            with Phase() as ph:
                if STOP < 6:
                    raise SkipPhase()
                cast_engs[:] = ["vector", "scalar"]
                wg2 = k.sb(ph, [128, 8, FF], BF16, "wg")
                wu2 = k.sb(ph, [128, 8, FF], BF16, "wu")
                wd2 = k.sb(ph, [128, 16, D], BF16, "wd")
                rw = load_rows(ph, [5])
                xe2 = [k.sb(ph, [128, D], BF16, "xe") for _ in range(4)]
                ae2 = [k.sb(ph, [128, NE], F32, "ae") for _ in range(8)]
                xeT = k.sb(ph, [128, 8, 512], BF16, "xeT")
                sa = k.sb(ph, [128, 512], F32, "sa")
                hTt = k.sb(ph, [128, 16, 512], BF16, "hTt")
                y2 = [k.sb(ph, [128, D], F32, "y") for _ in range(2)]
                cacc = k.sb(ph, [128, 2, D], F32, "cacc")
                op("gpsimd", lambda e: e.memset(cacc[:], 0.0), writes=[cacc])
                Rbuf = Buf("Rscatter")
                tgroups = []
                for s0 in range(0, NS, 4):
                    tgroups.append([("L", s) for s in range(s0, min(NS, s0 + 4))])
                if not last:
                    tgroups.append([("C", 0), ("C", 1)])
                yi = 0
                work = [(e_, tg_) for e_ in range(NE) for tg_ in tgroups]
                fetched = {}

                def fetch(wi):
                    e_, tg_ = work[wi]
                    out = []
                    for j, (kind, s_) in enumerate(tg_):
                        xe = xe2[j % 4]
                        ae = ae2[(wi % 2) * 4 + (j % 4)]
                        if kind == "L":
                            col = e_ * NS + s_
                            off = IndirectOffsetOnAxis(ap=idx_t[0:SP, col:col + 1], axis=0)
                            dma("gpsimd", lambda e, xe=xe, off=off: e.indirect_dma_start(
                                out=xe[0:SP, :], out_offset=None, in_=H2[:, :], in_offset=off),
                                reads=[idx_t], writes=[xe])
                            dma("gpsimd", lambda e, ae=ae, off=off: e.indirect_dma_start(
                                out=ae[0:SP, :], out_offset=None, in_=AFF[:, :], in_offset=off),
                                reads=[idx_t], writes=[ae])
                        else:
                            load_f32(xe[:], xe, H2[NL + s_ * 128:NL + (s_ + 1) * 128, :])
                        out.append((xe, ae))
                    fetched[wi] = out

                fetch(0)
                wi_ = -1
                for ex_ in range(NE):
                    for kc in range(8):
                        load_cast(wg2[:, kc, :], wg2, w_g[L, ex_, kc * 128:(kc + 1) * 128, :], [128, FF])
                        load_cast(wu2[:, kc, :], wu2, w_u[L, ex_, kc * 128:(kc + 1) * 128, :], [128, FF])
                    for kp in range(8):
                        load_cast(wd2[:, 2 * kp:2 * kp + 2, :], wd2,
                                  w_d[L, ex_, kp * 256:(kp + 1) * 256, :].rearrange("(a p) n -> p a n", p=128),
                                  [128, 2, D])
                    for tg in tgroups:
                        wi_ += 1
                        GWt = 0
                        gates_ = []
                        for j, (kind, s) in enumerate(tg):
                            xe, ae = fetched[wi_][j]
                            if kind == "L":
                                np_ = SP
                                col = ex_ * NS + s
                                gates_.append((ae, ae[0:SP, ex_:ex_ + 1], np_, kind, col))
                            else:
                                np_ = 128
                                gates_.append((gwc, gwc[:, s, ex_:ex_ + 1], np_, kind, s))
                            pt = PS[j % 2]
                            ptv = pt[:, :].bitcast(BF16)
                            for kc in range(8):
                                op("tensor", lambda e, kc=kc, ptv=ptv, xe=xe, np_=np_: e.transpose(
                                    ptv[:, kc * 128:kc * 128 + np_], xe[0:np_, kc * 128:(kc + 1) * 128],
                                    C["ident_bf"][0:np_, 0:np_]), reads=[xe, C["ident_bf"]], writes=[pt])
                            op("scalar", lambda e, ptv=ptv, np_=np_, GWt=GWt: e.copy(
                                out=xeT[:, :, GWt:GWt + np_],
                                in_=ptv.rearrange("p (k t) -> p k t", k=8)[:, :, 0:np_]), reads=[pt], writes=[xeT])
                            gates_[-1] = gates_[-1] + (GWt,)
                            GWt += np_
                        for fc in range(16):
                            fsl = slice(fc * 128, (fc + 1) * 128)
                            pa = PS[2 + (fc % 2) * 2]
                            pu = PS[3 + (fc % 2) * 2]
                            for kc in range(8):
                                op("tensor", lambda e, kc=kc, fsl=fsl, pa=pa: e.matmul(
                                    pa[:, 0:GWt], wg2[:, kc, fsl], xeT[:, kc, 0:GWt], start=(kc == 0), stop=(kc == 7)),
                                   reads=[wg2, xeT], writes=[pa])
                            for kc in range(8):
                                op("tensor", lambda e, kc=kc, fsl=fsl, pu=pu: e.matmul(
                                    pu[:, 0:GWt], wu2[:, kc, fsl], xeT[:, kc, 0:GWt], start=(kc == 0), stop=(kc == 7)),
                                   reads=[wu2, xeT], writes=[pu])
                            op("scalar", lambda e, pa=pa: e.activation(out=sa[:, 0:GWt], in_=pa[:, 0:GWt], func=AF.Silu),
                               reads=[pa], writes=[sa])
                            op("vector", lambda e, pu=pu, fc=fc: e.tensor_tensor(out=hTt[:, fc, 0:GWt], in0=pu[:, 0:GWt],
                                                                                 in1=sa[:, 0:GWt], op=ALU.mult),
                               reads=[pu, sa], writes=[hTt])
                        if wi_ + 1 < len(work):
                            fetch(wi_ + 1)
                        for (gt_, gap, np_, kind, ref_, g0) in gates_:
                            y = y2[yi % 2]
                            yi += 1
                            v = 0 if kind == "L" else 1
                            for hf in range(2):
                                hs = slice(hf * 512, (hf + 1) * 512)
                                py = PS[6 + hf]
                                for fc in range(16):
                                    op("tensor", lambda e, fc=fc, hs=hs, py=py, g0=g0, np_=np_: e.matmul(
                                        py[0:np_, :], hTt[:, fc, g0:g0 + np_], wd2[:, fc, hs],
                                        start=(fc == 0), stop=(fc == 15)), reads=[hTt, wd2], writes=[py])
                                op("vector", lambda e, py=py, hs=hs, y=y, gap=gap, np_=np_, v=v: e.scalar_tensor_tensor(
                                    out=y[0:np_, hs], in0=py[0:np_, :], scalar=gap, op0=ALU.mult,
                                    in1=rw[(5, v)][0:np_, hs], op1=ALU.mult), reads=[py, gt_, rw[(5, v)]], writes=[y])
                            if kind == "L":
                                off = IndirectOffsetOnAxis(ap=idx_t[0:SP, ref_:ref_ + 1], axis=0)
                                dma("gpsimd", lambda e, y=y, off=off: e.indirect_dma_start(
                                    out=R[:, :], out_offset=off, in_=y[0:SP, :], in_offset=None, compute_op=ALU.add),
                                    reads=[y, idx_t], writes=[Rbuf])
                            else:
                                op("gpsimd", lambda e, y=y, ref_=ref_: e.tensor_tensor(
                                    out=cacc[:, ref_, :], in0=cacc[:, ref_, :], in1=y[:], op=ALU.add),
                                   reads=[y, cacc], writes=[cacc])
                cast_engs[:] = ["gpsimd", "vector", "scalar"]
                if not last:
                    for s in range(2):
                        xt = y2[s]
                        load_f32(xt[:], xt, R[NL + s * 128:NL + (s + 1) * 128, :])
                        op("gpsimd", lambda e, xt=xt, s=s: e.tensor_tensor(out=xt[:], in0=xt[:], in1=cacc[:, s, :],
                                                                           op=ALU.add), reads=[xt, cacc], writes=[xt])
                        dma("sync", lambda e, xt=xt, s=s: e.dma_start(out=R[NL + s * 128:NL + (s + 1) * 128, :], in_=xt[:]),
                            reads=[xt])
                k.barrier()

        with contextlib.ExitStack() as ph:
            gfr = k.sb(ph, [128, D], F32, "gfr")
            load_f32(gfr[:], gfr, g_fin.partition_broadcast(128))
            xt2 = [k.sb(ph, [128, D], F32, "xt") for _ in range(2)]
            yo2 = [k.sb(ph, [128, D], F32, "yo") for _ in range(2)]
            junk = k.sb(ph, [128, D], F32, "junk")
            ss = k.sb(ph, [128, 1], F32, "ss")
            sq = k.sb(ph, [128, 1], F32, "sq")
            rstd = k.sb(ph, [128, 1], F32, "rstd")
            for t in range(NB):
                xt = xt2[t % 2]
                yo = yo2[t % 2]
                load_f32(xt[:], xt, R[t * 128:(t + 1) * 128, :])
                rms_rstd(ph, xt, ss, sq, rstd, junk)
                op("vector", lambda e, xt=xt, yo=yo: e.scalar_tensor_tensor(
                    out=yo[:], in0=xt[:], scalar=rstd[:, 0:1], op0=ALU.mult, in1=gfr[:], op1=ALU.mult),
                   reads=[xt, rstd, gfr], writes=[yo])
                dma("sync", lambda e, yo=yo, t=t: e.dma_start(out=y_out[t * 128:(t + 1) * 128, :], in_=yo[:]), reads=[yo])
            k.barrier()
    return nc, consts


def _prep_inputs(inp, NB, DEPTH):
    f = lambda a: np.ascontiguousarray(np.asarray(a, dtype=np.float32))
    qperm = []
    for c in range(4):
        qperm += list(range(c * 64, c * 64 + 64)) + list(range((4 + c) * 64, (4 + c) * 64 + 64))
    cols = qperm + list(range(512, 768)) + list(range(1024, 1280)) + list(range(1280, 1536)) + \
        list(range(768, 1024)) + list(range(1536, 4608))
    cols = np.asarray(cols)
    w_in = f(inp["w_in"])[:DEPTH][:, :, cols]
    fm = lambda v, n: f(np.asarray(v).reshape(v.shape[0], n, 128).transpose(0, 2, 1))
    shared = {
        "w_mod": f(inp["w_mod"])[:DEPTH],
        "b_modfm": fm(np.asarray(inp["b_mod"])[:DEPTH], 48),
        "g_mixfm": fm(np.asarray(inp["g_mix"])[:DEPTH], 8),
        "g_ffnfm": fm(np.asarray(inp["g_ffn"])[:DEPTH], 8),
        "w_in": f(w_in),
        "sink": f(inp["attn_sink"])[:DEPTH],
        "wsT": f(np.asarray(inp["w_spatial"])[:DEPTH].transpose(0, 3, 1, 2)),
        "b_sp": f(np.asarray(inp["b_spatial"])[:DEPTH].reshape(DEPTH, 512)),
        "w_ba": f(np.asarray(inp["w_branch_attn"])[:DEPTH].reshape(DEPTH, 8, 64, D).transpose(0, 2, 1, 3)),
        "w_bs": f(np.asarray(inp["w_branch_sgu"])[:DEPTH].reshape(DEPTH, 4, 64, D).transpose(0, 2, 1, 3)),
        "w_bf": f(np.asarray(inp["w_branch_fourier"])[:DEPTH].reshape(DEPTH, 4, 64, D).transpose(0, 2, 1, 3)),
        "w_o": f(inp["w_out"])[:DEPTH],
        "w_r": f(inp["w_router"])[:DEPTH],
        "w_g": f(inp["w_gate"])[:DEPTH],
        "w_u": f(inp["w_up"])[:DEPTH],
        "w_d": f(inp["w_down"])[:DEPTH],
        "g_fin": f(inp["g_final"]),
    }
    return shared


def run(inp, NB, DEPTH, dbg=False):
    nc, consts = build(NB, DEPTH, dbg)
    shared = _prep_inputs(inp, NB, DEPTH)
    if STOP < 6:
        for nm in ("w_g", "w_u", "w_d"):
            shared[nm] = np.zeros((1, 1, 8, 8), np.float32)
    for kname, v in consts.items():
        shared["c_" + kname] = np.ascontiguousarray(v, dtype=np.float32)
    x = np.asarray(inp["x"], dtype=np.float32)
    ctx = np.asarray(inp["ctx"], dtype=np.float32)
    c = np.asarray(inp["c"], dtype=np.float32)
    cc = np.asarray(inp["c_ctx"], dtype=np.float32)
    B = x.shape[0]
    in_maps = []
    for b in range(B):
        m = dict(shared)
        m["x"] = np.ascontiguousarray(x[b])
        m["ctx"] = np.ascontiguousarray(ctx[b])
        cv = np.stack([c[b].reshape(8, 128).T, cc.reshape(8, 128).T], axis=-1)
        m["cvec"] = np.ascontiguousarray(cv, dtype=np.float32)
        in_maps.append(m)
    res = run_bass_kernel_spmd(nc, in_maps, core_ids=list(range(B)))
    return res


def kernel(**inputs):
    NB = np.asarray(inputs["x"]).shape[1] // 128
    DEPTH = np.asarray(inputs["w_mod"]).shape[0]
    res = run(inputs, NB, DEPTH)
    out = np.stack([np.asarray(r["y"], dtype=np.float32) for r in res.results], axis=0)
    return out
```
